# Optimizing a Trainium2 kernel written in Bass

```python
import jax
import jax.numpy as jnp
from jax import lax
import numpy as np

D_MODEL = 2048
BATCH = 16
SEQ = 2048
DEPTH = 4

CTX_LEN = 256
GRID_W = 64
HEAD_DIM = 128
N_GROUPS = 4
GROUP_WIDTH = D_MODEL // N_GROUPS
D_MIX = N_GROUPS * GROUP_WIDTH
Q_BLOCK = 128
ROPE_THETA = 10000.0
NORM_EPS = 1e-6
NEG_INF = -1e30

MLA_HEADS = GROUP_WIDTH // HEAD_DIM
MLA_NOPE = HEAD_DIM
MLA_ROPE = HEAD_DIM // 2
MLA_V = HEAD_DIM
MLA_Q_RANK = 3 * D_MODEL // 16
MLA_KV_RANK = D_MODEL // 8
GQA_HEADS = GROUP_WIDTH // HEAD_DIM
GQA_KV_HEADS = GQA_HEADS // 2
GDN_HEADS = GROUP_WIDTH // HEAD_DIM
GDN_CONV = 5
GDN_CHUNK = 64
SWA_HEADS = GROUP_WIDTH // HEAD_DIM
SWA_KV_HEADS = SWA_HEADS // 2
SWA_WINDOW = 128
FFN_DIM = 5632
N_EXPERTS = 8
TOP_K = 2
EXPERT_DIM = 2 * D_MODEL

IN_SPLITS = (
    MLA_Q_RANK, MLA_KV_RANK, MLA_ROPE,
    GQA_HEADS * HEAD_DIM, GQA_KV_HEADS * HEAD_DIM, GQA_KV_HEADS * HEAD_DIM,
    GDN_HEADS * HEAD_DIM, GDN_HEADS * HEAD_DIM, GDN_HEADS * HEAD_DIM,
    GDN_HEADS * HEAD_DIM, 2 * GDN_HEADS, 2 * GDN_HEADS,
    SWA_HEADS * HEAD_DIM, SWA_KV_HEADS * HEAD_DIM, SWA_KV_HEADS * HEAD_DIM,
)
D_IN = sum(IN_SPLITS)

kernel_name = 'hybrid_parallel_group_flow_backbone'

F32 = jnp.float32


def _rmsnorm(x, g):
    xf = x.astype(F32)
    y = xf * lax.rsqrt(jnp.mean(xf * xf, axis=-1, keepdims=True) + NORM_EPS)
    return (y * g.astype(F32)).astype(x.dtype)


def _l2norm(x):
    return x * lax.rsqrt(jnp.sum(x * x, axis=-1, keepdims=True) + 1e-6)


def _modulate(h, shift, scale):
    return h * (1.0 + scale) + shift


def _split_in(p):
    offsets = np.cumsum(IN_SPLITS)[:-1].tolist()
    return jnp.split(p, offsets, axis=-1)


def _axial_rope_tables(n_tokens, rot_dim):
    rows = n_tokens // GRID_W
    row = jnp.repeat(jnp.arange(rows), GRID_W).astype(F32)
    col = jnp.tile(jnp.arange(GRID_W), rows).astype(F32)
    half = rot_dim // 2
    inv_freq = ROPE_THETA ** (-jnp.arange(0, half, 2, dtype=F32) / half)
    ang_r = row[:, None] * inv_freq
    ang_c = col[:, None] * inv_freq
    ang = jnp.concatenate([ang_r, ang_r, ang_c, ang_c], axis=-1)
    return jnp.cos(ang), jnp.sin(ang)


def _rotate_half_axial(x):
    q = x.shape[-1] // 4
    x1, x2, x3, x4 = x[..., :q], x[..., q:2 * q], x[..., 2 * q:3 * q], x[..., 3 * q:]
    return jnp.concatenate([-x2, x1, -x4, x3], axis=-1)


def _apply_rope(x, cos, sin):
    shape = (1, x.shape[1]) + (1,) * (x.ndim - 3) + (x.shape[-1],)
    cos = cos.reshape(shape).astype(x.dtype)
    sin = sin.reshape(shape).astype(x.dtype)
    return x * cos + _rotate_half_axial(x) * sin


def _flat(y):
    return None if y is None else y.reshape(y.shape[:2] + (-1,))


def _attend(q, k, v, scale):
    s = jnp.einsum('bqhgd,bkhd->bhgqk', q, k, preferred_element_type=F32) * scale
    p = jax.nn.softmax(s, axis=-1).astype(v.dtype)
    return jnp.einsum('bhgqk,bkhd->bqhgd', p, v)


def _attend_query_blocks(q, k, v, scale):
    B, S = q.shape[:2]
    nb = S // Q_BLOCK
    qb = jnp.moveaxis(q.reshape((B, nb, Q_BLOCK) + q.shape[2:]), 1, 0)
    ob = lax.map(lambda blk: _attend(blk, k, v, scale), qb)
    return jnp.moveaxis(ob, 0, 1).reshape((B, S) + ob.shape[3:])


def _softmax_with_sink(s, sink):
    m = jnp.maximum(jnp.max(s, axis=-1, keepdims=True), sink)
    e = jnp.exp(s - m)
    return e / (jnp.sum(e, axis=-1, keepdims=True) + jnp.exp(sink - m))


def _banded_sink_attention(q, k, v, kc, vc, sink, scale):
    B, S, Hk, G, d = q.shape
    nb = S // Q_BLOCK
    W = 3 * Q_BLOCK

    def windows(t):
        pad = jnp.zeros((B, Q_BLOCK) + t.shape[2:], t.dtype)
        tb = jnp.concatenate([pad, t, pad], axis=1).reshape((B, nb + 2, Q_BLOCK) + t.shape[2:])
        return jnp.concatenate([tb[:, :-2], tb[:, 1:-1], tb[:, 2:]], axis=2)

    kw, vw = windows(k), windows(v)
    qb = q.reshape(B, nb, Q_BLOCK, Hk, G, d)
    s_loc = jnp.einsum('bnqhgd,bnkhd->bhgnqk', qb, kw, preferred_element_type=F32) * scale
    q_pos = jnp.arange(nb)[:, None, None] * Q_BLOCK + jnp.arange(Q_BLOCK)[None, :, None]
    k_pos = jnp.arange(nb)[:, None, None] * Q_BLOCK - Q_BLOCK + jnp.arange(W)[None, None, :]
    valid = (jnp.abs(k_pos - q_pos) <= SWA_WINDOW) & (k_pos >= 0) & (k_pos < S)
    s_loc = jnp.where(valid, s_loc, NEG_INF)
    s_ctx = jnp.einsum('bnqhgd,bkhd->bhgnqk', qb, kc, preferred_element_type=F32) * scale
    p = _softmax_with_sink(jnp.concatenate([s_loc, s_ctx], axis=-1),
                           sink[:, :, None, None, None]).astype(v.dtype)
    o = (jnp.einsum('bhgnqk,bnkhd->bnqhgd', p[..., :W], vw)
         + jnp.einsum('bhgnqk,bkhd->bnqhgd', p[..., W:], vc))
    return o.reshape(B, S, Hk, G, d)


def _centred_depthwise_conv(x, w):
    K = w.shape[0]
    return lax.conv_general_dilated(
        x, w[:, None, :].astype(x.dtype), window_strides=(1,),
        padding=[((K - 1) // 2, (K - 1) // 2)],
        dimension_numbers=('NWC', 'WIO', 'NWC'), feature_group_count=x.shape[-1])


def _gated_delta_chunked(q, k, v, g, beta, state0):
    B, T, H, dk = q.shape
    dv = v.shape[-1]
    C = GDN_CHUNK
    n = T // C

    def chunks(t):
        return jnp.moveaxis(t.reshape((B, n, C, H) + t.shape[3:]), 3, 1)

    q = chunks(q * dk ** -0.5)
    k = chunks(k)
    v = chunks(v)
    g = jnp.cumsum(chunks(g), axis=-1)
    beta = chunks(beta)
    kb = k * beta[..., None]
    tri = jnp.tril(jnp.ones((C, C), bool))
    strict = jnp.tril(jnp.ones((C, C), bool), -1)
    diff = g[..., :, None] - g[..., None, :]
    decay = jnp.where(tri, jnp.exp(jnp.where(tri, diff, 0.0)), 0.0)
    lower = jnp.where(strict, jnp.einsum('bhncd,bhnjd->bhncj', kb, k) * decay, 0.0)
    a = lower + jnp.eye(C, dtype=lower.dtype)
    rhs = jnp.concatenate([v * beta[..., None], kb * jnp.exp(g)[..., None]], axis=-1)
    sol = lax.linalg.triangular_solve(a, rhs, left_side=True, lower=True, unit_diagonal=True)
    u, w = sol[..., :dv], sol[..., dv:]
    intra = jnp.einsum('bhncd,bhnjd->bhncj', q, k) * decay
    xs = tuple(jnp.moveaxis(t, 2, 0) for t in (q, k, u, w, g, intra))

    def step(state, inp):
        qi, ki, ui, wi, gi, ai = inp
        v_new = ui - jnp.einsum('bhcd,bhde->bhce', wi, state)
        o = (jnp.einsum('bhcd,bhde->bhce', qi * jnp.exp(gi)[..., None], state)
             + jnp.einsum('bhcj,bhje->bhce', ai, v_new))
        g_last = gi[..., -1:]
        state = (state * jnp.exp(g_last)[..., None]
                 + jnp.einsum('bhcd,bhce->bhde', ki * jnp.exp(g_last - gi)[..., None], v_new))
        return state, o

    state, o = lax.scan(step, state0, xs)
    o = jnp.moveaxis(jnp.moveaxis(o, 0, 2), 1, 3).reshape(B, T, H, dv)
    return o, state


def _mla(pc, pl, rope, q_norm_g, kv_norm_g, w_uq, w_ukv, with_ctx):
    cos, sin = rope

    def queries(cq, rotate):
        B, T = cq.shape[:2]
        q = (_rmsnorm(cq, q_norm_g) @ w_uq).reshape(B, T, MLA_HEADS, MLA_NOPE + MLA_ROPE)
        q_nope, q_rope = q[..., :MLA_NOPE], q[..., MLA_NOPE:]
        if rotate:
            q_rope = _apply_rope(q_rope, cos, sin)
        return jnp.concatenate([q_nope, q_rope], axis=-1)[:, :, :, None, :]

    def keys_values(ckv, k_rope, rotate):
        B, T = ckv.shape[:2]
        kv = (_rmsnorm(ckv, kv_norm_g) @ w_ukv).reshape(B, T, MLA_HEADS, MLA_NOPE + MLA_V)
        if rotate:
            k_rope = _apply_rope(k_rope, cos, sin)
        k_rope = jnp.broadcast_to(k_rope[:, :, None, :], (B, T, MLA_HEADS, MLA_ROPE))
        return jnp.concatenate([kv[..., :MLA_NOPE], k_rope], axis=-1), kv[..., MLA_NOPE:]

    scale = (MLA_NOPE + MLA_ROPE) ** -0.5
    kc, vc = keys_values(pc[1], pc[2], False)
    kl, vl = keys_values(pl[1], pl[2], True)
    yl = _attend_query_blocks(queries(pl[0], True), jnp.concatenate([kc, kl], axis=1),
                              jnp.concatenate([vc, vl], axis=1), scale)
    yc = _attend(queries(pc[0], False), kc, vc, scale) if with_ctx else None
    return _flat(yc), _flat(yl)


def _gqa(pc, pl, rope, q_norm_g, k_norm_g, with_ctx):
    cos, sin = rope
    G = GQA_HEADS // GQA_KV_HEADS

    def queries(qr, rotate):
        B, T = qr.shape[:2]
        q = _rmsnorm(qr.reshape(B, T, GQA_KV_HEADS, G, HEAD_DIM), q_norm_g)
        return _apply_rope(q, cos, sin) if rotate else q

    def keys_values(kr, vr, rotate):
        B, T = kr.shape[:2]
        k = _rmsnorm(kr.reshape(B, T, GQA_KV_HEADS, HEAD_DIM), k_norm_g)
        if rotate:
            k = _apply_rope(k, cos, sin)
        return k, vr.reshape(B, T, GQA_KV_HEADS, HEAD_DIM)

    scale = HEAD_DIM ** -0.5
    kc, vc = keys_values(pc[1], pc[2], False)
    kl, vl = keys_values(pl[1], pl[2], True)
    yl = _attend_query_blocks(queries(pl[0], True), jnp.concatenate([kc, kl], axis=1),
                              jnp.concatenate([vc, vl], axis=1), scale)
    yc = _attend(queries(pc[0], False), kc, vc, scale) if with_ctx else None
    return _flat(yc), _flat(yl)


def _gdn(pc, pl, conv_w, a_log, dt_bias, norm_g, with_ctx):
    def prep(q, k, v, a, b):
        B, T = q.shape[:2]
        qkv = jax.nn.silu(_centred_depthwise_conv(jnp.concatenate([q, k, v], axis=-1), conv_w)).astype(F32)
        q, k, v = jnp.split(qkv, 3, axis=-1)
        q = _l2norm(q.reshape(B, T, GDN_HEADS, HEAD_DIM))
        k = _l2norm(k.reshape(B, T, GDN_HEADS, HEAD_DIM))
        v = v.reshape(B, T, GDN_HEADS, HEAD_DIM)
        a = a.astype(F32).reshape(B, T, 2, GDN_HEADS)
        b = b.astype(F32).reshape(B, T, 2, GDN_HEADS)
        g = -jnp.exp(a_log.astype(F32)) * jax.nn.softplus(a + dt_bias.astype(F32))
        return q, k, v, g, jax.nn.sigmoid(b)

    qc, kc, vc, gc, bc = prep(pc[0], pc[1], pc[2], pc[4], pc[5])
    ql, kl, vl, gl, bl = prep(pl[0], pl[1], pl[2], pl[4], pl[5])
    zero = jnp.zeros((ql.shape[0], GDN_HEADS, HEAD_DIM, HEAD_DIM), F32)

    def rev(t):
        return jnp.flip(t, axis=1)

    oc_f, state_f = _gated_delta_chunked(qc, kc, vc, gc[:, :, 0], bc[:, :, 0], zero)
    ol_f, _ = _gated_delta_chunked(ql, kl, vl, gl[:, :, 0], bl[:, :, 0], state_f)
    oc_b, state_b = _gated_delta_chunked(rev(qc), rev(kc), rev(vc), rev(gc[:, :, 1]), rev(bc[:, :, 1]), zero)
    ol_b, _ = _gated_delta_chunked(rev(ql), rev(kl), rev(vl), rev(gl[:, :, 1]), rev(bl[:, :, 1]), state_b)

    def out(o, z):
        B, T = z.shape[:2]
        o = _rmsnorm(o, norm_g) * jax.nn.silu(z.astype(F32).reshape(B, T, GDN_HEADS, HEAD_DIM))
        return o.reshape(B, T, GROUP_WIDTH).astype(z.dtype)

    yl = out(ol_f + rev(ol_b), pl[3])
    yc = out(oc_f + rev(oc_b), pc[3]) if with_ctx else None
    return yc, yl


def _swa(pc, pl, rope, sink, with_ctx):
    cos, sin = rope
    G = SWA_HEADS // SWA_KV_HEADS
    sink = sink.astype(F32).reshape(SWA_KV_HEADS, G)

    def queries(qr, rotate):
        B, T = qr.shape[:2]
        q = qr.reshape(B, T, SWA_KV_HEADS, G, HEAD_DIM)
        return _apply_rope(q, cos, sin) if rotate else q

    def keys_values(kr, vr, rotate):
        B, T = kr.shape[:2]
        k = kr.reshape(B, T, SWA_KV_HEADS, HEAD_DIM)
        if rotate:
            k = _apply_rope(k, cos, sin)
        return k, vr.reshape(B, T, SWA_KV_HEADS, HEAD_DIM)

    scale = HEAD_DIM ** -0.5
    kc, vc = keys_values(pc[1], pc[2], False)
    kl, vl = keys_values(pl[1], pl[2], True)
    yl = _banded_sink_attention(queries(pl[0], True), kl, vl, kc, vc, sink, scale)
    yc = None
    if with_ctx:
        qc = queries(pc[0], False)
        s = jnp.einsum('bqhgd,bkhd->bhgqk', qc, kc, preferred_element_type=F32) * scale
        p = _softmax_with_sink(s, sink[:, :, None, None]).astype(vc.dtype)
        yc = jnp.einsum('bhgqk,bkhd->bqhgd', p, vc)
    return _flat(yc), _flat(yl)


def _swiglu(h, w1, w3, w2):
    return (jax.nn.silu(h @ w1) * (h @ w3)) @ w2


def _moe_swiglu(h, router, w1, w3, w2):
    B, T, D = h.shape
    t = h.reshape(-1, D)
    logits = jnp.dot(t, router, preferred_element_type=F32)
    top_v, top_i = lax.top_k(logits, TOP_K)
    gate = jax.nn.softmax(top_v, axis=-1).astype(h.dtype)
    flat = top_i.reshape(-1)
    order = jnp.argsort(flat)
    tok = order // TOP_K
    xs = t[tok]
    sizes = jnp.bincount(flat, length=N_EXPERTS).astype(jnp.int32)
    hid = jax.nn.silu(lax.ragged_dot(xs, w1, sizes)) * lax.ragged_dot(xs, w3, sizes)
    ys = lax.ragged_dot(hid, w2, sizes) * gate.reshape(-1)[order][:, None]
    return jnp.zeros_like(t).at[tok].add(ys).reshape(B, T, D)


def _channel_mixer(layer, h, ffn_w1, ffn_w3, ffn_w2, moe_router, moe_w1, moe_w3, moe_w2):
    i = layer // 2
    if layer % 2 == 0:
        return _swiglu(h, ffn_w1[i], ffn_w3[i], ffn_w2[i])
    return _moe_swiglu(h, moe_router[i], moe_w1[i], moe_w3[i], moe_w2[i])


def setup_inputs(seed: int = 0) -> dict:
    key = jax.random.key(seed)
    keys = iter(jax.random.split(key, 40))

    def normal(shape, scale):
        return jax.random.normal(next(keys), shape, jnp.float32) * scale

    def gain(shape):
        return 1.0 + normal(shape, 0.02)

    D = D_MODEL
    L = DEPTH
    n_dense = (DEPTH + 1) // 2
    n_moe = DEPTH // 2
    dt = jnp.exp(jax.random.uniform(next(keys), (L, 2, GDN_HEADS), jnp.float32,
                                    float(np.log(1e-3)), float(np.log(1e-1))))
    a_init = jax.random.uniform(next(keys), (L, 2, GDN_HEADS), jnp.float32, 1.0, 16.0)
    return {
        'x': normal((BATCH, SEQ, D), 1.0),
        'c': normal((BATCH, D), 1.0),
        'ctx': normal((BATCH, CTX_LEN, D), 1.0),
        'c_ctx': normal((D,), 1.0),
        'norm1_g': gain((L, D)),
        'norm2_g': gain((L, D)),
        'w_mod': normal((L, D, 6 * D), 0.5 * D ** -0.5),
        'b_mod': normal((L, 6 * D), 0.02),
        'w_in': normal((L, D, D_IN), D ** -0.5),
        'mla_q_norm_g': gain((L, MLA_Q_RANK)),
        'mla_kv_norm_g': gain((L, MLA_KV_RANK)),
        'mla_w_uq': normal((L, MLA_Q_RANK, MLA_HEADS * (MLA_NOPE + MLA_ROPE)), MLA_Q_RANK ** -0.5),
        'mla_w_ukv': normal((L, MLA_KV_RANK, MLA_HEADS * (MLA_NOPE + MLA_V)), MLA_KV_RANK ** -0.5),
        'gqa_q_norm_g': gain((L, HEAD_DIM)),
        'gqa_k_norm_g': gain((L, HEAD_DIM)),
        'gdn_conv_w': normal((L, GDN_CONV, 3 * GDN_HEADS * HEAD_DIM), GDN_CONV ** -0.5),
        'gdn_a_log': jnp.log(a_init),
        'gdn_dt_bias': dt + jnp.log(-jnp.expm1(-dt)),
        'gdn_norm_g': gain((L, HEAD_DIM)),
        'swa_sink': normal((L, SWA_HEADS), 0.5),
        'w_out': normal((L, D_MIX, D), D_MIX ** -0.5),
        'ffn_w1': normal((n_dense, D, FFN_DIM), D ** -0.5),
        'ffn_w3': normal((n_dense, D, FFN_DIM), D ** -0.5),
        'ffn_w2': normal((n_dense, FFN_DIM, D), FFN_DIM ** -0.5),
        'moe_router': normal((n_moe, D, N_EXPERTS), D ** -0.5),
        'moe_w1': normal((n_moe, N_EXPERTS, D, EXPERT_DIM), D ** -0.5),
        'moe_w3': normal((n_moe, N_EXPERTS, D, EXPERT_DIM), D ** -0.5),
        'moe_w2': normal((n_moe, N_EXPERTS, EXPERT_DIM, D), EXPERT_DIM ** -0.5),
        'final_norm_g': gain((D,)),
    }


def reference(x, c, ctx, c_ctx, norm1_g, norm2_g, w_mod, b_mod, w_in, mla_q_norm_g, mla_kv_norm_g,
              mla_w_uq, mla_w_ukv, gqa_q_norm_g, gqa_k_norm_g, gdn_conv_w, gdn_a_log, gdn_dt_bias,
              gdn_norm_g, swa_sink, w_out, ffn_w1, ffn_w3, ffn_w2, moe_router, moe_w1, moe_w3, moe_w2,
              final_norm_g):
    n_lat = x.shape[1]
    rope_mla = _axial_rope_tables(n_lat, MLA_ROPE)
    rope_head = _axial_rope_tables(n_lat, HEAD_DIM)
    silu_c = jax.nn.silu(c)[:, None, :]
    silu_cc = jax.nn.silu(c_ctx)[None, None, :]
    xl, xc = x, ctx
    for layer in range(DEPTH):
        with_ctx = layer < DEPTH - 1
        mods_l = jnp.split(silu_c @ w_mod[layer] + b_mod[layer], 6, axis=-1)
        mods_c = jnp.split(silu_cc @ w_mod[layer] + b_mod[layer], 6, axis=-1)
        hl = _modulate(_rmsnorm(xl, norm1_g[layer]), mods_l[0], mods_l[1])
        hc = _modulate(_rmsnorm(xc, norm1_g[layer]), mods_c[0], mods_c[1])
        pl = _split_in(hl @ w_in[layer])
        pc = _split_in(hc @ w_in[layer])
        a_c, a_l = _mla(pc[0:3], pl[0:3], rope_mla, mla_q_norm_g[layer], mla_kv_norm_g[layer],
                        mla_w_uq[layer], mla_w_ukv[layer], with_ctx)
        b_c, b_l = _gqa(pc[3:6], pl[3:6], rope_head, gqa_q_norm_g[layer], gqa_k_norm_g[layer], with_ctx)
        g_c, g_l = _gdn(pc[6:12], pl[6:12], gdn_conv_w[layer], gdn_a_log[layer], gdn_dt_bias[layer],
                        gdn_norm_g[layer], with_ctx)
        d_c, d_l = _swa(pc[12:15], pl[12:15], rope_head, swa_sink[layer], with_ctx)
        xl = xl + mods_l[2] * (jnp.concatenate([a_l, b_l, g_l, d_l], axis=-1) @ w_out[layer])
        hl2 = _modulate(_rmsnorm(xl, norm2_g[layer]), mods_l[3], mods_l[4])
        xl = xl + mods_l[5] * _channel_mixer(layer, hl2, ffn_w1, ffn_w3, ffn_w2,
                                             moe_router, moe_w1, moe_w3, moe_w2)
        if with_ctx:
            xc = xc + mods_c[2] * (jnp.concatenate([a_c, b_c, g_c, d_c], axis=-1) @ w_out[layer])
            hc2 = _modulate(_rmsnorm(xc, norm2_g[layer]), mods_c[3], mods_c[4])
            xc = xc + mods_c[5] * _channel_mixer(layer, hc2, ffn_w1, ffn_w3, ffn_w2,
                                                 moe_router, moe_w1, moe_w3, moe_w2)
    return _rmsnorm(xl, final_norm_g)
```

```python
import numpy as np
import concourse.bass as bass
import concourse.mybir as mybir
from concourse.bass_utils import run_bass_kernel_spmd
from contextlib import ExitStack

F32 = mybir.dt.float32
BF16 = mybir.dt.bfloat16
ALU = mybir.AluOpType
AF = mybir.ActivationFunctionType
AX = mybir.AxisListType

D = 2048
NT = 2304
NTILE = 18
DIN = 4816
FFN = 5632
EXD = 4096
NEG = -30000.0
EPS = 1e-6
O_CQ, O_CKV, O_KR = 0, 384, 640
O_BQ, O_BK, O_BV = 704, 1216, 1472
O_GQ, O_GK, O_GV, O_GZ, O_GA, O_GB = 1728, 2240, 2752, 3264, 3776, 3784
O_DQ, O_DK, O_DV = 3792, 4304, 4560

C_ID, C_ONE, C_SU, C_SL, C_U64, C_L64 = 0, 128, 256, 384, 512, 576
C_MN = (640, 640 + 768)
C_MM = (640 + 256, 640 + 768 + 256)
C_ST = (640 + 512, 640 + 768 + 512)
C_W = 640 + 1536

_DBG = {}


class Buf:
    __slots__ = ("w", "r")

    def __init__(self):
        self.w = None
        self.r = {}


class Eng:
    def __init__(self, key, obj, sem, is_pe=False):
        self.key, self.obj, self.sem, self.is_pe = key, obj, sem, is_pe
        self.cnt = 0
        self.seen = {}
        self.slots = []
        self.slot_i = 0


class Slot:
    def __init__(self, key, sem):
        self.key, self.sem, self.cnt = key, sem, 0


class Sched:
    def __init__(self, nc, stack, n_dma_slots=12):
        self.nc = nc
        mk = lambda n: stack.enter_context(nc.semaphore(n))
        self.pe = Eng("pe", nc.tensor, mk("s_pe"), is_pe=True)
        self.dve = Eng("dve", nc.vector, mk("s_dve"))
        self.act = Eng("act", nc.scalar, mk("s_act"))
        self.pool = Eng("pool", nc.gpsimd, mk("s_pool"))
        self.sp = Eng("sp", nc.sync, mk("s_sp"))
        self.engs = [self.pe, self.dve, self.act, self.pool, self.sp]
        for q in (self.sp, self.pool, self.act):
            q.slots = [Slot(f"d_{q.key}{i}", mk(f"d_{q.key}{i}")) for i in range(n_dma_slots)]

    def _wait(self, eng, dep):
        key, sem, val = dep
        if eng.seen.get(key, 0) >= val:
            return
        eng.obj.wait_ge(sem, val)
        eng.seen[key] = val

    def _deps(self, eng, reads, writes):
        for b in reads:
            if b.w is not None and not (eng.is_pe and b.w[0] == eng.key):
                self._wait(eng, b.w)
        for b in writes:
            if b.w is not None and not (eng.is_pe and b.w[0] == eng.key):
                self._wait(eng, b.w)
            for d in b.r.values():
                if d[0] != eng.key:
                    self._wait(eng, d)

    def _mark(self, tok, reads, writes):
        for b in reads:
            b.r[tok[0]] = tok
        for b in writes:
            b.w = tok
            b.r = {}

    def op(self, eng, fn, reads=(), writes=()):
        self._deps(eng, reads, writes)
        ins = fn()
        eng.cnt += 1
        ins.then_inc(eng.sem, 1)
        self._mark((eng.key, eng.sem, eng.cnt), reads, writes)

    def dma(self, q, out, in_, reads=(), writes=(), **kw):
        slot = q.slots[q.slot_i % len(q.slots)]
        q.slot_i += 1
        if slot.cnt > 0:
            self._wait(q, (slot.key, slot.sem, slot.cnt * 16))
        self._deps(q, reads, writes)
        ins = q.obj.dma_start(out=out, in_=in_, **kw)
        slot.cnt += 1
        ins.then_inc(slot.sem, 16)
        self._mark((slot.key, slot.sem, slot.cnt * 16), reads, writes)

    def barrier(self):
        toks = [(e.key, e.sem, e.cnt) for e in self.engs if e.cnt > 0]
        for q in (self.sp, self.pool, self.act):
            toks += [(s.key, s.sem, s.cnt * 16) for s in q.slots if s.cnt > 0]
        for e in self.engs:
            for tk in toks:
                if tk[0] != e.key:
                    self._wait(e, tk)

    def finish(self):
        self.barrier()


def build_program(n_seq=2, n_layers=4, stop=None, dbg=(), start=None, only=None):
    nc = bass.Bass("TRN2", target_bir_lowering=False)
    L = n_layers

    def din(name, shape, dt=F32):
        return nc.dram_tensor(name, list(shape), dt, kind="ExternalInput").ap()

    def dscr(name, shape, dt=F32):
        kind = "ExternalOutput" if name in dbg else "Internal"
        if start == "mix" and name == "P_d":
            kind = "ExternalInput"
        return nc.dram_tensor(name, list(shape), dt, kind=kind).ap()

    x_in = din("x", [n_seq, 2048, D]); ctx_in = din("ctx", [n_seq, 256, D])
    c_in = din("c", [n_seq, D]); cctx_in = din("c_ctx", [D])
    norm1_g = din("norm1_g", [L, D]); norm2_g = din("norm2_g", [L, D])
    w_mod = din("w_mod", [L, D, 6 * D]); b_mod = din("b_mod", [L, 6 * D])
    w_in = din("w_in", [L, D, DIN])
    mla_qg = din("mla_q_norm_g", [L, 384]); mla_kvg = din("mla_kv_norm_g", [L, 256])
    mla_wuq = din("mla_w_uq", [L, 384, 768]); mla_wukv = din("mla_w_ukv", [L, 256, 1024])
    gqa_qg = din("gqa_q_norm_g", [L, 128]); gqa_kg = din("gqa_k_norm_g", [L, 128])
    gdn_cw = din("gdn_conv_w", [L, 5, 1536]); gdn_alog = din("gdn_a_log", [L, 8]); gdn_dtb = din("gdn_dt_bias", [L, 8])
    gdn_ng = din("gdn_norm_g", [L, 128]); swa_sink = din("swa_sink", [L, 4])
    w_out = din("w_out", [L, D, D])
    NF = (L + 1) // 2; NM = L // 2
    ffn_w1 = din("ffn_w1", [NF, D, FFN]); ffn_w3 = din("ffn_w3", [NF, D, FFN]); ffn_w2 = din("ffn_w2", [NF, FFN, D])
    if NM > 0:
        moe_router = din("moe_router", [NM, D, 8])
        moe_w1 = din("moe_w1", [NM, 8, D, EXD]); moe_w3 = din("moe_w3", [NM, 8, D, EXD]); moe_w2 = din("moe_w2", [NM, 8, EXD, D])
    final_g = din("final_norm_g", [D])
    rc128 = din("rc128", [2048, 512]); rs128 = din("rs128", [2048, 512])
    rc64 = din("rc64", [2048, 256]); rs64 = din("rs64", [2048, 256])
    cst_in = din("cst", [128, C_W])
    out_d = nc.dram_tensor("out", [n_seq, 2048, D], F32, kind="ExternalOutput").ap()

    mods_d = dscr("mods_d", [L, n_seq + 1, 6 * D])
    resid_d = dscr("resid_d", [n_seq, NT, D])
    P_d = dscr("P_d", [n_seq, NT, DIN])
    Y_d = dscr("Y_d", [n_seq, NT, D], BF16)
    H2T_d = dscr("H2T_d", [n_seq, 128, 16, NT], BF16)
    GATES_d = dscr("GATES_d", [n_seq, NT, 8])
    GTOK_d = dscr("GTOK_d", [n_seq, NT, 1040])
    GFT_d = dscr("GFT_d", [n_seq, 8, 128, NT])
    GO_d = dscr("GO_d", [n_seq, 2, NT, 512])

    with ExitStack() as st:
        S = Sched(nc, st)

        def V(fn, r=(), w=()): S.op(S.dve, fn, r, w)
        def A(fn, r=(), w=()): S.op(S.act, fn, r, w)
        def G(fn, r=(), w=()): S.op(S.pool, fn, r, w)
        def T(fn, r=(), w=()): S.op(S.pe, fn, r, w)

        _cnt = [0]

        def sbt(stack, shape, dt=F32, name=None):
            _cnt[0] += 1
            return stack.enter_context(nc.sbuf_tensor(name or f"sb{_cnt[0]}", list(shape), dt))

        psall = st.enter_context(nc.psum_tensor("psall", [128, 4096], F32))
        psallb = psall.bitcast(BF16)
        pb = [Buf() for _ in range(8)]

        def ps(bank, n=512, parts=128, off=0):
            return psall[0:parts, bank * 512 + off: bank * 512 + off + n]

        def psb(bank, n=1024, parts=128, off=0):
            return psallb[0:parts, bank * 1024 + off: bank * 1024 + off + n]

        b_mods = Buf()
        b_resid = [[Buf() for _ in range(NTILE)] for _ in range(n_seq)]
        b_P = [[Buf() for _ in range(NTILE)] for _ in range(n_seq)]
        b_Y = [[Buf() for _ in range(NTILE)] for _ in range(n_seq)]
        b_H2T = [[Buf() for _ in range(NTILE)] for _ in range(n_seq)]
        b_GATES = [[Buf() for _ in range(NTILE)] for _ in range(n_seq)]
        b_GTOK = [[Buf() for _ in range(NTILE)] for _ in range(n_seq)]
        b_GFT = [[Buf() for _ in range(NTILE)] for _ in range(n_seq)]
        b_GO = [[[Buf() for _ in range(NTILE)] for _ in range(2)] for _ in range(n_seq)]
        b_out = Buf()

        cst = sbt(st, [128, C_W], F32, "cst_sb")
        identb = sbt(st, [128, 128], BF16, "identb")
        swab = sbt(st, [128, 256], BF16, "swab")
        b_cst = Buf()
        S.dma(S.sp, cst[:], cst_in[:, :], writes=[b_cst])
        V(lambda: nc.vector.tensor_copy(out=identb[:], in_=cst[:, C_ID:C_ID + 128]), [b_cst], [b_cst])
        V(lambda: nc.vector.tensor_copy(out=swab[:], in_=cst[:, C_SU:C_SU + 256]), [b_cst], [b_cst])
        ident = cst[:, C_ID:C_ID + 128]
        ones = cst[:, C_ONE:C_ONE + 128]

        def xsrc(l, b, t):
            if l == 0:
                if t < 2:
                    return ctx_in[b, t * 128:(t + 1) * 128, :]
                return x_in[b, (t - 2) * 128:(t - 1) * 128, :]
            return resid_d[b, t * 128:(t + 1) * 128, :]

        def xsrc_buf(l, b, t):
            return [] if l == 0 else [b_resid[b][t]]

        def rstd_col(out_col, ssq_col, scale, eps, b_in):
            A(lambda: nc.scalar.activation(out=out_col, in_=ssq_col, func=AF.Sqrt, scale=scale, bias=eps), [b_in], [b_in])
            V(lambda: nc.vector.reciprocal(out=out_col, in_=out_col), [b_in], [b_in])

        def transpose_group(dst, srcs, bank, b_src, b_dst, dt_bf=True, evac="act", parts_out=128):
            n = len(srcs)
            K = srcs[0].shape[0]
            M = srcs[0].shape[1]

            def tr():
                for i, s_ in enumerate(srcs):
                    if dt_bf:
                        ins = nc.tensor.transpose(out=psb(bank, K, M, i * K), in_=s_, identity=identb[0:K, 0:K])
                    else:
                        ins = nc.tensor.transpose(out=ps(bank, K, M, i * K), in_=s_, identity=ident[0:K, 0:K])
                return ins
            T(tr, [b_src, b_cst], [pb[bank]])
            src = (psb(bank, n * K, M) if dt_bf else ps(bank, n * K, M)).rearrange("p (n k) -> p n k", k=K)
            if evac == "act":
                A(lambda: nc.scalar.copy(out=dst, in_=src), [pb[bank]], [b_dst])
            else:
                V(lambda: nc.vector.tensor_copy(out=dst, in_=src), [pb[bank]], [b_dst])

        def rope(out3, x3, cos3, sin3, tmp1, tmp2, q, bufs_r, bufs_w):
            V(lambda: nc.vector.tensor_tensor(out=tmp1, in0=x3, in1=cos3, op=ALU.mult), bufs_r, bufs_w)

            def sw():
                for a in range(2):
                    for bd in range(2):
                        bs = 1 - bd
                        o0 = (2 * a + bd) * q
                        s0 = (2 * a + bs) * q
                        ins = nc.vector.tensor_tensor(out=tmp2[:, :, o0:o0 + q], in0=x3[:, :, s0:s0 + q], in1=sin3[:, :, o0:o0 + q], op=ALU.mult)
                return ins
            V(sw, bufs_r, bufs_w)
            V(lambda: nc.vector.tensor_tensor(out=out3, in0=tmp1, in1=tmp2, op=ALU.add), bufs_r, bufs_w)

        def prologue():
            R_ = n_seq + 1
            with ExitStack() as ph:
                cT = sbt(ph, [128, 16, R_]); b_cT = Buf()
                modsb = sbt(ph, [R_, 6 * D]); b_modsb = Buf()
                bm = sbt(ph, [R_, 6 * D]); b_bm = Buf()
                wbuf = [sbt(ph, [128, 16, 512]) for _ in range(2)]; b_w = [Buf(), Buf()]
                for r in range(R_):
                    src = c_in[r] if r < n_seq else cctx_in
                    S.dma(S.sp, cT[:, :, r:r + 1], src.rearrange("(j p o) -> p j o", p=128, o=1), writes=[b_cT], allow_slow_non_contiguous=True)
                A(lambda: nc.scalar.activation(out=cT[:], in_=cT[:], func=AF.Silu), [b_cT], [b_cT])
                for l in range(L):
                    S.dma(S.sp, bm[:], b_mod[l].partition_broadcast(R_), writes=[b_bm])
                    for n in range(24):
                        wb_ = wbuf[n % 2]
                        S.dma(S.sp if n % 2 else S.act, wb_[:], w_mod[l, :, n * 512:(n + 1) * 512].rearrange("(j p) n -> p j n", p=128), writes=[b_w[n % 2]])
                        bank = n % 2

                        def mm():
                            for k in range(16):
                                ins = nc.tensor.matmul(ps(bank, 512, R_), lhsT=cT[:, k, :], rhs=wb_[:, k, :], start=(k == 0), stop=(k == 15))
                            return ins
                        T(mm, [b_cT, b_w[n % 2]], [pb[bank]])
                        V(lambda: nc.vector.tensor_tensor(out=modsb[:, n * 512:(n + 1) * 512], in0=ps(bank, 512, R_), in1=bm[:, n * 512:(n + 1) * 512], op=ALU.add),
                          [pb[bank], b_bm], [b_modsb])
                    S.dma(S.sp, mods_d[l], modsb[:], reads=[b_modsb], writes=[b_mods])
                S.barrier()

        def mod_row(l, b, stream, idx):
            row = n_seq if stream == 0 else b
            return mods_d[l, row, idx * D:(idx + 1) * D].partition_broadcast(128)

        def build_mod_consts(ph_tiles, l, b, stream, gain_ap, i_shift, i_scale, b_c):
            Amul, Badd, tmp = ph_tiles
            S.dma(S.sp, tmp[:], gain_ap.partition_broadcast(128), writes=[b_c])
            S.dma(S.sp, Amul[:], mod_row(l, b, stream, i_scale), reads=[b_mods], writes=[b_c])
            S.dma(S.sp, Badd[:], mod_row(l, b, stream, i_shift), reads=[b_mods], writes=[b_c])
            V(lambda: nc.vector.scalar_tensor_tensor(out=Amul[:], in0=Amul[:], scalar=1.0, in1=tmp[:], op0=ALU.add, op1=ALU.mult), [b_c], [b_c])

        def phase_A(b, l, hT, b_hT):
            with ExitStack() as ph:
                Amul = sbt(ph, [128, D]); Badd = sbt(ph, [128, D]); tmp = sbt(ph, [128, D]); b_c = Buf()
                xt = [sbt(ph, [128, D]) for _ in range(2)]; b_xt = [Buf(), Buf()]
                hf = sbt(ph, [128, D]); b_hf = Buf()
                hb = [sbt(ph, [128, D], BF16) for _ in range(2)]; b_hb = [Buf(), Buf()]
                col = sbt(ph, [128, 4]); b_col = Buf()
                for t in range(NTILE):
                    if t == 0 or t == 2:
                        build_mod_consts((Amul, Badd, tmp), l, b, 0 if t == 0 else 1, norm1_g[l], 0, 1, b_c)
                    i = t % 2
                    S.dma(S.sp, xt[i][:], xsrc(l, b, t), reads=xsrc_buf(l, b, t), writes=[b_xt[i]])
                    A(lambda: nc.scalar.activation(out=hf[:], in_=xt[i][:], func=AF.Square, accum_out=col[:, 0:1]), [b_xt[i]], [b_hf, b_col])
                    rstd_col(col[:, 1:2], col[:, 0:1], 1.0 / D, EPS, b_col)
                    V(lambda: nc.vector.scalar_tensor_tensor(out=hf[:], in0=xt[i][:], scalar=col[:, 1:2], in1=Amul[:], op0=ALU.mult, op1=ALU.mult),
                      [b_xt[i], b_col, b_c], [b_hf])
                    G(lambda: nc.gpsimd.tensor_tensor(out=hb[i][:], in0=hf[:], in1=Badd[:], op=ALU.add), [b_hf, b_c], [b_hb[i]])
                    for g_ in range(2):
                        transpose_group(hT[:, g_ * 8:(g_ + 1) * 8, t * 128:(t + 1) * 128],
                                        [hb[i][:, (g_ * 8 + j) * 128:(g_ * 8 + j + 1) * 128] for j in range(8)],
                                        4 + g_, b_hb[i], b_hT, evac="act" if g_ else "dve")
                S.barrier()

        def phase_B(b, l, hT, b_hT):
            with ExitStack() as ph:
                wb_ = [sbt(ph, [128, 16, 512], BF16) for _ in range(2)]; b_wb = [Buf(), Buf()]
                stg = [sbt(ph, [128, 512]) for _ in range(4)]; b_stg = [Buf() for _ in range(4)]
                chunks = [(c0, min(512, DIN - c0)) for c0 in range(0, DIN, 512)]
                it = 0
                for ci, (c0, cw) in enumerate(chunks):
                    w_ = wb_[ci % 2]
                    S.dma(S.pool, w_[:, :, 0:cw], w_in[l, :, c0:c0 + cw].rearrange("(j p) n -> p j n", p=128), writes=[b_wb[ci % 2]])
                    for t in range(NTILE):
                        bank = it % 4
                        it += 1

                        def mm():
                            for k in range(16):
                                ins = nc.tensor.matmul(ps(bank, cw), lhsT=hT[:, k, t * 128:(t + 1) * 128], rhs=w_[:, k, 0:cw], start=(k == 0), stop=(k == 15))
                            return ins
                        T(mm, [b_hT, b_wb[ci % 2]], [pb[bank]])
                        if it % 2:
                            A(lambda: nc.scalar.copy(out=stg[bank][:, 0:cw], in_=ps(bank, cw)), [pb[bank]], [b_stg[bank]])
                        else:
                            V(lambda: nc.vector.tensor_copy(out=stg[bank][:, 0:cw], in_=ps(bank, cw)), [pb[bank]], [b_stg[bank]])
                        S.dma(S.sp, P_d[b, t * 128:(t + 1) * 128, c0:c0 + cw], stg[bank][:, 0:cw], reads=[b_stg[bank]], writes=[b_P[b][t]])
                S.barrier()

        def attn_core(A_, qparts, chunks, scale, out_ap, b_q, b_k, b_out, sink=None):
            n = len(chunks)
            nb = (n * 128 + 511) // 512
            sbufs = pb[0:nb]

            def qk():
                for ci, (kparts, v_ap, mask) in enumerate(chunks):
                    o = psall[:, ci * 128:(ci + 1) * 128]
                    np_ = len(kparts)
                    for pi in range(np_):
                        ins = nc.tensor.matmul(o, lhsT=qparts[pi], rhs=kparts[pi], start=(pi == 0), stop=(pi == np_ - 1 and mask is None))
                    if mask is not None:
                        ins = nc.tensor.matmul(o, lhsT=identb[:], rhs=mask, start=False, stop=True)
                return ins
            T(qk, [b_q, b_k, b_cst], sbufs)
            col = A_["col"]; b_col = A_["b_col"]
            V(lambda: nc.vector.reduce_max(out=col[:, 0:1], in_=psall[:, 0:n * 128], axis=AX.X), sbufs, [b_col])
            if sink is None:
                V(lambda: nc.vector.tensor_scalar(out=col[:, 1:2], in0=col[:, 0:1], scalar1=-scale, scalar2=None, op0=ALU.mult), [b_col], [b_col])
            else:
                V(lambda: nc.vector.tensor_scalar(out=col[:, 1:2], in0=col[:, 0:1], scalar1=-scale, scalar2=sink[1], op0=ALU.mult, op1=ALU.min), [b_col, b_cst], [b_col])
            pexp = A_["pexp"]; b_pexp = A_["b_pexp"]
            A(lambda: nc.scalar.activation(out=pexp[:, 0:n * 128], in_=psall[:, 0:n * 128], func=AF.Exp, bias=col[:, 1:2], scale=scale, accum_out=col[:, 2:3]),
              sbufs + [b_col], [b_pexp, b_col])
            if sink is not None:
                A(lambda: nc.scalar.activation(out=col[:, 3:4], in_=sink[0], func=AF.Exp, bias=col[:, 1:2], scale=1.0), [b_col, b_cst], [b_col])
                V(lambda: nc.vector.tensor_tensor(out=col[:, 2:3], in0=col[:, 2:3], in1=col[:, 3:4], op=ALU.add), [b_col], [b_col])
            V(lambda: nc.vector.reciprocal(out=col[:, 4:5], in_=col[:, 2:3]), [b_col], [b_col])
            PT = A_["PT"]; b_PT = A_["b_PT"]
            gi = 0
            for c0 in range(0, n, 8):
                c1 = min(n, c0 + 8)
                transpose_group(PT[:, c0:c1, :], [pexp[:, ci * 128:(ci + 1) * 128] for ci in range(c0, c1)], 5 + gi % 2, b_pexp, b_PT,
                                evac="act" if gi % 2 else "dve")
                gi += 1
            dv = chunks[0][1].shape[-1]

            def pv():
                for ci, (kparts, v_ap, mask) in enumerate(chunks):
                    ins = nc.tensor.matmul(ps(7, dv), lhsT=PT[:, ci, :], rhs=v_ap, start=(ci == 0), stop=(ci == n - 1))
                return ins
            T(pv, [b_PT, b_k], [pb[7]])
            V(lambda: nc.vector.tensor_scalar(out=out_ap, in0=ps(7, dv), scalar1=col[:, 4:5], scalar2=None, op0=ALU.mult), [pb[7], b_col], [b_out])

        def attn_scratch(ph):
            return dict(col=sbt(ph, [128, 8]), b_col=Buf(), pexp=sbt(ph, [128, NT], BF16), b_pexp=Buf(),
                        PT=sbt(ph, [128, NTILE, 128], BF16), b_PT=Buf())

        def qtiles(l):
            return range(NTILE) if l < L - 1 else range(2, NTILE)

        def load_rope(ph_t, src, t, w, b_r):
            S.dma(S.sp, ph_t[:, 0:w], src[(t - 2) * 128:(t - 1) * 128, 0:w], writes=[b_r])

        def mixer_mla(b, l):
            with ExitStack() as ph:
                wuq = sbt(ph, [128, 3, 768], BF16); wukv = sbt(ph, [128, 2, 1024], BF16); b_w = Buf()
                gq = sbt(ph, [128, 384]); gkv = sbt(ph, [128, 256])
                S.dma(S.pool, wuq[:], mla_wuq[l].rearrange("(j p) n -> p j n", p=128), writes=[b_w])
                S.dma(S.pool, wukv[:], mla_wukv[l].rearrange("(j p) n -> p j n", p=128), writes=[b_w])
                S.dma(S.sp, gq[:], mla_qg[l].partition_broadcast(128), writes=[b_w])
                S.dma(S.sp, gkv[:], mla_kvg[l].partition_broadcast(128), writes=[b_w])
                KnT = sbt(ph, [128, 4, NT], BF16); KrT = sbt(ph, [64, NT], BF16); Vv = sbt(ph, [128, NTILE, 4, 128], BF16); b_kv = Buf()
                pa = [sbt(ph, [128, 384]) for _ in range(2)]; b_pa = [Buf(), Buf()]
                junk = sbt(ph, [128, 768]); b_t = Buf()
                col = sbt(ph, [128, 4])
                xn = sbt(ph, [128, 384], BF16); xnT = sbt(ph, [128, 3, 128], BF16)
                knb = sbt(ph, [128, 4, 128], BF16); krb = sbt(ph, [128, 64], BF16)
                rc = sbt(ph, [128, 256]); rs = sbt(ph, [128, 256]); b_r = Buf()
                t1 = sbt(ph, [128, 256]); t2 = sbt(ph, [128, 256])
                qf = sbt(ph, [128, 768]); qb = sbt(ph, [128, 4, 192], BF16)
                QnT = sbt(ph, [128, 4, 128], BF16); QrT = sbt(ph, [64, 4, 128], BF16); b_q = Buf()
                yt = [sbt(ph, [128, 512], BF16) for _ in range(2)]; b_yt = [Buf(), Buf()]
                A_ = attn_scratch(ph)
                for t in range(NTILE):
                    i = t % 2
                    S.dma(S.sp, pa[i][:, 0:320], P_d[b, t * 128:(t + 1) * 128, O_CKV:O_CKV + 320], reads=[b_P[b][t]], writes=[b_pa[i]])
                    if t >= 2:
                        load_rope(rc, rc64, t, 64, b_r); load_rope(rs, rs64, t, 64, b_r)
                    s1 = _DBG.get("s1", 9)
                    if s1 < 2:
                        continue
                    A(lambda: nc.scalar.activation(out=junk[:, 0:256], in_=pa[i][:, 0:256], func=AF.Square, accum_out=col[:, 0:1]), [b_pa[i]], [b_t])
                    rstd_col(col[:, 1:2], col[:, 0:1], 1.0 / 256, EPS, b_t)
                    V(lambda: nc.vector.scalar_tensor_tensor(out=xn[:, 0:256], in0=pa[i][:, 0:256], scalar=col[:, 1:2], in1=gkv[:], op0=ALU.mult, op1=ALU.mult),
                      [b_pa[i], b_t, b_w], [b_t])
                    if s1 < 3:
                        continue
                    transpose_group(xnT[:, 0:2, :], [xn[:, j * 128:(j + 1) * 128] for j in range(2)], 5, b_t, b_t, evac="dve")

                    def mmkv():
                        for hb_ in range(2):
                            for k in range(2):
                                ins = nc.tensor.matmul(ps(5 + hb_, 512), lhsT=xnT[:, k, :], rhs=wukv[:, k, hb_ * 512:(hb_ + 1) * 512], start=(k == 0), stop=(k == 1))
                        return ins
                    if s1 < 3.2:
                        continue
                    T(mmkv, [b_t, b_w], [pb[5], pb[6]])
                    kv3 = psall[:, 5 * 512:7 * 512].rearrange("p (h c) -> p h c", c=256)
                    if s1 < 3.4:
                        continue
                    for hb_ in range(2):
                        kvb = ps(5 + hb_, 512).rearrange("p (h c) -> p h c", c=256)
                        A(lambda: nc.scalar.copy(out=Vv[:, t, 2 * hb_:2 * hb_ + 2, :], in_=kvb[:, :, 128:256]), [pb[5 + hb_]], [b_kv])
                    if s1 < 3.6:
                        continue
                    for hb_ in range(2):
                        kvb = ps(5 + hb_, 512).rearrange("p (h c) -> p h c", c=256)
                        A(lambda: nc.scalar.copy(out=knb[:, 2 * hb_:2 * hb_ + 2, :], in_=kvb[:, :, 0:128]), [pb[5 + hb_]], [b_t])
                    if s1 < 4:
                        continue
                    transpose_group(KnT[:, :, t * 128:(t + 1) * 128], [knb[:, h, :] for h in range(4)], 7, b_t, b_kv, evac="act")
                    if s1 < 5:
                        continue
                    if t >= 2:
                        x3 = pa[i][:, 256:320].unsqueeze(1)
                        rope(krb[:].unsqueeze(1), x3, rc[:, 0:64].unsqueeze(1), rs[:, 0:64].unsqueeze(1), t1[:, 0:64].unsqueeze(1), t2[:, 0:64].unsqueeze(1), 16,
                             [b_pa[i], b_r, b_t], [b_t])
                    else:
                        V(lambda: nc.vector.tensor_copy(out=krb[:], in_=pa[i][:, 256:320]), [b_pa[i]], [b_t])
                    if s1 < 6:
                        continue
                    transpose_group(KrT[:, t * 128:(t + 1) * 128].unsqueeze(1), [krb[:, :]], 7, b_t, b_kv, evac="dve")
                for t in (qtiles(l) if _DBG.get("mla_cut", 3) >= 2 else []):
                    i = t % 2
                    S.dma(S.sp, pa[i][:, 0:384], P_d[b, t * 128:(t + 1) * 128, O_CQ:O_CQ + 384], reads=[b_P[b][t]], writes=[b_pa[i]])
                    if t >= 2:
                        load_rope(rc, rc64, t, 256, b_r); load_rope(rs, rs64, t, 256, b_r)
                    A(lambda: nc.scalar.activation(out=junk[:, 0:384], in_=pa[i][:, 0:384], func=AF.Square, accum_out=col[:, 0:1]), [b_pa[i]], [b_t])
                    rstd_col(col[:, 1:2], col[:, 0:1], 1.0 / 384, EPS, b_t)
                    V(lambda: nc.vector.scalar_tensor_tensor(out=xn[:, 0:384], in0=pa[i][:, 0:384], scalar=col[:, 1:2], in1=gq[:], op0=ALU.mult, op1=ALU.mult),
                      [b_pa[i], b_t, b_w], [b_t])
                    transpose_group(xnT[:, 0:3, :], [xn[:, j * 128:(j + 1) * 128] for j in range(3)], 5, b_t, b_t, evac="dve")

                    def mmq():
                        for (bank, c0, cw) in ((5, 0, 512), (6, 512, 256)):
                            for k in range(3):
                                ins = nc.tensor.matmul(ps(bank, cw), lhsT=xnT[:, k, :], rhs=wuq[:, k, c0:c0 + cw], start=(k == 0), stop=(k == 2))
                        return ins
                    T(mmq, [b_t, b_w], [pb[5], pb[6]])
                    A(lambda: nc.scalar.copy(out=qf[:], in_=psall[:, 5 * 512:5 * 512 + 768]), [pb[5], pb[6]], [b_t])
                    qf3 = qf[:].rearrange("p (h c) -> p h c", c=192)
                    V(lambda: nc.vector.tensor_copy(out=qb[:, :, 0:128], in_=qf3[:, :, 0:128]), [b_t], [b_q])
                    if t >= 2:
                        rope(qb[:, :, 128:192], qf3[:, :, 128:192], rc[:].rearrange("p (h c) -> p h c", c=64), rs[:].rearrange("p (h c) -> p h c", c=64),
                             t1[:].rearrange("p (h c) -> p h c", c=64), t2[:].rearrange("p (h c) -> p h c", c=64), 16, [b_t, b_r], [b_q, b_t])
                    else:
                        V(lambda: nc.vector.tensor_copy(out=qb[:, :, 128:192], in_=qf3[:, :, 128:192]), [b_t], [b_q])
                    transpose_group(QnT[:], [qb[:, h, 0:128] for h in range(4)], 5, b_q, b_q, evac="act")
                    transpose_group(QrT[:], [qb[:, h, 128:192] for h in range(4)], 6, b_q, b_q, evac="dve")
                    nk = 2 if t < 2 else NTILE
                    for h in (range(4) if _DBG.get("mla_cut", 3) >= 3 else []):
                        chunks = [([KnT[:, h, kc * 128:(kc + 1) * 128], KrT[:, kc * 128:(kc + 1) * 128]], Vv[:, kc, h, :], None) for kc in range(nk)]
                        attn_core(A_, [QnT[:, h, :], QrT[:, h, :]], chunks, 192 ** -0.5, yt[i][:, h * 128:(h + 1) * 128], b_q, b_kv, b_yt[i])
                    S.dma(S.sp, Y_d[b, t * 128:(t + 1) * 128, 0:512], yt[i][:], reads=[b_yt[i]], writes=[b_Y[b][t]])
                S.barrier()

        def mixer_gqa_swa(b, l, swa):
            oq, ok, ov = (O_DQ, O_DK, O_DV) if swa else (O_BQ, O_BK, O_BV)
            ycol = 1536 if swa else 512
            with ExitStack() as ph:
                b_w = Buf()
                if not swa:
                    gq = sbt(ph, [128, 512]); gk = sbt(ph, [128, 256])
                    for h in range(4):
                        S.dma(S.sp, gq[:, h * 128:(h + 1) * 128], gqa_qg[l].partition_broadcast(128), writes=[b_w])
                    for h in range(2):
                        S.dma(S.sp, gk[:, h * 128:(h + 1) * 128], gqa_kg[l].partition_broadcast(128), writes=[b_w])
                else:
                    snk = sbt(ph, [128, 8])
                    S.dma(S.sp, snk[:, 0:4], swa_sink[l].partition_broadcast(128), writes=[b_w])
                    V(lambda: nc.vector.tensor_scalar(out=snk[:, 4:8], in0=snk[:, 0:4], scalar1=-1.0, scalar2=None, op0=ALU.mult), [b_w], [b_w])
                KT = sbt(ph, [128, 2, NT], BF16); Vv = sbt(ph, [128, NTILE, 2, 128], BF16); b_kv = Buf()
                pa = [sbt(ph, [128, 512]) for _ in range(2)]; b_pa = [Buf(), Buf()]
                sq = sbt(ph, [128, 512]); b_t = Buf()
                col = sbt(ph, [128, 16])
                xf = sbt(ph, [128, 512]); xb = sbt(ph, [128, 512], BF16)
                rc = sbt(ph, [128, 512]); rs = sbt(ph, [128, 512]); b_r = Buf()
                t1 = sbt(ph, [128, 512]); t2 = sbt(ph, [128, 512])
                QT = sbt(ph, [128, 4, 128], BF16); b_q = Buf()
                yt = [sbt(ph, [128, 512], BF16) for _ in range(2)]; b_yt = [Buf(), Buf()]
                A_ = attn_scratch(ph)
                if swa:
                    b_w = b_w

                def prep(src_ap, H, gain, t, b_src):
                    W_ = H * 128
                    cur = src_ap
                    if not swa:
                        V(lambda: nc.vector.tensor_tensor(out=sq[:, 0:W_], in0=src_ap, in1=src_ap, op=ALU.mult), [b_src], [b_t])
                        V(lambda: nc.vector.reduce_sum(out=col[:, 0:H], in_=sq[:, 0:W_].rearrange("p (h c) -> p h c", c=128), axis=AX.X), [b_t], [b_t])
                        rstd_col(col[:, 8:8 + H], col[:, 0:H], 1.0 / 128, EPS, b_t)
                        V(lambda: nc.vector.tensor_tensor(out=xf[:, 0:W_].rearrange("p (h c) -> p h c", c=128), in0=src_ap.rearrange("p (h c) -> p h c", c=128),
                                                          in1=col[:, 8:8 + H].unsqueeze(2).to_broadcast([128, H, 128]), op=ALU.mult), [b_src, b_t], [b_t])
                        V(lambda: nc.vector.tensor_tensor(out=xf[:, 0:W_], in0=xf[:, 0:W_], in1=gain[:, 0:W_], op=ALU.mult), [b_t, b_w], [b_t])
                        cur = xf[:, 0:W_]
                    r3 = lambda a: a[:, 0:W_].rearrange("p (h c) -> p h c", c=128)
                    if t >= 2:
                        rope(r3(xb), cur.rearrange("p (h c) -> p h c", c=128), r3(rc), r3(rs), r3(t1), r3(t2), 32, [b_src, b_t, b_r], [b_t])
                    else:
                        V(lambda: nc.vector.tensor_copy(out=xb[:, 0:W_], in_=cur), [b_src, b_t], [b_t])

                for t in range(NTILE):
                    i = t % 2
                    S.dma(S.sp, pa[i][:], P_d[b, t * 128:(t + 1) * 128, ok:ok + 512], reads=[b_P[b][t]], writes=[b_pa[i]])
                    if t >= 2:
                        load_rope(rc, rc128, t, 256, b_r); load_rope(rs, rs128, t, 256, b_r)
                    prep(pa[i][:, 0:256], 2, None if swa else gk, t, b_pa[i])
                    transpose_group(KT[:, :, t * 128:(t + 1) * 128], [xb[:, h * 128:(h + 1) * 128] for h in range(2)], 7, b_t, b_kv, evac="act")
                    V(lambda: nc.vector.tensor_copy(out=Vv[:, t, :, :], in_=pa[i][:, 256:512].rearrange("p (h c) -> p h c", c=128)), [b_pa[i]], [b_kv])
                for t in qtiles(l):
                    i = t % 2
                    S.dma(S.sp, pa[i][:], P_d[b, t * 128:(t + 1) * 128, oq:oq + 512], reads=[b_P[b][t]], writes=[b_pa[i]])
                    if t >= 2:
                        load_rope(rc, rc128, t, 512, b_r); load_rope(rs, rs128, t, 512, b_r)
                    prep(pa[i][:, 0:512], 4, None if swa else gq, t, b_pa[i])
                    transpose_group(QT[:], [xb[:, h * 128:(h + 1) * 128] for h in range(4)], 5, b_t, b_q, evac="act")
                    for h in range(4):
                        kvh = h // 2
                        if swa and t >= 2:
                            n_ = t - 2
                            kl = [(0, None), (1, None)]
                            if n_ >= 1:
                                kl.append((t - 1, swab[:, 0:128]))
                            kl.append((t, None))
                            if n_ <= 14:
                                kl.append((t + 1, swab[:, 128:256]))
                        else:
                            kl = [(kc, None) for kc in range(2 if t < 2 else NTILE)]
                        chunks = [([KT[:, kvh, kc * 128:(kc + 1) * 128]], Vv[:, kc, kvh, :], m) for kc, m in kl]
                        attn_core(A_, [QT[:, h, :]], chunks, 128 ** -0.5, yt[i][:, h * 128:(h + 1) * 128], b_q, b_kv, b_yt[i],
                                  sink=(snk[:, h:h + 1], snk[:, 4 + h:5 + h]) if swa else None)
                    S.dma(S.sp, Y_d[b, t * 128:(t + 1) * 128, ycol:ycol + 512], yt[i][:], reads=[b_yt[i]], writes=[b_Y[b][t]])
                S.barrier()

        def gdn_prep(b, l):
            with ExitStack() as ph:
                cwb2 = sbt(ph, [128, 5 * 1536]); b_w = Buf()
                S.dma(S.sp, cwb2[:], gdn_cw[l].rearrange("j c -> (j c)").partition_broadcast(128), writes=[b_w])
                cwb = cwb2[:].rearrange("p (j c) -> p j c", c=1536)
                gc = sbt(ph, [128, 16])
                S.dma(S.sp, gc[:, 0:8], gdn_alog[l].partition_broadcast(128), writes=[b_w])
                S.dma(S.sp, gc[:, 8:16], gdn_dtb[l].partition_broadcast(128), writes=[b_w])
                A(lambda: nc.scalar.activation(out=gc[:, 0:8], in_=gc[:, 0:8], func=AF.Exp), [b_w], [b_w])
                V(lambda: nc.vector.tensor_scalar(out=gc[:, 0:8], in0=gc[:, 0:8], scalar1=-1.0, scalar2=None, op0=ALU.mult), [b_w], [b_w])
                xs = [[sbt(ph, [128, 1536]) for _ in range(5)] for _ in range(2)]; b_xs = [[Buf() for _ in range(5)] for _ in range(2)]
                ab = [sbt(ph, [128, 16]) for _ in range(2)]; b_ab = [Buf(), Buf()]
                acc = sbt(ph, [128, 1536]); tm = sbt(ph, [128, 1536]); b_acc = Buf(); b_tm = Buf()
                col = sbt(ph, [128, 16]); b_col = Buf()
                tok = [sbt(ph, [128, 1040]) for _ in range(2)]; b_tok = [Buf(), Buf()]
                qkn = sbt(ph, [128, 1024]); b_qkn = Buf()
                ft = [sbt(ph, [128, 8, 128]) for _ in range(2)]; b_ft = [Buf(), Buf()]
                for t in range(NTILE):
                    i = t % 2
                    lo, hi = (0, 256) if t < 2 else (256, NT)
                    for j in range(5):
                        o = j - 2
                        r0 = t * 128 + o
                        r1 = r0 + 128
                        v0, v1 = max(r0, lo), min(r1, hi)
                        if v0 != r0 or v1 != r1:
                            G(lambda: nc.gpsimd.memset(xs[i][j][:], 0.0), [], [b_xs[i][j]])
                        t0_, t1_ = max(0, (v0 - 2) // 128), min(NTILE - 1, (v1 + 1) // 128)
                        S.dma(S.sp if j % 2 else S.act, xs[i][j][v0 - r0:v1 - r0, :], P_d[b, v0:v1, O_GQ:O_GQ + 1536],
                              reads=[b_P[b][tt] for tt in range(max(0, t - 1), min(NTILE, t + 2))], writes=[b_xs[i][j]])
                    S.dma(S.sp, ab[i][:], P_d[b, t * 128:(t + 1) * 128, O_GA:O_GA + 16], reads=[b_P[b][t]], writes=[b_ab[i]])
                    V(lambda: nc.vector.tensor_tensor(out=acc[:], in0=xs[i][2][:], in1=cwb[:, 2, :], op=ALU.mult), [b_xs[i][2], b_w], [b_acc])
                    for j in (0, 1, 3, 4):
                        G(lambda: nc.gpsimd.tensor_tensor(out=tm[:], in0=xs[i][j][:], in1=cwb[:, j, :], op=ALU.mult), [b_xs[i][j], b_w], [b_tm])
                        V(lambda: nc.vector.tensor_tensor(out=acc[:], in0=acc[:], in1=tm[:], op=ALU.add), [b_tm, b_acc], [b_acc])
                    A(lambda: nc.scalar.activation(out=acc[:], in_=acc[:], func=AF.Silu), [b_acc], [b_acc])
                    V(lambda: nc.vector.tensor_tensor(out=tm[:, 0:1024], in0=acc[:, 0:1024], in1=acc[:, 0:1024], op=ALU.mult), [b_acc], [b_tm])
                    V(lambda: nc.vector.reduce_sum(out=col[:, 0:8], in_=tm[:, 0:1024].rearrange("p (h c) -> p h c", c=128), axis=AX.X), [b_tm], [b_col])
                    rstd_col(col[:, 8:16], col[:, 0:8], 1.0, 1e-6, b_col)
                    V(lambda: nc.vector.tensor_scalar(out=col[:, 8:12], in0=col[:, 8:12], scalar1=128 ** -0.5, scalar2=None, op0=ALU.mult), [b_col], [b_col])
                    V(lambda: nc.vector.tensor_tensor(out=qkn[:].rearrange("p (h c) -> p h c", c=128), in0=acc[:, 0:1024].rearrange("p (h c) -> p h c", c=128),
                                                      in1=col[:, 8:16].unsqueeze(2).to_broadcast([128, 8, 128]), op=ALU.mult), [b_acc, b_col], [b_qkn])
                    A(lambda: nc.scalar.copy(out=tok[i][:, 0:512], in_=qkn[:, 512:1024]), [b_qkn], [b_tok[i]])
                    A(lambda: nc.scalar.copy(out=tok[i][:, 512:1024], in_=acc[:, 1024:1536]), [b_acc], [b_tok[i]])
                    V(lambda: nc.vector.tensor_tensor(out=col[:, 0:8], in0=ab[i][:, 0:8], in1=gc[:, 8:16], op=ALU.add), [b_ab[i], b_w, b_col], [b_col])
                    A(lambda: nc.scalar.activation(out=col[:, 0:8], in_=col[:, 0:8], func=AF.Exp), [b_col], [b_col])
                    A(lambda: nc.scalar.activation(out=col[:, 0:8], in_=col[:, 0:8], func=AF.Ln, bias=1.0), [b_col], [b_col])
                    V(lambda: nc.vector.tensor_tensor(out=tok[i][:, 1024:1032], in0=col[:, 0:8], in1=gc[:, 0:8], op=ALU.mult), [b_col, b_w], [b_tok[i]])
                    A(lambda: nc.scalar.activation(out=tok[i][:, 1032:1040], in_=ab[i][:, 8:16], func=AF.Sigmoid), [b_ab[i]], [b_tok[i]])
                    S.dma(S.sp, GTOK_d[b, t * 128:(t + 1) * 128, :], tok[i][:], reads=[b_tok[i]], writes=[b_GTOK[b][t]])
                    for g_ in range(2):
                        transpose_group(ft[i][:, g_ * 4:(g_ + 1) * 4, :], [qkn[:, (g_ * 4 + j) * 128:(g_ * 4 + j + 1) * 128] for j in range(4)],
                                        4 + g_, b_qkn, b_ft[i], dt_bf=False, evac="act" if g_ else "dve")
                    S.dma(S.act, GFT_d[b, :, :, t * 128:(t + 1) * 128].rearrange("g d t -> d g t"), ft[i][:], reads=[b_ft[i]], writes=[b_GFT[b][t]])
                S.barrier()

        def gdn_scan(b, l):
            with ExitStack() as ph:
                orders = [list(range(36)), [3, 2, 1, 0] + list(range(35, 3, -1))]
                st_ = []
                for d in range(2):
                    Z = dict(d=d)
                    Z["S"] = sbt(ph, [128, 4, 128]); Z["b_S"] = Buf()
                    Z["tok"] = [sbt(ph, [64, 1040]) for _ in range(2)]; Z["b_tok"] = [Buf(), Buf()]
                    Z["ft"] = [sbt(ph, [128, 8, 64]) for _ in range(2)]; Z["b_ft"] = [Buf(), Buf()]
                    for nm in ("gcol", "egl", "dl", "bg", "eg"):
                        Z[nm] = sbt(ph, [128, 4])
                    for nm in ("diagG", "diagB", "KK", "KQ", "DN", "DM", "Nm", "Mm", "X", "N2", "M2", "intraT", "tA"):
                        Z[nm] = sbt(ph, [64, 4, 64])
                    for nm in ("vb", "kbg", "kdec", "u", "vnew", "o"):
                        Z[nm] = sbt(ph, [64, 4, 128])
                    Z["wT"] = sbt(ph, [128, 4, 64]); Z["qgT"] = sbt(ph, [128, 4, 64]); Z["eG"] = sbt(ph, [128, 4, 64])
                    Z["b"] = Buf()
                    Z["banks"] = (0, 1, 2, 3) if d == 0 else (4, 5, 6, 7)
                    st_.append(Z)
                    V(lambda: nc.vector.memset(Z["S"][:], 0.0), [], [Z["b_S"]])
                I64 = cst[0:64, C_ID:C_ID + 64]
                for step in range(36):
                    for d in range(2):
                        Z = st_[d]
                        c = orders[d][step]
                        i = step % 2
                        bz = Z["b"]
                        B0, B1, B2, B3 = Z["banks"]
                        tk = Z["tok"][i]; ft = Z["ft"][i]
                        S.dma(S.sp, tk[:], GTOK_d[b, c * 64:(c + 1) * 64, :], reads=[b_GTOK[b][c // 2]], writes=[Z["b_tok"][i]])
                        S.dma(S.act, ft[:], GFT_d[b, :, :, c * 64:(c + 1) * 64].rearrange("g d t -> d g t"), reads=[b_GFT[b][c // 2]], writes=[Z["b_ft"][i]])
                        rd = [Z["b_tok"][i], Z["b_ft"][i], b_cst, bz]
                        k_tok = tk[:, 0:512].rearrange("p (h c) -> p h c", c=128)
                        v_tok = tk[:, 512:1024].rearrange("p (h c) -> p h c", c=128)
                        gs = tk[:, 1024 + 4 * d:1028 + 4 * d]
                        beta = tk[:, 1032 + 4 * d:1036 + 4 * d]
                        qT = ft[:, 0:4, :]; kT = ft[:, 4:8, :]
                        tri = cst[0:64, (C_U64 if d == 0 else C_L64):(C_U64 if d == 0 else C_L64) + 64]
                        mN = cst[0:64, C_MN[d]:C_MN[d] + 256].rearrange("p (h c) -> p h c", c=64)
                        mM = cst[0:64, C_MM[d]:C_MM[d] + 256].rearrange("p (h c) -> p h c", c=64)
                        mS = cst[0:64, C_ST[d]:C_ST[d] + 256].rearrange("p (h c) -> p h c", c=64)
                        bc64 = lambda a: a.unsqueeze(2).to_broadcast([64, 4, 64])
                        bc128 = lambda a: a.unsqueeze(2).to_broadcast([64, 4, 128])
                        p3 = lambda bank, parts=64, w=64: ps(bank, 4 * w, parts).rearrange("p (h c) -> p h c", c=w)
                        def mm1():
                            nc.tensor.matmul(ps(B0, 4, 64), lhsT=tri, rhs=gs, start=True, stop=True)
                            return nc.tensor.matmul(ps(B0, 4, 128, 8), lhsT=ones[0:64, :], rhs=gs, start=True, stop=True)
                        T(mm1, rd, [pb[B0]])
                        V(lambda: nc.vector.tensor_copy(out=Z["gcol"][0:64, :], in_=ps(B0, 4, 64)), [pb[B0]], [bz])
                        A(lambda: nc.scalar.activation(out=Z["egl"][:], in_=ps(B0, 4, 128, 8), func=AF.Exp), [pb[B0]], [bz])
                        V(lambda: nc.vector.tensor_tensor(out=Z["dl"][0:64, :], in0=ps(B0, 4, 64, 8), in1=Z["gcol"][0:64, :], op=ALU.subtract), [pb[B0], bz], [bz])
                        A(lambda: nc.scalar.activation(out=Z["dl"][0:64, :], in_=Z["dl"][0:64, :], func=AF.Exp), [bz], [bz])
                        A(lambda: nc.scalar.activation(out=Z["eg"][0:64, :], in_=Z["gcol"][0:64, :], func=AF.Exp), [bz], [bz])
                        V(lambda: nc.vector.tensor_tensor(out=Z["bg"][0:64, :], in0=Z["eg"][0:64, :], in1=beta, op=ALU.mult), rd, [bz])
                        V(lambda: nc.vector.tensor_tensor(out=Z["diagG"][:], in0=I64.unsqueeze(1).to_broadcast([64, 4, 64]), in1=bc64(Z["gcol"][0:64, :]), op=ALU.mult), rd, [bz])
                        V(lambda: nc.vector.tensor_tensor(out=Z["diagB"][:], in0=I64.unsqueeze(1).to_broadcast([64, 4, 64]), in1=bc64(beta), op=ALU.mult), rd, [bz])

                        def mm2():
                            nc.tensor.matmul(ps(B1, 256, 128), lhsT=ones[0:64, :], rhs=Z["diagG"][:].rearrange("p h c -> p (h c)"), start=True, stop=True)
                            return nc.tensor.matmul(ps(B1, 256, 64, 256), lhsT=ones[0:64, 0:64], rhs=Z["diagB"][:].rearrange("p h c -> p (h c)"), start=True, stop=True)
                        T(mm2, rd, [pb[B1]])
                        Grow = ps(B1, 256, 64).rearrange("p (h c) -> p h c", c=64)
                        Brow = ps(B1, 256, 64, 256).rearrange("p (h c) -> p h c", c=64)
                        def mm3():
                            for h in range(4):
                                nc.tensor.matmul(ps(B2, 64, 64, h * 64), lhsT=kT[:, h, :], rhs=kT[:, h, :], start=True, stop=True)
                            for h in range(4):
                                ins = nc.tensor.matmul(ps(B2, 64, 64, 256 + h * 64), lhsT=kT[:, h, :], rhs=qT[:, h, :], start=True, stop=True)
                            return ins
                        T(mm3, rd, [pb[B2]])
                        A(lambda: nc.scalar.copy(out=Z["KK"][:], in_=ps(B2, 256, 64).rearrange("p (h c) -> p h c", c=64)), [pb[B2]], [bz])
                        A(lambda: nc.scalar.copy(out=Z["KQ"][:], in_=ps(B2, 256, 64, 256).rearrange("p (h c) -> p h c", c=64)), [pb[B2]], [bz])
                        V(lambda: nc.vector.tensor_tensor(out=Z["DN"][:], in0=bc64(Z["gcol"][0:64, :]), in1=Grow, op=ALU.subtract), [pb[B1], bz], [bz])
                        V(lambda: nc.vector.tensor_tensor(out=Z["DN"][:], in0=Z["DN"][:], in1=mN, op=ALU.add), rd, [bz])
                        A(lambda: nc.scalar.activation(out=Z["DN"][:], in_=Z["DN"][:], func=AF.Exp), [bz], [bz])
                        V(lambda: nc.vector.scalar_tensor_tensor(out=Z["tA"][:], in0=Z["DN"][:], scalar=-1.0, in1=Z["KK"][:], op0=ALU.mult, op1=ALU.mult), [bz], [bz])
                        V(lambda: nc.vector.tensor_tensor(out=Z["Nm"][:], in0=Z["tA"][:], in1=bc64(beta), op=ALU.mult), rd, [bz])
                        V(lambda: nc.vector.tensor_tensor(out=Z["DM"][:], in0=Grow, in1=bc64(Z["gcol"][0:64, :]), op=ALU.subtract), [pb[B1], bz], [bz])
                        V(lambda: nc.vector.tensor_tensor(out=Z["DM"][:], in0=Z["DM"][:], in1=mM, op=ALU.add), rd, [bz])
                        A(lambda: nc.scalar.activation(out=Z["DM"][:], in_=Z["DM"][:], func=AF.Exp), [bz], [bz])
                        V(lambda: nc.vector.tensor_tensor(out=Z["intraT"][:], in0=Z["DM"][:], in1=Z["KQ"][:], op=ALU.mult), [bz], [bz])
                        V(lambda: nc.vector.tensor_tensor(out=Z["tA"][:], in0=Z["DM"][:], in1=mS, op=ALU.mult), rd, [bz])
                        V(lambda: nc.vector.scalar_tensor_tensor(out=Z["tA"][:], in0=Z["tA"][:], scalar=-1.0, in1=Z["KK"][:], op0=ALU.mult, op1=ALU.mult), [bz], [bz])
                        V(lambda: nc.vector.tensor_tensor(out=Z["Mm"][:], in0=Z["tA"][:], in1=Brow, op=ALU.mult), [pb[B1], bz], [bz])
                        V(lambda: nc.vector.tensor_tensor(out=Z["X"][:], in0=Z["Mm"][:], in1=I64.unsqueeze(1).to_broadcast([64, 4, 64]), op=ALU.add), rd, [bz])
                        A(lambda: nc.scalar.activation(out=Z["eG"][:], in_=ps(B1, 256, 128).rearrange("p (h c) -> p h c", c=64), func=AF.Exp), [pb[B1]], [bz])
                        V(lambda: nc.vector.tensor_tensor(out=Z["qgT"][:], in0=qT, in1=Z["eG"][:], op=ALU.mult), rd, [bz])
                        Ncur, Mcur = Z["Nm"], Z["Mm"]
                        Nn, Mn = Z["N2"], Z["M2"]
                        for s_ in range(5):
                            last = (s_ == 4)

                            def sqr():
                                for h in range(4):
                                    ins = nc.tensor.matmul(ps(B2, 64, 64, h * 64), lhsT=Mcur[:, h, :], rhs=Ncur[:, h, :], start=True, stop=True)
                                if not last:
                                    for h in range(4):
                                        ins = nc.tensor.matmul(ps(B2, 64, 64, 256 + h * 64), lhsT=Ncur[:, h, :], rhs=Mcur[:, h, :], start=True, stop=True)
                                return ins
                            T(sqr, [bz], [pb[B2]])
                            A(lambda: nc.scalar.copy(out=Nn[:], in_=p3(B2)), [pb[B2]], [bz])
                            if not last:
                                V(lambda: nc.vector.tensor_copy(out=Mn[:], in_=ps(B2, 256, 64, 256).rearrange("p (h c) -> p h c", c=64)), [pb[B2]], [bz])

                            def xup():
                                for h in range(4):
                                    ins = nc.tensor.matmul(ps(B3, 64, 64, h * 64), lhsT=Nn[:, h, :], rhs=Z["X"][:, h, :], start=True, stop=True)
                                return ins
                            T(xup, [bz], [pb[B3]])
                            V(lambda: nc.vector.tensor_tensor(out=Z["X"][:], in0=Z["X"][:], in1=p3(B3), op=ALU.add), [pb[B3], bz], [bz])
                            Ncur, Nn = Nn, Ncur
                            Mcur, Mn = Mn, Mcur
                        V(lambda: nc.vector.tensor_tensor(out=Z["vb"][:], in0=v_tok, in1=bc128(beta), op=ALU.mult), rd, [bz])
                        G(lambda: nc.gpsimd.tensor_tensor(out=Z["kbg"][:], in0=k_tok, in1=bc128(Z["bg"][0:64, :]), op=ALU.mult), rd, [bz])
                        G(lambda: nc.gpsimd.tensor_tensor(out=Z["kdec"][:], in0=k_tok, in1=bc128(Z["dl"][0:64, :]), op=ALU.mult), rd, [bz])

                        def mm7():
                            for h in range(4):
                                nc.tensor.matmul(ps(B2, 128, 64, h * 128), lhsT=Z["X"][:, h, :], rhs=Z["vb"][:, h, :], start=True, stop=True)
                            for h in range(4):
                                ins = nc.tensor.matmul(ps(B3, 64, 128, h * 64), lhsT=Z["kbg"][:, h, :], rhs=Z["X"][:, h, :], start=True, stop=True)
                            return ins
                        T(mm7, [bz], [pb[B2], pb[B3]])
                        A(lambda: nc.scalar.copy(out=Z["u"][:], in_=p3(B2, 64, 128)), [pb[B2]], [bz])
                        V(lambda: nc.vector.tensor_copy(out=Z["wT"][:], in_=p3(B3, 128, 64)), [pb[B3]], [bz])
                        Sx = Z["S"]

                        def mm8():
                            for h in range(4):
                                ins = nc.tensor.matmul(ps(B0, 128, 64, h * 128), lhsT=Z["wT"][:, h, :], rhs=Sx[:, h, :], start=True, stop=True)
                            return ins
                        T(mm8, [bz, Z["b_S"]], [pb[B0]])
                        V(lambda: nc.vector.tensor_tensor(out=Z["vnew"][:], in0=Z["u"][:], in1=p3(B0, 64, 128), op=ALU.subtract), [pb[B0], bz], [bz])

                        def mm9():
                            for h in range(4):
                                nc.tensor.matmul(ps(B2, 128, 64, h * 128), lhsT=Z["qgT"][:, h, :], rhs=Sx[:, h, :], start=True, stop=False)
                                nc.tensor.matmul(ps(B2, 128, 64, h * 128), lhsT=Z["intraT"][:, h, :], rhs=Z["vnew"][:, h, :], start=False, stop=True)
                            for h in range(4):
                                ins = nc.tensor.matmul(ps(B3, 128, 128, h * 128), lhsT=Z["kdec"][:, h, :], rhs=Z["vnew"][:, h, :], start=True, stop=True)
                            return ins
                        T(mm9, [bz, Z["b_S"]], [pb[B2], pb[B3]])
                        A(lambda: nc.scalar.copy(out=Z["o"][:], in_=p3(B2, 64, 128)), [pb[B2]], [bz])
                        S.dma(S.sp, GO_d[b, d, c * 64:(c + 1) * 64, :], Z["o"][:].rearrange("p h c -> p (h c)"), reads=[bz], writes=[b_GO[b][d][c // 2]])
                        V(lambda: nc.vector.tensor_tensor(out=Sx[:], in0=Sx[:], in1=Z["egl"][:].unsqueeze(2).to_broadcast([128, 4, 128]), op=ALU.mult), [bz, Z["b_S"]], [Z["b_S"]])
                        V(lambda: nc.vector.tensor_tensor(out=Sx[:], in0=Sx[:], in1=p3(B3, 128, 128), op=ALU.add), [pb[B3], Z["b_S"]], [Z["b_S"]])
                S.barrier()

        def gdn_final(b, l):
            with ExitStack() as ph:
                gn = sbt(ph, [128, 512]); b_w = Buf()
                for h in range(4):
                    S.dma(S.sp, gn[:, h * 128:(h + 1) * 128], gdn_ng[l].partition_broadcast(128), writes=[b_w])
                of = [sbt(ph, [128, 512]) for _ in range(2)]; ob = [sbt(ph, [128, 512]) for _ in range(2)]; zz = [sbt(ph, [128, 512]) for _ in range(2)]
                b_in = [Buf(), Buf()]
                sq = sbt(ph, [128, 512]); col = sbt(ph, [128, 8]); b_t = Buf()
                yt = [sbt(ph, [128, 512], BF16) for _ in range(2)]; b_yt = [Buf(), Buf()]
                for t in qtiles(l):
                    i = t % 2
                    S.dma(S.sp, of[i][:], GO_d[b, 0, t * 128:(t + 1) * 128, :], reads=[b_GO[b][0][t]], writes=[b_in[i]])
                    S.dma(S.act, ob[i][:], GO_d[b, 1, t * 128:(t + 1) * 128, :], reads=[b_GO[b][1][t]], writes=[b_in[i]])
                    S.dma(S.sp, zz[i][:], P_d[b, t * 128:(t + 1) * 128, O_GZ:O_GZ + 512], reads=[b_P[b][t]], writes=[b_in[i]])
                    V(lambda: nc.vector.tensor_tensor(out=of[i][:], in0=of[i][:], in1=ob[i][:], op=ALU.add), [b_in[i]], [b_in[i]])
                    V(lambda: nc.vector.tensor_tensor(out=sq[:], in0=of[i][:], in1=of[i][:], op=ALU.mult), [b_in[i]], [b_t])
                    V(lambda: nc.vector.reduce_sum(out=col[:, 0:4], in_=sq[:].rearrange("p (h c) -> p h c", c=128), axis=AX.X), [b_t], [b_t])
                    rstd_col(col[:, 4:8], col[:, 0:4], 1.0 / 128, EPS, b_t)
                    V(lambda: nc.vector.tensor_tensor(out=sq[:].rearrange("p (h c) -> p h c", c=128), in0=of[i][:].rearrange("p (h c) -> p h c", c=128),
                                                      in1=col[:, 4:8].unsqueeze(2).to_broadcast([128, 4, 128]), op=ALU.mult), [b_in[i], b_t], [b_t])
                    V(lambda: nc.vector.tensor_tensor(out=sq[:], in0=sq[:], in1=gn[:], op=ALU.mult), [b_t, b_w], [b_t])
                    A(lambda: nc.scalar.activation(out=zz[i][:], in_=zz[i][:], func=AF.Silu), [b_in[i]], [b_in[i]])
                    V(lambda: nc.vector.tensor_tensor(out=yt[i][:], in0=sq[:], in1=zz[i][:], op=ALU.mult), [b_t, b_in[i]], [b_yt[i]])
                    S.dma(S.sp, Y_d[b, t * 128:(t + 1) * 128, 1024:1536], yt[i][:], reads=[b_yt[i]], writes=[b_Y[b][t]])
                S.barrier()

        def phase_D(b, l):
            moe = (l % 2 == 1)
            with ExitStack() as ph:
                wo = sbt(ph, [128, 16, D], BF16); b_wo = Buf()
                for c4 in range(4):
                    S.dma(S.pool, wo[:, :, c4 * 512:(c4 + 1) * 512], w_out[l, :, c4 * 512:(c4 + 1) * 512].rearrange("(j p) n -> p j n", p=128), writes=[b_wo])
                Amul = sbt(ph, [128, D]); Badd = sbt(ph, [128, D]); Gmsa = sbt(ph, [128, D]); b_c = Buf()
                tmp = sbt(ph, [128, D]); b_tmp = Buf()
                yb = [sbt(ph, [128, D], BF16) for _ in range(2)]; b_yb = [Buf(), Buf()]
                yT = sbt(ph, [128, 16, 128], BF16); b_yT = Buf()
                xt = [sbt(ph, [128, D]) for _ in range(2)]; b_xt = [Buf(), Buf()]
                xm = sbt(ph, [128, D]); b_xm = Buf()
                hf = sbt(ph, [128, D]); b_hf = Buf()
                hb = sbt(ph, [128, D], BF16); b_hb = Buf()
                hT_ = [sbt(ph, [128, 16, 128], BF16) for _ in range(2)]; b_hT_ = [Buf(), Buf()]
                col = sbt(ph, [128, 16]); b_col = Buf()
                if moe:
                    rt = sbt(ph, [128, 16, 8]); b_rt = Buf()
                    S.dma(S.sp, rt[:], moe_router[l // 2].rearrange("(j p) e -> p j e", p=128), writes=[b_rt])
                    h32T = sbt(ph, [128, 16, 128]); b_h32T = Buf()
                    lg = sbt(ph, [128, 32]); b_lg = Buf()
                    gt = [sbt(ph, [128, 8]) for _ in range(2)]; b_gt = [Buf(), Buf()]
                for t in qtiles(l):
                    i = t % 2
                    if t == 0 or t == 2:
                        build_mod_consts((Amul, Badd, tmp), l, b, 0 if t == 0 else 1, norm2_g[l], 3, 4, b_c)
                        S.dma(S.sp, Gmsa[:], mod_row(l, b, 0 if t == 0 else 1, 2), reads=[b_mods], writes=[b_c])
                    S.dma(S.sp, yb[i][:], Y_d[b, t * 128:(t + 1) * 128, :], reads=[b_Y[b][t]], writes=[b_yb[i]])
                    S.dma(S.act, xt[i][:], xsrc(l, b, t), reads=xsrc_buf(l, b, t), writes=[b_xt[i]])
                    for g_ in range(2):
                        transpose_group(yT[:, g_ * 8:(g_ + 1) * 8, :], [yb[i][:, (g_ * 8 + j) * 128:(g_ * 8 + j + 1) * 128] for j in range(8)],
                                        4 + g_, b_yb[i], b_yT, evac="act" if g_ else "dve")

                    def mm():
                        for c4 in range(4):
                            for k in range(16):
                                ins = nc.tensor.matmul(ps(c4, 512), lhsT=yT[:, k, :], rhs=wo[:, k, c4 * 512:(c4 + 1) * 512], start=(k == 0), stop=(k == 15))
                        return ins
                    T(mm, [b_yT, b_wo], pb[0:4])
                    V(lambda: nc.vector.tensor_tensor(out=xm[:], in0=psall[:, 0:D], in1=Gmsa[:], op=ALU.mult), pb[0:4] + [b_c], [b_xm])
                    G(lambda: nc.gpsimd.tensor_tensor(out=xm[:], in0=xm[:], in1=xt[i][:], op=ALU.add), [b_xm, b_xt[i]], [b_xm])
                    S.dma(S.sp, resid_d[b, t * 128:(t + 1) * 128, :], xm[:], reads=[b_xm] + xsrc_buf(l, b, t), writes=[b_resid[b][t]])
                    A(lambda: nc.scalar.activation(out=hf[:], in_=xm[:], func=AF.Square, accum_out=col[:, 0:1]), [b_xm], [b_hf, b_col])
                    rstd_col(col[:, 1:2], col[:, 0:1], 1.0 / D, EPS, b_col)
                    V(lambda: nc.vector.scalar_tensor_tensor(out=hf[:], in0=xm[:], scalar=col[:, 1:2], in1=Amul[:], op0=ALU.mult, op1=ALU.mult), [b_xm, b_col, b_c], [b_hf])
                    V(lambda: nc.vector.tensor_tensor(out=hf[:], in0=hf[:], in1=Badd[:], op=ALU.add), [b_hf, b_c], [b_hf])
                    G(lambda: nc.gpsimd.tensor_copy(out=hb[:], in_=hf[:]), [b_hf], [b_hb])
                    for g_ in range(2):
                        transpose_group(hT_[i][:, g_ * 8:(g_ + 1) * 8, :], [hb[:, (g_ * 8 + j) * 128:(g_ * 8 + j + 1) * 128] for j in range(8)],
                                        4 + g_, b_hb, b_hT_[i], evac="act" if g_ else "dve")
                    S.dma(S.sp, H2T_d[b, :, :, t * 128:(t + 1) * 128], hT_[i][:], reads=[b_hT_[i]], writes=[b_H2T[b][t]])
                    if moe:
                        for g_ in range(4):
                            transpose_group(h32T[:, g_ * 4:(g_ + 1) * 4, :], [hf[:, (g_ * 4 + j) * 128:(g_ * 4 + j + 1) * 128] for j in range(4)],
                                            6 + g_ % 2, b_hf, b_h32T, dt_bf=False, evac="act" if g_ % 2 else "dve")

                        def mmr():
                            for k in range(16):
                                ins = nc.tensor.matmul(ps(6, 8), lhsT=h32T[:, k, :], rhs=rt[:, k, :], start=(k == 0), stop=(k == 15))
                            return ins
                        T(mmr, [b_h32T, b_rt], [pb[6]])
                        V(lambda: nc.vector.tensor_copy(out=lg[:, 0:8], in_=ps(6, 8)), [pb[6]], [b_lg])
                        V(lambda: nc.vector.reduce_max(out=col[:, 4:5], in_=lg[:, 0:8], axis=AX.X), [b_lg], [b_col])
                        V(lambda: nc.vector.tensor_scalar(out=lg[:, 8:16], in0=lg[:, 0:8], scalar1=col[:, 4:5], scalar2=-1e30, op0=ALU.is_ge, op1=ALU.mult), [b_lg, b_col], [b_lg])
                        V(lambda: nc.vector.tensor_tensor(out=lg[:, 8:16], in0=lg[:, 8:16], in1=lg[:, 0:8], op=ALU.add), [b_lg], [b_lg])
                        V(lambda: nc.vector.reduce_max(out=col[:, 5:6], in_=lg[:, 8:16], axis=AX.X), [b_lg], [b_col])
                        V(lambda: nc.vector.tensor_scalar(out=col[:, 6:7], in0=col[:, 4:5], scalar1=-1.0, scalar2=None, op0=ALU.mult), [b_col], [b_col])
                        A(lambda: nc.scalar.activation(out=lg[:, 16:24], in_=lg[:, 0:8], func=AF.Exp, bias=col[:, 6:7], scale=1.0), [b_lg, b_col], [b_lg])
                        V(lambda: nc.vector.tensor_scalar(out=lg[:, 24:32], in0=lg[:, 0:8], scalar1=col[:, 5:6], scalar2=None, op0=ALU.is_ge), [b_lg, b_col], [b_lg])
                        V(lambda: nc.vector.tensor_tensor(out=lg[:, 16:24], in0=lg[:, 16:24], in1=lg[:, 24:32], op=ALU.mult), [b_lg], [b_lg])
                        V(lambda: nc.vector.reduce_sum(out=col[:, 7:8], in_=lg[:, 16:24], axis=AX.X), [b_lg], [b_col])
                        V(lambda: nc.vector.reciprocal(out=col[:, 8:9], in_=col[:, 7:8]), [b_col], [b_col])
                        V(lambda: nc.vector.tensor_scalar(out=gt[i][:], in0=lg[:, 16:24], scalar1=col[:, 8:9], scalar2=None, op0=ALU.mult), [b_lg, b_col], [b_gt[i]])
                        S.dma(S.sp, GATES_d[b, t * 128:(t + 1) * 128, :], gt[i][:], reads=[b_gt[i]], writes=[b_GATES[b][t]])
                S.barrier()

        def phase_E(b, l, tiles):
            moe = (l % 2 == 1)
            last = (l == L - 1)
            nt = len(tiles)
            t0 = tiles[0]
            ntok = nt * 128
            FB = 256
            if ntok % 512 == 0:
                tblocks = [(o, 512) for o in range(0, ntok, 512)]
            else:
                tblocks = [(o, 384) for o in range(0, ntok, 384)]
            with ExitStack() as ph:
                h2T = sbt(ph, [128, 16, ntok], BF16); b_h2T = Buf()
                S.dma(S.sp, h2T[:], H2T_d[b, :, :, t0 * 128:t0 * 128 + ntok], reads=[b_H2T[b][t] for t in tiles], writes=[b_h2T])
                acc = sbt(ph, [128, nt, D]); b_acc = [Buf() for _ in range(nt)]
                if moe:
                    gts = sbt(ph, [128, nt, 8]); b_g = Buf()
                    S.dma(S.sp, gts[:], GATES_d[b, t0 * 128:t0 * 128 + ntok, :].rearrange("(n p) e -> p n e", p=128), reads=[b_GATES[b][t] for t in tiles], writes=[b_g])
                ph2 = ExitStack()
                w1b = [sbt(ph2, [128, 16, FB], BF16) for _ in range(2)]; w3b = [sbt(ph2, [128, 16, FB], BF16) for _ in range(2)]
                w2b = [sbt(ph2, [128, FB // 128, D], BF16) for _ in range(2)]; b_wb = [Buf(), Buf()]
                sg = [sbt(ph2, [128, 512]) for _ in range(2)]; b_sg = [Buf(), Buf()]
                aT = [sbt(ph2, [128, FB // 128, ntok], BF16) for _ in range(2)]; b_aT = [Buf(), Buf()]
                if moe:
                    experts = [(moe_w1[l // 2, e], moe_w3[l // 2, e], moe_w2[l // 2, e], EXD, e) for e in range(8)]
                else:
                    experts = [(ffn_w1[l // 2], ffn_w3[l // 2], ffn_w2[l // 2], FFN, None)]
                it = 0
                gu = 0
                ob = 0
                for (W1, W3, W2, F_, e) in experts:
                    for fb in range(F_ // FB):
                        f0 = fb * FB
                        wi = it % 2
                        S.dma(S.pool, w1b[wi][:], W1[:, f0:f0 + FB].rearrange("(j p) n -> p j n", p=128), writes=[b_wb[wi]])
                        S.dma(S.pool, w3b[wi][:], W3[:, f0:f0 + FB].rearrange("(j p) n -> p j n", p=128), writes=[b_wb[wi]])
                        S.dma(S.pool, w2b[wi][:], W2[f0:f0 + FB, :].rearrange("(c p) n -> p c n", p=128), writes=[b_wb[wi]])
                        for (o_, w_) in tblocks:
                            for fc in range(FB // 128):
                                bg_, bu_ = (0, 1) if gu % 2 == 0 else (2, 3)
                                si = gu % 2
                                gu += 1

                                def mmgu():
                                    for k in range(16):
                                        nc.tensor.matmul(ps(bg_, w_), lhsT=w1b[wi][:, k, fc * 128:(fc + 1) * 128], rhs=h2T[:, k, o_:o_ + w_], start=(k == 0), stop=(k == 15))
                                    for k in range(16):
                                        ins = nc.tensor.matmul(ps(bu_, w_), lhsT=w3b[wi][:, k, fc * 128:(fc + 1) * 128], rhs=h2T[:, k, o_:o_ + w_], start=(k == 0), stop=(k == 15))
                                    return ins
                                T(mmgu, [b_wb[wi], b_h2T], [pb[bg_], pb[bu_]])
                                A(lambda: nc.scalar.activation(out=sg[si][:, 0:w_], in_=ps(bg_, w_), func=AF.Silu), [pb[bg_]], [b_sg[si]])
                                V(lambda: nc.vector.tensor_tensor(out=aT[wi][:, fc, o_:o_ + w_], in0=sg[si][:, 0:w_], in1=ps(bu_, w_), op=ALU.mult), [pb[bu_], b_sg[si]], [b_aT[wi]])
                        first = (it == 0)
                        for ti in range(nt):
                            for dblk in range(4):
                                bo = 4 + ob % 4
                                ob += 1

                                def mm2_():
                                    for fc in range(FB // 128):
                                        ins = nc.tensor.matmul(ps(bo, 512), lhsT=aT[wi][:, fc, ti * 128:(ti + 1) * 128], rhs=w2b[wi][:, fc, dblk * 512:(dblk + 1) * 512],
                                                               start=(fc == 0), stop=(fc == FB // 128 - 1))
                                    return ins
                                T(mm2_, [b_aT[wi], b_wb[wi]], [pb[bo]])
                                dst = acc[:, ti, dblk * 512:(dblk + 1) * 512]
                                if moe:
                                    gcol_ = gts[:, ti, e:e + 1]
                                    if first:
                                        V(lambda: nc.vector.tensor_scalar(out=dst, in0=ps(bo, 512), scalar1=gcol_, scalar2=None, op0=ALU.mult), [pb[bo], b_g], [b_acc[ti]])
                                    else:
                                        V(lambda: nc.vector.scalar_tensor_tensor(out=dst, in0=ps(bo, 512), scalar=gcol_, in1=dst, op0=ALU.mult, op1=ALU.add), [pb[bo], b_g, b_acc[ti]], [b_acc[ti]])
                                else:
                                    if first:
                                        V(lambda: nc.vector.tensor_copy(out=dst, in_=ps(bo, 512)), [pb[bo]], [b_acc[ti]])
                                    else:
                                        V(lambda: nc.vector.tensor_tensor(out=dst, in0=dst, in1=ps(bo, 512), op=ALU.add), [pb[bo], b_acc[ti]], [b_acc[ti]])
                        it += 1
                S.barrier()
                ph2.close()
                G5 = sbt(ph, [128, D]); b_c = Buf()
                xt = [sbt(ph, [128, D]) for _ in range(2)]; b_xt = [Buf(), Buf()]
                if last:
                    fg = sbt(ph, [128, D]); col = sbt(ph, [128, 4]); b_col = Buf()
                    S.dma(S.sp, fg[:], final_g.partition_broadcast(128), writes=[b_c])
                cur_stream = None
                for ti, t in enumerate(tiles):
                    i = ti % 2
                    stream = 0 if t < 2 else 1
                    if stream != cur_stream:
                        S.dma(S.sp, G5[:], mod_row(l, b, stream, 5), reads=[b_mods], writes=[b_c])
                        cur_stream = stream
                    S.dma(S.sp, xt[i][:], resid_d[b, t * 128:(t + 1) * 128, :], reads=[b_resid[b][t]], writes=[b_xt[i]])
                    V(lambda: nc.vector.tensor_tensor(out=acc[:, ti, :], in0=acc[:, ti, :], in1=G5[:], op=ALU.mult), [b_acc[ti], b_c], [b_acc[ti]])
                    G(lambda: nc.gpsimd.tensor_tensor(out=xt[i][:], in0=xt[i][:], in1=acc[:, ti, :], op=ALU.add), [b_acc[ti], b_xt[i]], [b_xt[i]])
                    if not last:
                        S.dma(S.sp, resid_d[b, t * 128:(t + 1) * 128, :], xt[i][:], reads=[b_xt[i]], writes=[b_resid[b][t]])
                    else:
                        A(lambda: nc.scalar.activation(out=acc[:, ti, :], in_=xt[i][:], func=AF.Square, accum_out=col[:, 0:1]), [b_xt[i]], [b_acc[ti], b_col])
                        rstd_col(col[:, 1:2], col[:, 0:1], 1.0 / D, EPS, b_col)
                        V(lambda: nc.vector.scalar_tensor_tensor(out=xt[i][:], in0=xt[i][:], scalar=col[:, 1:2], in1=fg[:], op0=ALU.mult, op1=ALU.mult), [b_col, b_c, b_xt[i]], [b_xt[i]])
                        S.dma(S.sp, out_d[b, (t - 2) * 128:(t - 1) * 128, :], xt[i][:], reads=[b_xt[i]], writes=[b_out])
                S.barrier()

        if start is None:
            prologue()
        done = False
        for b in range(n_seq):
            for l in range(L):
                if start is None:
                    with ExitStack() as ph0:
                        hT = sbt(ph0, [128, 16, NT], BF16); b_hT = Buf()
                        phase_A(b, l, hT, b_hT)
                        phase_B(b, l, hT, b_hT)
                        S.barrier()
                if stop == "B":
                    done = True; break
                if only in (None, "mla"):
                    mixer_mla(b, l)
                if stop == "mla":
                    done = True; break
                if only in (None, "gqa"):
                    mixer_gqa_swa(b, l, False)
                if only in (None, "swa"):
                    mixer_gqa_swa(b, l, True)
                if stop == "attn":
                    done = True; break
                if only in (None, "gdn"):
                    gdn_prep(b, l)
                    gdn_scan(b, l)
                    gdn_final(b, l)
                if stop == "mix":
                    done = True; break
                phase_D(b, l)
                if stop == "D":
                    done = True; break
                tl = list(qtiles(l))
                half = len(tl) // 2
                phase_E(b, l, tl[:half])
                phase_E(b, l, tl[half:])
            if done:
                break
        S.finish()
    return nc


def _host_consts():
    def tables(rot):
        rows = 2048 // 64
        row = np.repeat(np.arange(rows), 64).astype(np.float32)
        colp = np.tile(np.arange(64), rows).astype(np.float32)
        half = rot // 2
        inv = (10000.0 ** (-np.arange(0, half, 2, dtype=np.float32) / half)).astype(np.float32)
        ar = row[:, None] * inv
        ac = colp[:, None] * inv
        ang = np.concatenate([ar, ar, ac, ac], -1).astype(np.float32)
        q = rot // 4
        sign = np.concatenate([-np.ones(q), np.ones(q), -np.ones(q), np.ones(q)]).astype(np.float32)
        return np.cos(ang).astype(np.float32), (np.sin(ang) * sign[None]).astype(np.float32)
    c128, s128 = tables(128)
    c64, s64 = tables(64)
    cst = np.zeros((128, C_W), np.float32)
    i = np.arange(128)[:, None]
    j = np.arange(128)[None, :]
    cst[:, C_ID:C_ID + 128] = (i == j)
    cst[:, C_ONE:C_ONE + 128] = 1.0
    cst[:, C_SU:C_SU + 128] = np.where(j >= i, 0.0, NEG)
    cst[:, C_SL:C_SL + 128] = np.where(j <= i, 0.0, NEG)
    p = np.arange(64)[:, None]
    f = np.arange(64)[None, :]
    cst[0:64, C_U64:C_U64 + 64] = (p <= f)
    cst[0:64, C_L64:C_L64 + 64] = (p >= f)
    for d in range(2):
        vN = (f < p) if d == 0 else (f > p)
        vM = (f >= p) if d == 0 else (f <= p)
        vS = (f > p) if d == 0 else (f < p)
        cst[0:64, C_MN[d]:C_MN[d] + 256] = np.tile(np.where(vN, 0.0, NEG), (1, 4))
        cst[0:64, C_MM[d]:C_MM[d] + 256] = np.tile(np.where(vM, 0.0, NEG), (1, 4))
        cst[0:64, C_ST[d]:C_ST[d] + 256] = np.tile(vS.astype(np.float32), (1, 4))
    return dict(rc128=np.tile(c128, (1, 4)), rs128=np.tile(s128, (1, 4)), rc64=np.tile(c64, (1, 4)), rs64=np.tile(s64, (1, 4)), cst=cst)


_PROG = {}


def kernel(**inputs):
    n_cores = 8
    n_seq = 2
    if "full" not in _PROG:
        _PROG["full"] = build_program(n_seq=n_seq, n_layers=4)
    nc = _PROG["full"]
    consts = _host_consts()
    shared = {k: np.ascontiguousarray(v) for k, v in inputs.items() if k not in ("x", "c", "ctx")}
    shared["gdn_a_log"] = np.ascontiguousarray(inputs["gdn_a_log"]).reshape(4, 8)
    shared["gdn_dt_bias"] = np.ascontiguousarray(inputs["gdn_dt_bias"]).reshape(4, 8)
    shared.update(consts)
    in_maps = []
    for i in range(n_cores):
        m = dict(shared)
        m["x"] = np.ascontiguousarray(inputs["x"][i * n_seq:(i + 1) * n_seq])
        m["c"] = np.ascontiguousarray(inputs["c"][i * n_seq:(i + 1) * n_seq])
        m["ctx"] = np.ascontiguousarray(inputs["ctx"][i * n_seq:(i + 1) * n_seq])
        in_maps.append(m)
    res = run_bass_kernel_spmd(nc, in_maps, core_ids=list(range(n_cores)))
    return np.concatenate([np.asarray(r["out"]) for r in res.results], axis=0).astype(np.float32)
```

```python
import numpy as np
import concourse.bass as bass
import concourse.mybir as mybir
from concourse.bass_utils import run_bass_kernel_spmd
from contextlib import ExitStack

F32 = mybir.dt.float32
BF16 = mybir.dt.bfloat16
ALU = mybir.AluOpType
AF = mybir.ActivationFunctionType
AX = mybir.AxisListType

D = 2048
NT = 2304
NTILE = 18
DIN = 4816
FFN = 5632
EXD = 4096
NEG = -30000.0
EPS = 1e-6
O_CQ, O_CKV, O_KR = 0, 384, 640
O_BQ, O_BK, O_BV = 704, 1216, 1472
O_GQ, O_GK, O_GV, O_GZ, O_GA, O_GB = 1728, 2240, 2752, 3264, 3776, 3784
O_DQ, O_DK, O_DV = 3792, 4304, 4560

C_ID, C_ONE, C_SU, C_SL, C_U64, C_L64 = 0, 128, 256, 384, 512, 576
C_MN = (640, 640 + 768)
C_MM = (640 + 256, 640 + 768 + 256)
C_ST = (640 + 512, 640 + 768 + 512)
C_W = 640 + 1536

_DBG = {}


class Buf:
    __slots__ = ("w", "r")

    def __init__(self):
        self.w = None
        self.r = {}


class Eng:
    def __init__(self, key, obj, sem, is_pe=False):
        self.key, self.obj, self.sem, self.is_pe = key, obj, sem, is_pe
        self.cnt = 0
        self.seen = {}
        self.slots = []
        self.slot_i = 0


class Slot:
    def __init__(self, key, sem):
        self.key, self.sem, self.cnt = key, sem, 0


class Sched:
    def __init__(self, nc, stack, n_dma_slots=12):
        self.nc = nc
        mk = lambda n: stack.enter_context(nc.semaphore(n))
        self.pe = Eng("pe", nc.tensor, mk("s_pe"), is_pe=True)
        self.dve = Eng("dve", nc.vector, mk("s_dve"))
        self.act = Eng("act", nc.scalar, mk("s_act"))
        self.pool = Eng("pool", nc.gpsimd, mk("s_pool"))
        self.sp = Eng("sp", nc.sync, mk("s_sp"))
        self.engs = [self.pe, self.dve, self.act, self.pool, self.sp]
        for q in (self.sp, self.pool, self.act):
            q.slots = [Slot(f"d_{q.key}{i}", mk(f"d_{q.key}{i}")) for i in range(n_dma_slots)]

    def _wait(self, eng, dep):
        key, sem, val = dep
        if eng.seen.get(key, 0) >= val:
            return
        eng.obj.wait_ge(sem, val)
        eng.seen[key] = val

    def _deps(self, eng, reads, writes):
        for b in reads:
            if b.w is not None and not (eng.is_pe and b.w[0] == eng.key):
                self._wait(eng, b.w)
        for b in writes:
            if b.w is not None and not (eng.is_pe and b.w[0] == eng.key):
                self._wait(eng, b.w)
            for d in b.r.values():
                if d[0] != eng.key:
                    self._wait(eng, d)

    def _mark(self, tok, reads, writes):
        for b in reads:
            b.r[tok[0]] = tok
        for b in writes:
            b.w = tok
            b.r = {}

    def op(self, eng, fn, reads=(), writes=()):
        self._deps(eng, reads, writes)
        ins = fn()
        eng.cnt += 1
        ins.then_inc(eng.sem, 1)
        self._mark((eng.key, eng.sem, eng.cnt), reads, writes)

    def dma(self, q, out, in_, reads=(), writes=(), **kw):
        slot = q.slots[q.slot_i % len(q.slots)]
        q.slot_i += 1
        if slot.cnt > 0:
            self._wait(q, (slot.key, slot.sem, slot.cnt * 16))
        self._deps(q, reads, writes)
        ins = q.obj.dma_start(out=out, in_=in_, **kw)
        slot.cnt += 1
        ins.then_inc(slot.sem, 16)
        self._mark((slot.key, slot.sem, slot.cnt * 16), reads, writes)

    def barrier(self):
        toks = [(e.key, e.sem, e.cnt) for e in self.engs if e.cnt > 0]
        for q in (self.sp, self.pool, self.act):
            toks += [(s.key, s.sem, s.cnt * 16) for s in q.slots if s.cnt > 0]
        for e in self.engs:
            for tk in toks:
                if tk[0] != e.key:
                    self._wait(e, tk)

    def finish(self):
        self.barrier()


def build_program(n_seq=2, n_layers=4, stop=None, dbg=(), start=None, only=None):
    nc = bass.Bass("TRN2", target_bir_lowering=False)
    L = n_layers

    def din(name, shape, dt=F32):
        return nc.dram_tensor(name, list(shape), dt, kind="ExternalInput").ap()

    def dscr(name, shape, dt=F32):
        kind = "ExternalOutput" if name in dbg else "Internal"
        if start == "mix" and name == "P_d":
            kind = "ExternalInput"
        return nc.dram_tensor(name, list(shape), dt, kind=kind).ap()

    x_in = din("x", [n_seq, 2048, D]); ctx_in = din("ctx", [n_seq, 256, D])
    c_in = din("c", [n_seq, D]); cctx_in = din("c_ctx", [D])
    norm1_g = din("norm1_g", [L, D]); norm2_g = din("norm2_g", [L, D])
    w_mod = din("w_mod", [L, D, 6 * D]); b_mod = din("b_mod", [L, 6 * D])
    w_in = din("w_in", [L, D, DIN])
    mla_qg = din("mla_q_norm_g", [L, 384]); mla_kvg = din("mla_kv_norm_g", [L, 256])
    mla_wuq = din("mla_w_uq", [L, 384, 768]); mla_wukv = din("mla_w_ukv", [L, 256, 1024])
    gqa_qg = din("gqa_q_norm_g", [L, 128]); gqa_kg = din("gqa_k_norm_g", [L, 128])
    gdn_cw = din("gdn_conv_w", [L, 5, 1536]); gdn_alog = din("gdn_a_log", [L, 8]); gdn_dtb = din("gdn_dt_bias", [L, 8])
    gdn_ng = din("gdn_norm_g", [L, 128]); swa_sink = din("swa_sink", [L, 4])
    w_out = din("w_out", [L, D, D])
    NF = (L + 1) // 2; NM = L // 2
    ffn_w1 = din("ffn_w1", [NF, D, FFN]); ffn_w3 = din("ffn_w3", [NF, D, FFN]); ffn_w2 = din("ffn_w2", [NF, FFN, D])
    if NM > 0:
        moe_router = din("moe_router", [NM, D, 8])
        moe_w1 = din("moe_w1", [NM, 8, D, EXD]); moe_w3 = din("moe_w3", [NM, 8, D, EXD]); moe_w2 = din("moe_w2", [NM, 8, EXD, D])
    final_g = din("final_norm_g", [D])
    rc128 = din("rc128", [2048, 512]); rs128 = din("rs128", [2048, 512])
    rc64 = din("rc64", [2048, 256]); rs64 = din("rs64", [2048, 256])
    cst_in = din("cst", [128, C_W])
    out_d = nc.dram_tensor("out", [n_seq, 2048, D], F32, kind="ExternalOutput").ap()

    mods_d = dscr("mods_d", [L, n_seq + 1, 6 * D])
    resid_d = dscr("resid_d", [n_seq, NT, D])
    P_d = dscr("P_d", [n_seq, NT, DIN])
    Y_d = dscr("Y_d", [n_seq, NT, D], BF16)
    H2T_d = dscr("H2T_d", [n_seq, 128, 16, NT], BF16)
    GATES_d = dscr("GATES_d", [n_seq, NT, 8])
    GTOK_d = dscr("GTOK_d", [n_seq, NT, 1040])
    GFT_d = dscr("GFT_d", [n_seq, 8, 128, NT])
    GO_d = dscr("GO_d", [n_seq, 2, NT, 512])

    with ExitStack() as st:
        S = Sched(nc, st)

        def V(fn, r=(), w=()): S.op(S.dve, fn, r, w)
        def A(fn, r=(), w=()): S.op(S.act, fn, r, w)
        def G(fn, r=(), w=()): S.op(S.pool, fn, r, w)
        def T(fn, r=(), w=()): S.op(S.pe, fn, r, w)

        _cnt = [0]

        def sbt(stack, shape, dt=F32, name=None):
            _cnt[0] += 1
            return stack.enter_context(nc.sbuf_tensor(name or f"sb{_cnt[0]}", list(shape), dt))

        psall = st.enter_context(nc.psum_tensor("psall", [128, 4096], F32))
        psallb = psall.bitcast(BF16)
        pb = [Buf() for _ in range(8)]

        def ps(bank, n=512, parts=128, off=0):
            return psall[0:parts, bank * 512 + off: bank * 512 + off + n]

        def psb(bank, n=1024, parts=128, off=0):
            return psallb[0:parts, bank * 1024 + off: bank * 1024 + off + n]

        b_mods = Buf()
        b_resid = [[Buf() for _ in range(NTILE)] for _ in range(n_seq)]
        b_P = [[Buf() for _ in range(NTILE)] for _ in range(n_seq)]
        b_Y = [[Buf() for _ in range(NTILE)] for _ in range(n_seq)]
        b_H2T = [[Buf() for _ in range(NTILE)] for _ in range(n_seq)]
        b_GATES = [[Buf() for _ in range(NTILE)] for _ in range(n_seq)]
        b_GTOK = [[Buf() for _ in range(NTILE)] for _ in range(n_seq)]
        b_GFT = [[Buf() for _ in range(NTILE)] for _ in range(n_seq)]
        b_GO = [[[Buf() for _ in range(NTILE)] for _ in range(2)] for _ in range(n_seq)]
        b_out = Buf()

        cst = sbt(st, [128, C_W], F32, "cst_sb")
        identb = sbt(st, [128, 128], BF16, "identb")
        swab = sbt(st, [128, 256], BF16, "swab")
        b_cst = Buf()
        S.dma(S.sp, cst[:], cst_in[:, :], writes=[b_cst])
        V(lambda: nc.vector.tensor_copy(out=identb[:], in_=cst[:, C_ID:C_ID + 128]), [b_cst], [b_cst])
        V(lambda: nc.vector.tensor_copy(out=swab[:], in_=cst[:, C_SU:C_SU + 256]), [b_cst], [b_cst])
        ident = cst[:, C_ID:C_ID + 128]
        ones = cst[:, C_ONE:C_ONE + 128]

        def xsrc(l, b, t):
            if l == 0:
                if t < 2:
                    return ctx_in[b, t * 128:(t + 1) * 128, :]
                return x_in[b, (t - 2) * 128:(t - 1) * 128, :]
            return resid_d[b, t * 128:(t + 1) * 128, :]

        def xsrc_buf(l, b, t):
            return [] if l == 0 else [b_resid[b][t]]

        def rstd_col(out_col, ssq_col, scale, eps, b_in):
            A(lambda: nc.scalar.activation(out=out_col, in_=ssq_col, func=AF.Sqrt, scale=scale, bias=eps), [b_in], [b_in])
            V(lambda: nc.vector.reciprocal(out=out_col, in_=out_col), [b_in], [b_in])

        def transpose_group(dst, srcs, bank, b_src, b_dst, dt_bf=True, evac="act", parts_out=128):
            n = len(srcs)
            K = srcs[0].shape[0]
            M = srcs[0].shape[1]

            def tr():
                for i, s_ in enumerate(srcs):
                    if dt_bf:
                        ins = nc.tensor.transpose(out=psb(bank, K, M, i * K), in_=s_, identity=identb[0:K, 0:K])
                    else:
                        ins = nc.tensor.transpose(out=ps(bank, K, M, i * K), in_=s_, identity=ident[0:K, 0:K])
                return ins
            T(tr, [b_src, b_cst], [pb[bank]])
            src = (psb(bank, n * K, M) if dt_bf else ps(bank, n * K, M)).rearrange("p (n k) -> p n k", k=K)
            if evac == "act":
                A(lambda: nc.scalar.copy(out=dst, in_=src), [pb[bank]], [b_dst])
            else:
                V(lambda: nc.vector.tensor_copy(out=dst, in_=src), [pb[bank]], [b_dst])

        def rope(out3, x3, cos3, sin3, tmp1, tmp2, q, bufs_r, bufs_w):
            V(lambda: nc.vector.tensor_tensor(out=tmp1, in0=x3, in1=cos3, op=ALU.mult), bufs_r, bufs_w)

            def sw():
                for a in range(2):
                    for bd in range(2):
                        bs = 1 - bd
                        o0 = (2 * a + bd) * q
                        s0 = (2 * a + bs) * q
                        ins = nc.vector.tensor_tensor(out=tmp2[:, :, o0:o0 + q], in0=x3[:, :, s0:s0 + q], in1=sin3[:, :, o0:o0 + q], op=ALU.mult)
                return ins
            V(sw, bufs_r, bufs_w)
            V(lambda: nc.vector.tensor_tensor(out=out3, in0=tmp1, in1=tmp2, op=ALU.add), bufs_r, bufs_w)

        def prologue():
            R_ = n_seq + 1
            with ExitStack() as ph:
                cT = sbt(ph, [128, 16, R_]); b_cT = Buf()
                modsb = sbt(ph, [R_, 6 * D]); b_modsb = Buf()
                bm = sbt(ph, [R_, 6 * D]); b_bm = Buf()
                wbuf = [sbt(ph, [128, 16, 512]) for _ in range(2)]; b_w = [Buf(), Buf()]
                for r in range(R_):
                    src = c_in[r] if r < n_seq else cctx_in
                    S.dma(S.sp, cT[:, :, r:r + 1], src.rearrange("(j p o) -> p j o", p=128, o=1), writes=[b_cT], allow_slow_non_contiguous=True)
                A(lambda: nc.scalar.activation(out=cT[:], in_=cT[:], func=AF.Silu), [b_cT], [b_cT])
                for l in range(L):
                    S.dma(S.sp, bm[:], b_mod[l].partition_broadcast(R_), writes=[b_bm])
                    for n in range(24):
                        wb_ = wbuf[n % 2]
                        S.dma(S.sp if n % 2 else S.act, wb_[:], w_mod[l, :, n * 512:(n + 1) * 512].rearrange("(j p) n -> p j n", p=128), writes=[b_w[n % 2]])
                        bank = n % 2

                        def mm():
                            for k in range(16):
                                ins = nc.tensor.matmul(ps(bank, 512, R_), lhsT=cT[:, k, :], rhs=wb_[:, k, :], start=(k == 0), stop=(k == 15))
                            return ins
                        T(mm, [b_cT, b_w[n % 2]], [pb[bank]])
                        V(lambda: nc.vector.tensor_tensor(out=modsb[:, n * 512:(n + 1) * 512], in0=ps(bank, 512, R_), in1=bm[:, n * 512:(n + 1) * 512], op=ALU.add),
                          [pb[bank], b_bm], [b_modsb])
                    S.dma(S.sp, mods_d[l], modsb[:], reads=[b_modsb], writes=[b_mods])
                S.barrier()

        def mod_row(l, b, stream, idx):
            row = n_seq if stream == 0 else b
            return mods_d[l, row, idx * D:(idx + 1) * D].partition_broadcast(128)

        def build_mod_consts(ph_tiles, l, b, stream, gain_ap, i_shift, i_scale, b_c):
            Amul, Badd, tmp = ph_tiles
            S.dma(S.sp, tmp[:], gain_ap.partition_broadcast(128), writes=[b_c])
            S.dma(S.sp, Amul[:], mod_row(l, b, stream, i_scale), reads=[b_mods], writes=[b_c])
            S.dma(S.sp, Badd[:], mod_row(l, b, stream, i_shift), reads=[b_mods], writes=[b_c])
            V(lambda: nc.vector.scalar_tensor_tensor(out=Amul[:], in0=Amul[:], scalar=1.0, in1=tmp[:], op0=ALU.add, op1=ALU.mult), [b_c], [b_c])

        def phase_A(b, l, hT, b_hT):
            with ExitStack() as ph:
                Amul = sbt(ph, [128, D]); Badd = sbt(ph, [128, D]); tmp = sbt(ph, [128, D]); b_c = Buf()
                xt = [sbt(ph, [128, D]) for _ in range(2)]; b_xt = [Buf(), Buf()]
                hf = sbt(ph, [128, D]); b_hf = Buf()
                hb = [sbt(ph, [128, D], BF16) for _ in range(2)]; b_hb = [Buf(), Buf()]
                col = sbt(ph, [128, 4]); b_col = Buf()
                for t in range(NTILE):
                    if t == 0 or t == 2:
                        build_mod_consts((Amul, Badd, tmp), l, b, 0 if t == 0 else 1, norm1_g[l], 0, 1, b_c)
                    i = t % 2
                    S.dma(S.sp, xt[i][:], xsrc(l, b, t), reads=xsrc_buf(l, b, t), writes=[b_xt[i]])
                    A(lambda: nc.scalar.activation(out=hf[:], in_=xt[i][:], func=AF.Square, accum_out=col[:, 0:1]), [b_xt[i]], [b_hf, b_col])
                    rstd_col(col[:, 1:2], col[:, 0:1], 1.0 / D, EPS, b_col)
                    V(lambda: nc.vector.scalar_tensor_tensor(out=hf[:], in0=xt[i][:], scalar=col[:, 1:2], in1=Amul[:], op0=ALU.mult, op1=ALU.mult),
                      [b_xt[i], b_col, b_c], [b_hf])
                    G(lambda: nc.gpsimd.tensor_tensor(out=hb[i][:], in0=hf[:], in1=Badd[:], op=ALU.add), [b_hf, b_c], [b_hb[i]])
                    for g_ in range(2):
                        transpose_group(hT[:, g_ * 8:(g_ + 1) * 8, t * 128:(t + 1) * 128],
                                        [hb[i][:, (g_ * 8 + j) * 128:(g_ * 8 + j + 1) * 128] for j in range(8)],
                                        4 + g_, b_hb[i], b_hT, evac="act" if g_ else "dve")
                S.barrier()

        def phase_B(b, l, hT, b_hT):
            with ExitStack() as ph:
                wb_ = [sbt(ph, [128, 16, 512], BF16) for _ in range(2)]; b_wb = [Buf(), Buf()]
                stg = [sbt(ph, [128, 512]) for _ in range(4)]; b_stg = [Buf() for _ in range(4)]
                chunks = [(c0, min(512, DIN - c0)) for c0 in range(0, DIN, 512)]
                it = 0
                for ci, (c0, cw) in enumerate(chunks):
                    w_ = wb_[ci % 2]
                    S.dma(S.pool, w_[:, :, 0:cw], w_in[l, :, c0:c0 + cw].rearrange("(j p) n -> p j n", p=128), writes=[b_wb[ci % 2]])
                    for t in range(NTILE):
                        bank = it % 4
                        it += 1

                        def mm():
                            for k in range(16):
                                ins = nc.tensor.matmul(ps(bank, cw), lhsT=hT[:, k, t * 128:(t + 1) * 128], rhs=w_[:, k, 0:cw], start=(k == 0), stop=(k == 15))
                            return ins
                        T(mm, [b_hT, b_wb[ci % 2]], [pb[bank]])
                        if it % 2:
                            A(lambda: nc.scalar.copy(out=stg[bank][:, 0:cw], in_=ps(bank, cw)), [pb[bank]], [b_stg[bank]])
                        else:
                            V(lambda: nc.vector.tensor_copy(out=stg[bank][:, 0:cw], in_=ps(bank, cw)), [pb[bank]], [b_stg[bank]])
                        S.dma(S.sp, P_d[b, t * 128:(t + 1) * 128, c0:c0 + cw], stg[bank][:, 0:cw], reads=[b_stg[bank]], writes=[b_P[b][t]])
                S.barrier()

        def attn_core(A_, qparts, chunks, scale, out_ap, b_q, b_k, b_out, sink=None):
            n = len(chunks)
            nb = (n * 128 + 511) // 512
            sbufs = pb[0:nb]

            def qk():
                for ci, (kparts, v_ap, mask) in enumerate(chunks):
                    o = psall[:, ci * 128:(ci + 1) * 128]
                    np_ = len(kparts)
                    for pi in range(np_):
                        ins = nc.tensor.matmul(o, lhsT=qparts[pi], rhs=kparts[pi], start=(pi == 0), stop=(pi == np_ - 1 and mask is None))
                    if mask is not None:
                        ins = nc.tensor.matmul(o, lhsT=identb[:], rhs=mask, start=False, stop=True)
                return ins
            T(qk, [b_q, b_k, b_cst], sbufs)
            col = A_["col"]; b_col = A_["b_col"]
            V(lambda: nc.vector.reduce_max(out=col[:, 0:1], in_=psall[:, 0:n * 128], axis=AX.X), sbufs, [b_col])
            if sink is None:
                V(lambda: nc.vector.tensor_scalar(out=col[:, 1:2], in0=col[:, 0:1], scalar1=-scale, scalar2=None, op0=ALU.mult), [b_col], [b_col])
            else:
                V(lambda: nc.vector.tensor_scalar(out=col[:, 1:2], in0=col[:, 0:1], scalar1=-scale, scalar2=sink[1], op0=ALU.mult, op1=ALU.min), [b_col, b_cst], [b_col])
            pexp = A_["pexp"]; b_pexp = A_["b_pexp"]
            A(lambda: nc.scalar.activation(out=pexp[:, 0:n * 128], in_=psall[:, 0:n * 128], func=AF.Exp, bias=col[:, 1:2], scale=scale, accum_out=col[:, 2:3]),
              sbufs + [b_col], [b_pexp, b_col])
            if sink is not None:
                A(lambda: nc.scalar.activation(out=col[:, 3:4], in_=sink[0], func=AF.Exp, bias=col[:, 1:2], scale=1.0), [b_col, b_cst], [b_col])
                V(lambda: nc.vector.tensor_tensor(out=col[:, 2:3], in0=col[:, 2:3], in1=col[:, 3:4], op=ALU.add), [b_col], [b_col])
            V(lambda: nc.vector.reciprocal(out=col[:, 4:5], in_=col[:, 2:3]), [b_col], [b_col])
            PT = A_["PT"]; b_PT = A_["b_PT"]
            gi = 0
            for c0 in range(0, n, 8):
                c1 = min(n, c0 + 8)
                transpose_group(PT[:, c0:c1, :], [pexp[:, ci * 128:(ci + 1) * 128] for ci in range(c0, c1)], 5 + gi % 2, b_pexp, b_PT,
                                evac="act" if gi % 2 else "dve")
                gi += 1
            dv = chunks[0][1].shape[-1]

            def pv():
                for ci, (kparts, v_ap, mask) in enumerate(chunks):
                    ins = nc.tensor.matmul(ps(7, dv), lhsT=PT[:, ci, :], rhs=v_ap, start=(ci == 0), stop=(ci == n - 1))
                return ins
            T(pv, [b_PT, b_k], [pb[7]])
            V(lambda: nc.vector.tensor_scalar(out=out_ap, in0=ps(7, dv), scalar1=col[:, 4:5], scalar2=None, op0=ALU.mult), [pb[7], b_col], [b_out])

        def attn_scratch(ph):
            return dict(col=sbt(ph, [128, 8]), b_col=Buf(), pexp=sbt(ph, [128, NT], BF16), b_pexp=Buf(),
                        PT=sbt(ph, [128, NTILE, 128], BF16), b_PT=Buf())

        def qtiles(l):
            return range(NTILE) if l < L - 1 else range(2, NTILE)

        def load_rope(ph_t, src, t, w, b_r):
            S.dma(S.sp, ph_t[:, 0:w], src[(t - 2) * 128:(t - 1) * 128, 0:w], writes=[b_r])

        def mixer_mla(b, l):
            with ExitStack() as ph:
                wuq = sbt(ph, [128, 3, 768], BF16); wukv = sbt(ph, [128, 2, 1024], BF16); b_w = Buf()
                gq = sbt(ph, [128, 384]); gkv = sbt(ph, [128, 256])
                S.dma(S.pool, wuq[:], mla_wuq[l].rearrange("(j p) n -> p j n", p=128), writes=[b_w])
                S.dma(S.pool, wukv[:], mla_wukv[l].rearrange("(j p) n -> p j n", p=128), writes=[b_w])
                S.dma(S.sp, gq[:], mla_qg[l].partition_broadcast(128), writes=[b_w])
                S.dma(S.sp, gkv[:], mla_kvg[l].partition_broadcast(128), writes=[b_w])
                KnT = sbt(ph, [128, 4, NT], BF16); KrT = sbt(ph, [64, NT], BF16); Vv = sbt(ph, [128, NTILE, 4, 128], BF16); b_kv = Buf()
                pa = [sbt(ph, [128, 384]) for _ in range(2)]; b_pa = [Buf(), Buf()]
                junk = sbt(ph, [128, 768]); b_t = Buf()
                col = sbt(ph, [128, 4])
                xn = sbt(ph, [128, 384], BF16); xnT = sbt(ph, [128, 3, 128], BF16)
                knb = sbt(ph, [128, 4, 128], BF16); krb = sbt(ph, [128, 64], BF16)
                rc = sbt(ph, [128, 256]); rs = sbt(ph, [128, 256]); b_r = Buf()
                t1 = sbt(ph, [128, 256]); t2 = sbt(ph, [128, 256])
                qf = sbt(ph, [128, 768]); qb = sbt(ph, [128, 4, 192], BF16)
                QnT = sbt(ph, [128, 4, 128], BF16); QrT = sbt(ph, [64, 4, 128], BF16); b_q = Buf()
                yt = [sbt(ph, [128, 512], BF16) for _ in range(2)]; b_yt = [Buf(), Buf()]
                A_ = attn_scratch(ph)
                for t in range(NTILE):
                    i = t % 2
                    S.dma(S.sp, pa[i][:, 0:320], P_d[b, t * 128:(t + 1) * 128, O_CKV:O_CKV + 320], reads=[b_P[b][t]], writes=[b_pa[i]])
                    if t >= 2:
                        load_rope(rc, rc64, t, 64, b_r); load_rope(rs, rs64, t, 64, b_r)
                    s1 = _DBG.get("s1", 9)
                    if s1 < 2:
                        continue
                    A(lambda: nc.scalar.activation(out=junk[:, 0:256], in_=pa[i][:, 0:256], func=AF.Square, accum_out=col[:, 0:1]), [b_pa[i]], [b_t])
                    rstd_col(col[:, 1:2], col[:, 0:1], 1.0 / 256, EPS, b_t)
                    V(lambda: nc.vector.scalar_tensor_tensor(out=xn[:, 0:256], in0=pa[i][:, 0:256], scalar=col[:, 1:2], in1=gkv[:], op0=ALU.mult, op1=ALU.mult),
                      [b_pa[i], b_t, b_w], [b_t])
                    if s1 < 3:
                        continue
                    transpose_group(xnT[:, 0:2, :], [xn[:, j * 128:(j + 1) * 128] for j in range(2)], 5, b_t, b_t, evac="dve")

                    def mmkv():
                        for hb_ in range(2):
                            for k in range(2):
                                ins = nc.tensor.matmul(ps(5 + hb_, 512), lhsT=xnT[:, k, :], rhs=wukv[:, k, hb_ * 512:(hb_ + 1) * 512], start=(k == 0), stop=(k == 1))
                        return ins
                    if s1 < 3.2:
                        continue
                    T(mmkv, [b_t, b_w], [pb[5], pb[6]])
                    kv3 = psall[:, 5 * 512:7 * 512].rearrange("p (h c) -> p h c", c=256)
                    if s1 < 3.4:
                        continue
                    for hb_ in range(2):
                        kvb = ps(5 + hb_, 512).rearrange("p (h c) -> p h c", c=256)
                        A(lambda: nc.scalar.copy(out=Vv[:, t, 2 * hb_:2 * hb_ + 2, :], in_=kvb[:, :, 128:256]), [pb[5 + hb_]], [b_kv])
                    if s1 < 3.6:
                        continue
                    for hb_ in range(2):
                        kvb = ps(5 + hb_, 512).rearrange("p (h c) -> p h c", c=256)
                        A(lambda: nc.scalar.copy(out=knb[:, 2 * hb_:2 * hb_ + 2, :], in_=kvb[:, :, 0:128]), [pb[5 + hb_]], [b_t])
                    if s1 < 4:
                        continue
                    transpose_group(KnT[:, :, t * 128:(t + 1) * 128], [knb[:, h, :] for h in range(4)], 7, b_t, b_kv, evac="act")
                    if s1 < 5:
                        continue
                    if t >= 2:
                        x3 = pa[i][:, 256:320].unsqueeze(1)
                        rope(krb[:].unsqueeze(1), x3, rc[:, 0:64].unsqueeze(1), rs[:, 0:64].unsqueeze(1), t1[:, 0:64].unsqueeze(1), t2[:, 0:64].unsqueeze(1), 16,
                             [b_pa[i], b_r, b_t], [b_t])
                    else:
                        V(lambda: nc.vector.tensor_copy(out=krb[:], in_=pa[i][:, 256:320]), [b_pa[i]], [b_t])
                    if s1 < 6:
                        continue
                    transpose_group(KrT[:, t * 128:(t + 1) * 128].unsqueeze(1), [krb[:, :]], 7, b_t, b_kv, evac="dve")
                for t in (qtiles(l) if _DBG.get("mla_cut", 3) >= 2 else []):
                    i = t % 2
                    S.dma(S.sp, pa[i][:, 0:384], P_d[b, t * 128:(t + 1) * 128, O_CQ:O_CQ + 384], reads=[b_P[b][t]], writes=[b_pa[i]])
                    if t >= 2:
                        load_rope(rc, rc64, t, 256, b_r); load_rope(rs, rs64, t, 256, b_r)
                    A(lambda: nc.scalar.activation(out=junk[:, 0:384], in_=pa[i][:, 0:384], func=AF.Square, accum_out=col[:, 0:1]), [b_pa[i]], [b_t])
                    rstd_col(col[:, 1:2], col[:, 0:1], 1.0 / 384, EPS, b_t)
                    V(lambda: nc.vector.scalar_tensor_tensor(out=xn[:, 0:384], in0=pa[i][:, 0:384], scalar=col[:, 1:2], in1=gq[:], op0=ALU.mult, op1=ALU.mult),
                      [b_pa[i], b_t, b_w], [b_t])
                    transpose_group(xnT[:, 0:3, :], [xn[:, j * 128:(j + 1) * 128] for j in range(3)], 5, b_t, b_t, evac="dve")

                    def mmq():
                        for (bank, c0, cw) in ((5, 0, 512), (6, 512, 256)):
                            for k in range(3):
                                ins = nc.tensor.matmul(ps(bank, cw), lhsT=xnT[:, k, :], rhs=wuq[:, k, c0:c0 + cw], start=(k == 0), stop=(k == 2))
                        return ins
                    T(mmq, [b_t, b_w], [pb[5], pb[6]])
                    A(lambda: nc.scalar.copy(out=qf[:], in_=psall[:, 5 * 512:5 * 512 + 768]), [pb[5], pb[6]], [b_t])
                    qf3 = qf[:].rearrange("p (h c) -> p h c", c=192)
                    V(lambda: nc.vector.tensor_copy(out=qb[:, :, 0:128], in_=qf3[:, :, 0:128]), [b_t], [b_q])
                    if t >= 2:
                        rope(qb[:, :, 128:192], qf3[:, :, 128:192], rc[:].rearrange("p (h c) -> p h c", c=64), rs[:].rearrange("p (h c) -> p h c", c=64),
                             t1[:].rearrange("p (h c) -> p h c", c=64), t2[:].rearrange("p (h c) -> p h c", c=64), 16, [b_t, b_r], [b_q, b_t])
                    else:
                        V(lambda: nc.vector.tensor_copy(out=qb[:, :, 128:192], in_=qf3[:, :, 128:192]), [b_t], [b_q])
                    transpose_group(QnT[:], [qb[:, h, 0:128] for h in range(4)], 5, b_q, b_q, evac="act")
                    transpose_group(QrT[:], [qb[:, h, 128:192] for h in range(4)], 6, b_q, b_q, evac="dve")
                    nk = 2 if t < 2 else NTILE
                    for h in (range(4) if _DBG.get("mla_cut", 3) >= 3 else []):
                        chunks = [([KnT[:, h, kc * 128:(kc + 1) * 128], KrT[:, kc * 128:(kc + 1) * 128]], Vv[:, kc, h, :], None) for kc in range(nk)]
                        attn_core(A_, [QnT[:, h, :], QrT[:, h, :]], chunks, 192 ** -0.5, yt[i][:, h * 128:(h + 1) * 128], b_q, b_kv, b_yt[i])
                    S.dma(S.sp, Y_d[b, t * 128:(t + 1) * 128, 0:512], yt[i][:], reads=[b_yt[i]], writes=[b_Y[b][t]])
                S.barrier()

        def mixer_gqa_swa(b, l, swa):
            oq, ok, ov = (O_DQ, O_DK, O_DV) if swa else (O_BQ, O_BK, O_BV)
            ycol = 1536 if swa else 512
            with ExitStack() as ph:
                b_w = Buf()
                if not swa:
                    gq = sbt(ph, [128, 512]); gk = sbt(ph, [128, 256])
                    for h in range(4):
                        S.dma(S.sp, gq[:, h * 128:(h + 1) * 128], gqa_qg[l].partition_broadcast(128), writes=[b_w])
                    for h in range(2):
                        S.dma(S.sp, gk[:, h * 128:(h + 1) * 128], gqa_kg[l].partition_broadcast(128), writes=[b_w])
                else:
                    snk = sbt(ph, [128, 8])
                    S.dma(S.sp, snk[:, 0:4], swa_sink[l].partition_broadcast(128), writes=[b_w])
                    V(lambda: nc.vector.tensor_scalar(out=snk[:, 4:8], in0=snk[:, 0:4], scalar1=-1.0, scalar2=None, op0=ALU.mult), [b_w], [b_w])
                KT = sbt(ph, [128, 2, NT], BF16); Vv = sbt(ph, [128, NTILE, 2, 128], BF16); b_kv = Buf()
                pa = [sbt(ph, [128, 512]) for _ in range(2)]; b_pa = [Buf(), Buf()]
                sq = sbt(ph, [128, 512]); b_t = Buf()
                col = sbt(ph, [128, 16])
                xf = sbt(ph, [128, 512]); xb = sbt(ph, [128, 512], BF16)
                rc = sbt(ph, [128, 512]); rs = sbt(ph, [128, 512]); b_r = Buf()
                t1 = sbt(ph, [128, 512]); t2 = sbt(ph, [128, 512])
                QT = sbt(ph, [128, 4, 128], BF16); b_q = Buf()
                yt = [sbt(ph, [128, 512], BF16) for _ in range(2)]; b_yt = [Buf(), Buf()]
                A_ = attn_scratch(ph)
                if swa:
                    b_w = b_w

                def prep(src_ap, H, gain, t, b_src):
                    W_ = H * 128
                    cur = src_ap
                    if not swa:
                        V(lambda: nc.vector.tensor_tensor(out=sq[:, 0:W_], in0=src_ap, in1=src_ap, op=ALU.mult), [b_src], [b_t])
                        V(lambda: nc.vector.reduce_sum(out=col[:, 0:H], in_=sq[:, 0:W_].rearrange("p (h c) -> p h c", c=128), axis=AX.X), [b_t], [b_t])
                        rstd_col(col[:, 8:8 + H], col[:, 0:H], 1.0 / 128, EPS, b_t)
                        V(lambda: nc.vector.tensor_tensor(out=xf[:, 0:W_].rearrange("p (h c) -> p h c", c=128), in0=src_ap.rearrange("p (h c) -> p h c", c=128),
                                                          in1=col[:, 8:8 + H].unsqueeze(2).to_broadcast([128, H, 128]), op=ALU.mult), [b_src, b_t], [b_t])
                        V(lambda: nc.vector.tensor_tensor(out=xf[:, 0:W_], in0=xf[:, 0:W_], in1=gain[:, 0:W_], op=ALU.mult), [b_t, b_w], [b_t])
                        cur = xf[:, 0:W_]
                    r3 = lambda a: a[:, 0:W_].rearrange("p (h c) -> p h c", c=128)
                    if t >= 2:
                        rope(r3(xb), cur.rearrange("p (h c) -> p h c", c=128), r3(rc), r3(rs), r3(t1), r3(t2), 32, [b_src, b_t, b_r], [b_t])
                    else:
                        V(lambda: nc.vector.tensor_copy(out=xb[:, 0:W_], in_=cur), [b_src, b_t], [b_t])

                for t in range(NTILE):
                    i = t % 2
                    S.dma(S.sp, pa[i][:], P_d[b, t * 128:(t + 1) * 128, ok:ok + 512], reads=[b_P[b][t]], writes=[b_pa[i]])
                    if t >= 2:
                        load_rope(rc, rc128, t, 256, b_r); load_rope(rs, rs128, t, 256, b_r)
                    prep(pa[i][:, 0:256], 2, None if swa else gk, t, b_pa[i])
                    transpose_group(KT[:, :, t * 128:(t + 1) * 128], [xb[:, h * 128:(h + 1) * 128] for h in range(2)], 7, b_t, b_kv, evac="act")
                    V(lambda: nc.vector.tensor_copy(out=Vv[:, t, :, :], in_=pa[i][:, 256:512].rearrange("p (h c) -> p h c", c=128)), [b_pa[i]], [b_kv])
                for t in qtiles(l):
                    i = t % 2
                    S.dma(S.sp, pa[i][:], P_d[b, t * 128:(t + 1) * 128, oq:oq + 512], reads=[b_P[b][t]], writes=[b_pa[i]])
                    if t >= 2:
                        load_rope(rc, rc128, t, 512, b_r); load_rope(rs, rs128, t, 512, b_r)
                    prep(pa[i][:, 0:512], 4, None if swa else gq, t, b_pa[i])
                    transpose_group(QT[:], [xb[:, h * 128:(h + 1) * 128] for h in range(4)], 5, b_t, b_q, evac="act")
                    for h in range(4):
                        kvh = h // 2
                        if swa and t >= 2:
                            n_ = t - 2
                            kl = [(0, None), (1, None)]
                            if n_ >= 1:
                                kl.append((t - 1, swab[:, 0:128]))
                            kl.append((t, None))
                            if n_ <= 14:
                                kl.append((t + 1, swab[:, 128:256]))
                        else:
                            kl = [(kc, None) for kc in range(2 if t < 2 else NTILE)]
                        chunks = [([KT[:, kvh, kc * 128:(kc + 1) * 128]], Vv[:, kc, kvh, :], m) for kc, m in kl]
                        attn_core(A_, [QT[:, h, :]], chunks, 128 ** -0.5, yt[i][:, h * 128:(h + 1) * 128], b_q, b_kv, b_yt[i],
                                  sink=(snk[:, h:h + 1], snk[:, 4 + h:5 + h]) if swa else None)
                    S.dma(S.sp, Y_d[b, t * 128:(t + 1) * 128, ycol:ycol + 512], yt[i][:], reads=[b_yt[i]], writes=[b_Y[b][t]])
                S.barrier()

        def gdn_prep(b, l):
            with ExitStack() as ph:
                cwb2 = sbt(ph, [128, 5 * 1536]); b_w = Buf()
                S.dma(S.sp, cwb2[:], gdn_cw[l].rearrange("j c -> (j c)").partition_broadcast(128), writes=[b_w])
                cwb = cwb2[:].rearrange("p (j c) -> p j c", c=1536)
                gc = sbt(ph, [128, 16])
                S.dma(S.sp, gc[:, 0:8], gdn_alog[l].partition_broadcast(128), writes=[b_w])
                S.dma(S.sp, gc[:, 8:16], gdn_dtb[l].partition_broadcast(128), writes=[b_w])
                A(lambda: nc.scalar.activation(out=gc[:, 0:8], in_=gc[:, 0:8], func=AF.Exp), [b_w], [b_w])
                V(lambda: nc.vector.tensor_scalar(out=gc[:, 0:8], in0=gc[:, 0:8], scalar1=-1.0, scalar2=None, op0=ALU.mult), [b_w], [b_w])
                xs = [[sbt(ph, [128, 1536]) for _ in range(5)] for _ in range(2)]; b_xs = [[Buf() for _ in range(5)] for _ in range(2)]
                ab = [sbt(ph, [128, 16]) for _ in range(2)]; b_ab = [Buf(), Buf()]
                acc = sbt(ph, [128, 1536]); tm = sbt(ph, [128, 1536]); b_acc = Buf(); b_tm = Buf()
                col = sbt(ph, [128, 16]); b_col = Buf()
                tok = [sbt(ph, [128, 1040]) for _ in range(2)]; b_tok = [Buf(), Buf()]
                qkn = sbt(ph, [128, 1024]); b_qkn = Buf()
                ft = [sbt(ph, [128, 8, 128]) for _ in range(2)]; b_ft = [Buf(), Buf()]
                for t in range(NTILE):
                    i = t % 2
                    lo, hi = (0, 256) if t < 2 else (256, NT)
                    for j in range(5):
                        o = j - 2
                        r0 = t * 128 + o
                        r1 = r0 + 128
                        v0, v1 = max(r0, lo), min(r1, hi)
                        if v0 != r0 or v1 != r1:
                            G(lambda: nc.gpsimd.memset(xs[i][j][:], 0.0), [], [b_xs[i][j]])
                        t0_, t1_ = max(0, (v0 - 2) // 128), min(NTILE - 1, (v1 + 1) // 128)
                        S.dma(S.sp if j % 2 else S.act, xs[i][j][v0 - r0:v1 - r0, :], P_d[b, v0:v1, O_GQ:O_GQ + 1536],
                              reads=[b_P[b][tt] for tt in range(max(0, t - 1), min(NTILE, t + 2))], writes=[b_xs[i][j]])
                    S.dma(S.sp, ab[i][:], P_d[b, t * 128:(t + 1) * 128, O_GA:O_GA + 16], reads=[b_P[b][t]], writes=[b_ab[i]])
                    V(lambda: nc.vector.tensor_tensor(out=acc[:], in0=xs[i][2][:], in1=cwb[:, 2, :], op=ALU.mult), [b_xs[i][2], b_w], [b_acc])
                    for j in (0, 1, 3, 4):
                        G(lambda: nc.gpsimd.tensor_tensor(out=tm[:], in0=xs[i][j][:], in1=cwb[:, j, :], op=ALU.mult), [b_xs[i][j], b_w], [b_tm])
                        V(lambda: nc.vector.tensor_tensor(out=acc[:], in0=acc[:], in1=tm[:], op=ALU.add), [b_tm, b_acc], [b_acc])
                    A(lambda: nc.scalar.activation(out=acc[:], in_=acc[:], func=AF.Silu), [b_acc], [b_acc])
                    V(lambda: nc.vector.tensor_tensor(out=tm[:, 0:1024], in0=acc[:, 0:1024], in1=acc[:, 0:1024], op=ALU.mult), [b_acc], [b_tm])
                    V(lambda: nc.vector.reduce_sum(out=col[:, 0:8], in_=tm[:, 0:1024].rearrange("p (h c) -> p h c", c=128), axis=AX.X), [b_tm], [b_col])
                    rstd_col(col[:, 8:16], col[:, 0:8], 1.0, 1e-6, b_col)
                    V(lambda: nc.vector.tensor_scalar(out=col[:, 8:12], in0=col[:, 8:12], scalar1=128 ** -0.5, scalar2=None, op0=ALU.mult), [b_col], [b_col])
                    V(lambda: nc.vector.tensor_tensor(out=qkn[:].rearrange("p (h c) -> p h c", c=128), in0=acc[:, 0:1024].rearrange("p (h c) -> p h c", c=128),
                                                      in1=col[:, 8:16].unsqueeze(2).to_broadcast([128, 8, 128]), op=ALU.mult), [b_acc, b_col], [b_qkn])
                    A(lambda: nc.scalar.copy(out=tok[i][:, 0:512], in_=qkn[:, 512:1024]), [b_qkn], [b_tok[i]])
                    A(lambda: nc.scalar.copy(out=tok[i][:, 512:1024], in_=acc[:, 1024:1536]), [b_acc], [b_tok[i]])
                    V(lambda: nc.vector.tensor_tensor(out=col[:, 0:8], in0=ab[i][:, 0:8], in1=gc[:, 8:16], op=ALU.add), [b_ab[i], b_w, b_col], [b_col])
                    A(lambda: nc.scalar.activation(out=col[:, 0:8], in_=col[:, 0:8], func=AF.Exp), [b_col], [b_col])
                    A(lambda: nc.scalar.activation(out=col[:, 0:8], in_=col[:, 0:8], func=AF.Ln, bias=1.0), [b_col], [b_col])
                    V(lambda: nc.vector.tensor_tensor(out=tok[i][:, 1024:1032], in0=col[:, 0:8], in1=gc[:, 0:8], op=ALU.mult), [b_col, b_w], [b_tok[i]])
                    A(lambda: nc.scalar.activation(out=tok[i][:, 1032:1040], in_=ab[i][:, 8:16], func=AF.Sigmoid), [b_ab[i]], [b_tok[i]])
                    S.dma(S.sp, GTOK_d[b, t * 128:(t + 1) * 128, :], tok[i][:], reads=[b_tok[i]], writes=[b_GTOK[b][t]])
                    for g_ in range(2):
                        transpose_group(ft[i][:, g_ * 4:(g_ + 1) * 4, :], [qkn[:, (g_ * 4 + j) * 128:(g_ * 4 + j + 1) * 128] for j in range(4)],
                                        4 + g_, b_qkn, b_ft[i], dt_bf=False, evac="act" if g_ else "dve")
                    S.dma(S.act, GFT_d[b, :, :, t * 128:(t + 1) * 128].rearrange("g d t -> d g t"), ft[i][:], reads=[b_ft[i]], writes=[b_GFT[b][t]])
                S.barrier()

        def gdn_scan(b, l):
            with ExitStack() as ph:
                orders = [list(range(36)), [3, 2, 1, 0] + list(range(35, 3, -1))]
                st_ = []
                for d in range(2):
                    Z = dict(d=d)
                    Z["S"] = sbt(ph, [128, 4, 128]); Z["b_S"] = Buf()
                    Z["tok"] = [sbt(ph, [64, 1040]) for _ in range(2)]; Z["b_tok"] = [Buf(), Buf()]
                    Z["ft"] = [sbt(ph, [128, 8, 64]) for _ in range(2)]; Z["b_ft"] = [Buf(), Buf()]
                    for nm in ("gcol", "egl", "dl", "bg", "eg"):
                        Z[nm] = sbt(ph, [128, 4])
                    for nm in ("diagG", "diagB", "KK", "KQ", "DN", "DM", "Nm", "Mm", "X", "N2", "M2", "intraT", "tA"):
                        Z[nm] = sbt(ph, [64, 4, 64])
                    for nm in ("vb", "kbg", "kdec", "u", "vnew", "o"):
                        Z[nm] = sbt(ph, [64, 4, 128])
                    Z["wT"] = sbt(ph, [128, 4, 64]); Z["qgT"] = sbt(ph, [128, 4, 64]); Z["eG"] = sbt(ph, [128, 4, 64])
                    Z["b"] = Buf()
                    Z["banks"] = (0, 1, 2, 3) if d == 0 else (4, 5, 6, 7)
                    st_.append(Z)
                    V(lambda: nc.vector.memset(Z["S"][:], 0.0), [], [Z["b_S"]])
                I64 = cst[0:64, C_ID:C_ID + 64]
                def body(d, step):
                    if True:
                        Z = st_[d]
                        c = orders[d][step]
                        i = step % 2
                        bz = Z["b"]
                        B0, B1, B2, B3 = Z["banks"]
                        tk = Z["tok"][i]; ft = Z["ft"][i]
                        S.dma(S.sp, tk[:], GTOK_d[b, c * 64:(c + 1) * 64, :], reads=[b_GTOK[b][c // 2]], writes=[Z["b_tok"][i]])
                        yield
                        S.dma(S.act, ft[:], GFT_d[b, :, :, c * 64:(c + 1) * 64].rearrange("g d t -> d g t"), reads=[b_GFT[b][c // 2]], writes=[Z["b_ft"][i]])
                        yield
                        rd = [Z["b_tok"][i], Z["b_ft"][i], b_cst, bz]
                        k_tok = tk[:, 0:512].rearrange("p (h c) -> p h c", c=128)
                        v_tok = tk[:, 512:1024].rearrange("p (h c) -> p h c", c=128)
                        gs = tk[:, 1024 + 4 * d:1028 + 4 * d]
                        beta = tk[:, 1032 + 4 * d:1036 + 4 * d]
                        qT = ft[:, 0:4, :]; kT = ft[:, 4:8, :]
                        tri = cst[0:64, (C_U64 if d == 0 else C_L64):(C_U64 if d == 0 else C_L64) + 64]
                        mN = cst[0:64, C_MN[d]:C_MN[d] + 256].rearrange("p (h c) -> p h c", c=64)
                        mM = cst[0:64, C_MM[d]:C_MM[d] + 256].rearrange("p (h c) -> p h c", c=64)
                        mS = cst[0:64, C_ST[d]:C_ST[d] + 256].rearrange("p (h c) -> p h c", c=64)
                        bc64 = lambda a: a.unsqueeze(2).to_broadcast([64, 4, 64])
                        bc128 = lambda a: a.unsqueeze(2).to_broadcast([64, 4, 128])
                        p3 = lambda bank, parts=64, w=64: ps(bank, 4 * w, parts).rearrange("p (h c) -> p h c", c=w)
                        def mm1():
                            nc.tensor.matmul(ps(B0, 4, 64), lhsT=tri, rhs=gs, start=True, stop=True)
                            return nc.tensor.matmul(ps(B0, 4, 128, 8), lhsT=ones[0:64, :], rhs=gs, start=True, stop=True)
                        T(mm1, rd, [pb[B0]])
                        yield
                        V(lambda: nc.vector.tensor_copy(out=Z["gcol"][0:64, :], in_=ps(B0, 4, 64)), [pb[B0]], [bz])
                        yield
                        A(lambda: nc.scalar.activation(out=Z["egl"][:], in_=ps(B0, 4, 128, 8), func=AF.Exp), [pb[B0]], [bz])
                        yield
                        V(lambda: nc.vector.tensor_tensor(out=Z["dl"][0:64, :], in0=ps(B0, 4, 64, 8), in1=Z["gcol"][0:64, :], op=ALU.subtract), [pb[B0], bz], [bz])
                        yield
                        A(lambda: nc.scalar.activation(out=Z["dl"][0:64, :], in_=Z["dl"][0:64, :], func=AF.Exp), [bz], [bz])
                        yield
                        A(lambda: nc.scalar.activation(out=Z["eg"][0:64, :], in_=Z["gcol"][0:64, :], func=AF.Exp), [bz], [bz])
                        yield
                        V(lambda: nc.vector.tensor_tensor(out=Z["bg"][0:64, :], in0=Z["eg"][0:64, :], in1=beta, op=ALU.mult), rd, [bz])
                        yield
                        V(lambda: nc.vector.tensor_tensor(out=Z["diagG"][:], in0=I64.unsqueeze(1).to_broadcast([64, 4, 64]), in1=bc64(Z["gcol"][0:64, :]), op=ALU.mult), rd, [bz])
                        yield
                        V(lambda: nc.vector.tensor_tensor(out=Z["diagB"][:], in0=I64.unsqueeze(1).to_broadcast([64, 4, 64]), in1=bc64(beta), op=ALU.mult), rd, [bz])
                        yield

                        def mm2():
                            nc.tensor.matmul(ps(B1, 256, 128), lhsT=ones[0:64, :], rhs=Z["diagG"][:].rearrange("p h c -> p (h c)"), start=True, stop=True)
                            return nc.tensor.matmul(ps(B1, 256, 64, 256), lhsT=ones[0:64, 0:64], rhs=Z["diagB"][:].rearrange("p h c -> p (h c)"), start=True, stop=True)
                        T(mm2, rd, [pb[B1]])
                        yield
                        Grow = ps(B1, 256, 64).rearrange("p (h c) -> p h c", c=64)
                        Brow = ps(B1, 256, 64, 256).rearrange("p (h c) -> p h c", c=64)
                        def mm3():
                            for h in range(4):
                                nc.tensor.matmul(ps(B2, 64, 64, h * 64), lhsT=kT[:, h, :], rhs=kT[:, h, :], start=True, stop=True)
                            for h in range(4):
                                ins = nc.tensor.matmul(ps(B2, 64, 64, 256 + h * 64), lhsT=kT[:, h, :], rhs=qT[:, h, :], start=True, stop=True)
                            return ins
                        T(mm3, rd, [pb[B2]])
                        yield
                        A(lambda: nc.scalar.copy(out=Z["KK"][:], in_=ps(B2, 256, 64).rearrange("p (h c) -> p h c", c=64)), [pb[B2]], [bz])
                        yield
                        A(lambda: nc.scalar.copy(out=Z["KQ"][:], in_=ps(B2, 256, 64, 256).rearrange("p (h c) -> p h c", c=64)), [pb[B2]], [bz])
                        yield
                        V(lambda: nc.vector.tensor_tensor(out=Z["DN"][:], in0=bc64(Z["gcol"][0:64, :]), in1=Grow, op=ALU.subtract), [pb[B1], bz], [bz])
                        yield
                        V(lambda: nc.vector.tensor_tensor(out=Z["DN"][:], in0=Z["DN"][:], in1=mN, op=ALU.add), rd, [bz])
                        yield
                        A(lambda: nc.scalar.activation(out=Z["DN"][:], in_=Z["DN"][:], func=AF.Exp), [bz], [bz])
                        yield
                        V(lambda: nc.vector.scalar_tensor_tensor(out=Z["tA"][:], in0=Z["DN"][:], scalar=-1.0, in1=Z["KK"][:], op0=ALU.mult, op1=ALU.mult), [bz], [bz])
                        yield
                        V(lambda: nc.vector.tensor_tensor(out=Z["Nm"][:], in0=Z["tA"][:], in1=bc64(beta), op=ALU.mult), rd, [bz])
                        yield
                        V(lambda: nc.vector.tensor_tensor(out=Z["DM"][:], in0=Grow, in1=bc64(Z["gcol"][0:64, :]), op=ALU.subtract), [pb[B1], bz], [bz])
                        yield
                        V(lambda: nc.vector.tensor_tensor(out=Z["DM"][:], in0=Z["DM"][:], in1=mM, op=ALU.add), rd, [bz])
                        yield
                        A(lambda: nc.scalar.activation(out=Z["DM"][:], in_=Z["DM"][:], func=AF.Exp), [bz], [bz])
                        yield
                        V(lambda: nc.vector.tensor_tensor(out=Z["intraT"][:], in0=Z["DM"][:], in1=Z["KQ"][:], op=ALU.mult), [bz], [bz])
                        yield
                        V(lambda: nc.vector.tensor_tensor(out=Z["tA"][:], in0=Z["DM"][:], in1=mS, op=ALU.mult), rd, [bz])
                        yield
                        V(lambda: nc.vector.scalar_tensor_tensor(out=Z["tA"][:], in0=Z["tA"][:], scalar=-1.0, in1=Z["KK"][:], op0=ALU.mult, op1=ALU.mult), [bz], [bz])
                        yield
                        V(lambda: nc.vector.tensor_tensor(out=Z["Mm"][:], in0=Z["tA"][:], in1=Brow, op=ALU.mult), [pb[B1], bz], [bz])
                        yield
                        V(lambda: nc.vector.tensor_tensor(out=Z["X"][:], in0=Z["Mm"][:], in1=I64.unsqueeze(1).to_broadcast([64, 4, 64]), op=ALU.add), rd, [bz])
                        yield
                        A(lambda: nc.scalar.activation(out=Z["eG"][:], in_=ps(B1, 256, 128).rearrange("p (h c) -> p h c", c=64), func=AF.Exp), [pb[B1]], [bz])
                        yield
                        V(lambda: nc.vector.tensor_tensor(out=Z["qgT"][:], in0=qT, in1=Z["eG"][:], op=ALU.mult), rd, [bz])
                        yield
                        Ncur, Mcur = Z["Nm"], Z["Mm"]
                        Nn, Mn = Z["N2"], Z["M2"]
                        for s_ in range(5):
                            last = (s_ == 4)

                            def sqr():
                                for h in range(4):
                                    ins = nc.tensor.matmul(ps(B2, 64, 64, h * 64), lhsT=Mcur[:, h, :], rhs=Ncur[:, h, :], start=True, stop=True)
                                if not last:
                                    for h in range(4):
                                        ins = nc.tensor.matmul(ps(B2, 64, 64, 256 + h * 64), lhsT=Ncur[:, h, :], rhs=Mcur[:, h, :], start=True, stop=True)
                                return ins
                            T(sqr, [bz], [pb[B2]])
                            yield
                            A(lambda: nc.scalar.copy(out=Nn[:], in_=p3(B2)), [pb[B2]], [bz])
                            yield
                            if not last:
                                V(lambda: nc.vector.tensor_copy(out=Mn[:], in_=ps(B2, 256, 64, 256).rearrange("p (h c) -> p h c", c=64)), [pb[B2]], [bz])

                            def xup():
                                for h in range(4):
                                    ins = nc.tensor.matmul(ps(B3, 64, 64, h * 64), lhsT=Nn[:, h, :], rhs=Z["X"][:, h, :], start=True, stop=True)
                                return ins
                            T(xup, [bz], [pb[B3]])
                            yield
                            V(lambda: nc.vector.tensor_tensor(out=Z["X"][:], in0=Z["X"][:], in1=p3(B3), op=ALU.add), [pb[B3], bz], [bz])
                            yield
                            Ncur, Nn = Nn, Ncur
                            Mcur, Mn = Mn, Mcur
                        V(lambda: nc.vector.tensor_tensor(out=Z["vb"][:], in0=v_tok, in1=bc128(beta), op=ALU.mult), rd, [bz])
                        yield
                        G(lambda: nc.gpsimd.tensor_tensor(out=Z["kbg"][:], in0=k_tok, in1=bc128(Z["bg"][0:64, :]), op=ALU.mult), rd, [bz])
                        yield
                        G(lambda: nc.gpsimd.tensor_tensor(out=Z["kdec"][:], in0=k_tok, in1=bc128(Z["dl"][0:64, :]), op=ALU.mult), rd, [bz])
                        yield

                        def mm7():
                            for h in range(4):
                                nc.tensor.matmul(ps(B2, 128, 64, h * 128), lhsT=Z["X"][:, h, :], rhs=Z["vb"][:, h, :], start=True, stop=True)
                            for h in range(4):
                                ins = nc.tensor.matmul(ps(B3, 64, 128, h * 64), lhsT=Z["kbg"][:, h, :], rhs=Z["X"][:, h, :], start=True, stop=True)
                            return ins
                        T(mm7, [bz], [pb[B2], pb[B3]])
                        yield
                        A(lambda: nc.scalar.copy(out=Z["u"][:], in_=p3(B2, 64, 128)), [pb[B2]], [bz])
                        yield
                        V(lambda: nc.vector.tensor_copy(out=Z["wT"][:], in_=p3(B3, 128, 64)), [pb[B3]], [bz])
                        yield
                        Sx = Z["S"]

                        def mm8():
                            for h in range(4):
                                ins = nc.tensor.matmul(ps(B0, 128, 64, h * 128), lhsT=Z["wT"][:, h, :], rhs=Sx[:, h, :], start=True, stop=True)
                            return ins
                        T(mm8, [bz, Z["b_S"]], [pb[B0]])
                        yield
                        V(lambda: nc.vector.tensor_tensor(out=Z["vnew"][:], in0=Z["u"][:], in1=p3(B0, 64, 128), op=ALU.subtract), [pb[B0], bz], [bz])
                        yield

                        def mm9():
                            for h in range(4):
                                nc.tensor.matmul(ps(B2, 128, 64, h * 128), lhsT=Z["qgT"][:, h, :], rhs=Sx[:, h, :], start=True, stop=False)
                                nc.tensor.matmul(ps(B2, 128, 64, h * 128), lhsT=Z["intraT"][:, h, :], rhs=Z["vnew"][:, h, :], start=False, stop=True)
                            for h in range(4):
                                ins = nc.tensor.matmul(ps(B3, 128, 128, h * 128), lhsT=Z["kdec"][:, h, :], rhs=Z["vnew"][:, h, :], start=True, stop=True)
                            return ins
                        T(mm9, [bz, Z["b_S"]], [pb[B2], pb[B3]])
                        yield
                        A(lambda: nc.scalar.copy(out=Z["o"][:], in_=p3(B2, 64, 128)), [pb[B2]], [bz])
                        yield
                        S.dma(S.sp, GO_d[b, d, c * 64:(c + 1) * 64, :], Z["o"][:].rearrange("p h c -> p (h c)"), reads=[bz], writes=[b_GO[b][d][c // 2]])
                        yield
                        V(lambda: nc.vector.tensor_tensor(out=Sx[:], in0=Sx[:], in1=Z["egl"][:].unsqueeze(2).to_broadcast([128, 4, 128]), op=ALU.mult), [bz, Z["b_S"]], [Z["b_S"]])
                        yield
                        V(lambda: nc.vector.tensor_tensor(out=Sx[:], in0=Sx[:], in1=p3(B3, 128, 128), op=ALU.add), [pb[B3], Z["b_S"]], [Z["b_S"]])
                        yield
                for step in range(36):
                    alive = [body(0, step), body(1, step)]
                    while alive:
                        for g_ in list(alive):
                            try:
                                next(g_)
                            except StopIteration:
                                alive.remove(g_)
                S.barrier()

        def gdn_final(b, l):
            with ExitStack() as ph:
                gn = sbt(ph, [128, 512]); b_w = Buf()
                for h in range(4):
                    S.dma(S.sp, gn[:, h * 128:(h + 1) * 128], gdn_ng[l].partition_broadcast(128), writes=[b_w])
                of = [sbt(ph, [128, 512]) for _ in range(2)]; ob = [sbt(ph, [128, 512]) for _ in range(2)]; zz = [sbt(ph, [128, 512]) for _ in range(2)]
                b_in = [Buf(), Buf()]
                sq = sbt(ph, [128, 512]); col = sbt(ph, [128, 8]); b_t = Buf()
                yt = [sbt(ph, [128, 512], BF16) for _ in range(2)]; b_yt = [Buf(), Buf()]
                for t in qtiles(l):
                    i = t % 2
                    S.dma(S.sp, of[i][:], GO_d[b, 0, t * 128:(t + 1) * 128, :], reads=[b_GO[b][0][t]], writes=[b_in[i]])
                    S.dma(S.act, ob[i][:], GO_d[b, 1, t * 128:(t + 1) * 128, :], reads=[b_GO[b][1][t]], writes=[b_in[i]])
                    S.dma(S.sp, zz[i][:], P_d[b, t * 128:(t + 1) * 128, O_GZ:O_GZ + 512], reads=[b_P[b][t]], writes=[b_in[i]])
                    V(lambda: nc.vector.tensor_tensor(out=of[i][:], in0=of[i][:], in1=ob[i][:], op=ALU.add), [b_in[i]], [b_in[i]])
                    V(lambda: nc.vector.tensor_tensor(out=sq[:], in0=of[i][:], in1=of[i][:], op=ALU.mult), [b_in[i]], [b_t])
                    V(lambda: nc.vector.reduce_sum(out=col[:, 0:4], in_=sq[:].rearrange("p (h c) -> p h c", c=128), axis=AX.X), [b_t], [b_t])
                    rstd_col(col[:, 4:8], col[:, 0:4], 1.0 / 128, EPS, b_t)
                    V(lambda: nc.vector.tensor_tensor(out=sq[:].rearrange("p (h c) -> p h c", c=128), in0=of[i][:].rearrange("p (h c) -> p h c", c=128),
                                                      in1=col[:, 4:8].unsqueeze(2).to_broadcast([128, 4, 128]), op=ALU.mult), [b_in[i], b_t], [b_t])
                    V(lambda: nc.vector.tensor_tensor(out=sq[:], in0=sq[:], in1=gn[:], op=ALU.mult), [b_t, b_w], [b_t])
                    A(lambda: nc.scalar.activation(out=zz[i][:], in_=zz[i][:], func=AF.Silu), [b_in[i]], [b_in[i]])
                    V(lambda: nc.vector.tensor_tensor(out=yt[i][:], in0=sq[:], in1=zz[i][:], op=ALU.mult), [b_t, b_in[i]], [b_yt[i]])
                    S.dma(S.sp, Y_d[b, t * 128:(t + 1) * 128, 1024:1536], yt[i][:], reads=[b_yt[i]], writes=[b_Y[b][t]])
                S.barrier()

        def phase_D(b, l):
            moe = (l % 2 == 1)
            with ExitStack() as ph:
                wo = sbt(ph, [128, 16, D], BF16); b_wo = Buf()
                for c4 in range(4):
                    S.dma(S.pool, wo[:, :, c4 * 512:(c4 + 1) * 512], w_out[l, :, c4 * 512:(c4 + 1) * 512].rearrange("(j p) n -> p j n", p=128), writes=[b_wo])
                Amul = sbt(ph, [128, D]); Badd = sbt(ph, [128, D]); Gmsa = sbt(ph, [128, D]); b_c = Buf()
                tmp = sbt(ph, [128, D]); b_tmp = Buf()
                yb = [sbt(ph, [128, D], BF16) for _ in range(2)]; b_yb = [Buf(), Buf()]
                yT = sbt(ph, [128, 16, 128], BF16); b_yT = Buf()
                xt = [sbt(ph, [128, D]) for _ in range(2)]; b_xt = [Buf(), Buf()]
                xm = sbt(ph, [128, D]); b_xm = Buf()
                hf = sbt(ph, [128, D]); b_hf = Buf()
                hb = sbt(ph, [128, D], BF16); b_hb = Buf()
                hT_ = [sbt(ph, [128, 16, 128], BF16) for _ in range(2)]; b_hT_ = [Buf(), Buf()]
                col = sbt(ph, [128, 16]); b_col = Buf()
                if moe:
                    rt = sbt(ph, [128, 16, 8]); b_rt = Buf()
                    S.dma(S.sp, rt[:], moe_router[l // 2].rearrange("(j p) e -> p j e", p=128), writes=[b_rt])
                    h32T = sbt(ph, [128, 16, 128]); b_h32T = Buf()
                    lg = sbt(ph, [128, 32]); b_lg = Buf()
                    gt = [sbt(ph, [128, 8]) for _ in range(2)]; b_gt = [Buf(), Buf()]
                for t in qtiles(l):
                    i = t % 2
                    if t == 0 or t == 2:
                        build_mod_consts((Amul, Badd, tmp), l, b, 0 if t == 0 else 1, norm2_g[l], 3, 4, b_c)
                        S.dma(S.sp, Gmsa[:], mod_row(l, b, 0 if t == 0 else 1, 2), reads=[b_mods], writes=[b_c])
                    S.dma(S.sp, yb[i][:], Y_d[b, t * 128:(t + 1) * 128, :], reads=[b_Y[b][t]], writes=[b_yb[i]])
                    S.dma(S.act, xt[i][:], xsrc(l, b, t), reads=xsrc_buf(l, b, t), writes=[b_xt[i]])
                    for g_ in range(2):
                        transpose_group(yT[:, g_ * 8:(g_ + 1) * 8, :], [yb[i][:, (g_ * 8 + j) * 128:(g_ * 8 + j + 1) * 128] for j in range(8)],
                                        4 + g_, b_yb[i], b_yT, evac="act" if g_ else "dve")

                    def mm():
                        for c4 in range(4):
                            for k in range(16):
                                ins = nc.tensor.matmul(ps(c4, 512), lhsT=yT[:, k, :], rhs=wo[:, k, c4 * 512:(c4 + 1) * 512], start=(k == 0), stop=(k == 15))
                        return ins
                    T(mm, [b_yT, b_wo], pb[0:4])
                    V(lambda: nc.vector.tensor_tensor(out=xm[:], in0=psall[:, 0:D], in1=Gmsa[:], op=ALU.mult), pb[0:4] + [b_c], [b_xm])
                    G(lambda: nc.gpsimd.tensor_tensor(out=xm[:], in0=xm[:], in1=xt[i][:], op=ALU.add), [b_xm, b_xt[i]], [b_xm])
                    S.dma(S.sp, resid_d[b, t * 128:(t + 1) * 128, :], xm[:], reads=[b_xm] + xsrc_buf(l, b, t), writes=[b_resid[b][t]])
                    A(lambda: nc.scalar.activation(out=hf[:], in_=xm[:], func=AF.Square, accum_out=col[:, 0:1]), [b_xm], [b_hf, b_col])
                    rstd_col(col[:, 1:2], col[:, 0:1], 1.0 / D, EPS, b_col)
                    V(lambda: nc.vector.scalar_tensor_tensor(out=hf[:], in0=xm[:], scalar=col[:, 1:2], in1=Amul[:], op0=ALU.mult, op1=ALU.mult), [b_xm, b_col, b_c], [b_hf])
                    V(lambda: nc.vector.tensor_tensor(out=hf[:], in0=hf[:], in1=Badd[:], op=ALU.add), [b_hf, b_c], [b_hf])
                    G(lambda: nc.gpsimd.tensor_copy(out=hb[:], in_=hf[:]), [b_hf], [b_hb])
                    for g_ in range(2):
                        transpose_group(hT_[i][:, g_ * 8:(g_ + 1) * 8, :], [hb[:, (g_ * 8 + j) * 128:(g_ * 8 + j + 1) * 128] for j in range(8)],
                                        4 + g_, b_hb, b_hT_[i], evac="act" if g_ else "dve")
                    S.dma(S.sp, H2T_d[b, :, :, t * 128:(t + 1) * 128], hT_[i][:], reads=[b_hT_[i]], writes=[b_H2T[b][t]])
                    if moe:
                        for g_ in range(4):
                            transpose_group(h32T[:, g_ * 4:(g_ + 1) * 4, :], [hf[:, (g_ * 4 + j) * 128:(g_ * 4 + j + 1) * 128] for j in range(4)],
                                            6 + g_ % 2, b_hf, b_h32T, dt_bf=False, evac="act" if g_ % 2 else "dve")

                        def mmr():
                            for k in range(16):
                                ins = nc.tensor.matmul(ps(6, 8), lhsT=h32T[:, k, :], rhs=rt[:, k, :], start=(k == 0), stop=(k == 15))
                            return ins
                        T(mmr, [b_h32T, b_rt], [pb[6]])
                        V(lambda: nc.vector.tensor_copy(out=lg[:, 0:8], in_=ps(6, 8)), [pb[6]], [b_lg])
                        V(lambda: nc.vector.reduce_max(out=col[:, 4:5], in_=lg[:, 0:8], axis=AX.X), [b_lg], [b_col])
                        V(lambda: nc.vector.tensor_scalar(out=lg[:, 8:16], in0=lg[:, 0:8], scalar1=col[:, 4:5], scalar2=-1e30, op0=ALU.is_ge, op1=ALU.mult), [b_lg, b_col], [b_lg])
                        V(lambda: nc.vector.tensor_tensor(out=lg[:, 8:16], in0=lg[:, 8:16], in1=lg[:, 0:8], op=ALU.add), [b_lg], [b_lg])
                        V(lambda: nc.vector.reduce_max(out=col[:, 5:6], in_=lg[:, 8:16], axis=AX.X), [b_lg], [b_col])
                        V(lambda: nc.vector.tensor_scalar(out=col[:, 6:7], in0=col[:, 4:5], scalar1=-1.0, scalar2=None, op0=ALU.mult), [b_col], [b_col])
                        A(lambda: nc.scalar.activation(out=lg[:, 16:24], in_=lg[:, 0:8], func=AF.Exp, bias=col[:, 6:7], scale=1.0), [b_lg, b_col], [b_lg])
                        V(lambda: nc.vector.tensor_scalar(out=lg[:, 24:32], in0=lg[:, 0:8], scalar1=col[:, 5:6], scalar2=None, op0=ALU.is_ge), [b_lg, b_col], [b_lg])
                        V(lambda: nc.vector.tensor_tensor(out=lg[:, 16:24], in0=lg[:, 16:24], in1=lg[:, 24:32], op=ALU.mult), [b_lg], [b_lg])
                        V(lambda: nc.vector.reduce_sum(out=col[:, 7:8], in_=lg[:, 16:24], axis=AX.X), [b_lg], [b_col])
                        V(lambda: nc.vector.reciprocal(out=col[:, 8:9], in_=col[:, 7:8]), [b_col], [b_col])
                        V(lambda: nc.vector.tensor_scalar(out=gt[i][:], in0=lg[:, 16:24], scalar1=col[:, 8:9], scalar2=None, op0=ALU.mult), [b_lg, b_col], [b_gt[i]])
                        S.dma(S.sp, GATES_d[b, t * 128:(t + 1) * 128, :], gt[i][:], reads=[b_gt[i]], writes=[b_GATES[b][t]])
                S.barrier()

        def phase_E(b, l, tiles):
            moe = (l % 2 == 1)
            last = (l == L - 1)
            nt = len(tiles)
            t0 = tiles[0]
            ntok = nt * 128
            FB = 256
            if ntok % 512 == 0:
                tblocks = [(o, 512) for o in range(0, ntok, 512)]
            else:
                tblocks = [(o, 384) for o in range(0, ntok, 384)]
            with ExitStack() as ph:
                h2T = sbt(ph, [128, 16, ntok], BF16); b_h2T = Buf()
                S.dma(S.sp, h2T[:], H2T_d[b, :, :, t0 * 128:t0 * 128 + ntok], reads=[b_H2T[b][t] for t in tiles], writes=[b_h2T])
                acc = sbt(ph, [128, nt, D]); b_acc = [Buf() for _ in range(nt)]
                if moe:
                    gts = sbt(ph, [128, nt, 8]); b_g = Buf()
                    S.dma(S.sp, gts[:], GATES_d[b, t0 * 128:t0 * 128 + ntok, :].rearrange("(n p) e -> p n e", p=128), reads=[b_GATES[b][t] for t in tiles], writes=[b_g])
                ph2 = ExitStack()
                w1b = [sbt(ph2, [128, 16, FB], BF16) for _ in range(2)]; w3b = [sbt(ph2, [128, 16, FB], BF16) for _ in range(2)]
                w2b = [sbt(ph2, [128, FB // 128, D], BF16) for _ in range(2)]; b_wb = [Buf(), Buf()]
                sg = [sbt(ph2, [128, 512]) for _ in range(2)]; b_sg = [Buf(), Buf()]
                aT = [sbt(ph2, [128, FB // 128, ntok], BF16) for _ in range(2)]; b_aT = [Buf(), Buf()]
                if moe:
                    experts = [(moe_w1[l // 2, e], moe_w3[l // 2, e], moe_w2[l // 2, e], EXD, e) for e in range(8)]
                else:
                    experts = [(ffn_w1[l // 2], ffn_w3[l // 2], ffn_w2[l // 2], FFN, None)]
                it = 0
                gu = 0
                ob = 0
                for (W1, W3, W2, F_, e) in experts:
                    for fb in range(F_ // FB):
                        f0 = fb * FB
                        wi = it % 2
                        S.dma(S.pool, w1b[wi][:], W1[:, f0:f0 + FB].rearrange("(j p) n -> p j n", p=128), writes=[b_wb[wi]])
                        S.dma(S.pool, w3b[wi][:], W3[:, f0:f0 + FB].rearrange("(j p) n -> p j n", p=128), writes=[b_wb[wi]])
                        S.dma(S.pool, w2b[wi][:], W2[f0:f0 + FB, :].rearrange("(c p) n -> p c n", p=128), writes=[b_wb[wi]])
                        for (o_, w_) in tblocks:
                            for fc in range(FB // 128):
                                bg_, bu_ = (0, 1) if gu % 2 == 0 else (2, 3)
                                si = gu % 2
                                gu += 1

                                def mmgu():
                                    for k in range(16):
                                        nc.tensor.matmul(ps(bg_, w_), lhsT=w1b[wi][:, k, fc * 128:(fc + 1) * 128], rhs=h2T[:, k, o_:o_ + w_], start=(k == 0), stop=(k == 15))
                                    for k in range(16):
                                        ins = nc.tensor.matmul(ps(bu_, w_), lhsT=w3b[wi][:, k, fc * 128:(fc + 1) * 128], rhs=h2T[:, k, o_:o_ + w_], start=(k == 0), stop=(k == 15))
                                    return ins
                                T(mmgu, [b_wb[wi], b_h2T], [pb[bg_], pb[bu_]])
                                A(lambda: nc.scalar.activation(out=sg[si][:, 0:w_], in_=ps(bg_, w_), func=AF.Silu), [pb[bg_]], [b_sg[si]])
                                V(lambda: nc.vector.tensor_tensor(out=aT[wi][:, fc, o_:o_ + w_], in0=sg[si][:, 0:w_], in1=ps(bu_, w_), op=ALU.mult), [pb[bu_], b_sg[si]], [b_aT[wi]])
                        first = (it == 0)
                        for ti in range(nt):
                            for dblk in range(4):
                                bo = 4 + ob % 4
                                ob += 1

                                def mm2_():
                                    for fc in range(FB // 128):
                                        ins = nc.tensor.matmul(ps(bo, 512), lhsT=aT[wi][:, fc, ti * 128:(ti + 1) * 128], rhs=w2b[wi][:, fc, dblk * 512:(dblk + 1) * 512],
                                                               start=(fc == 0), stop=(fc == FB // 128 - 1))
                                    return ins
                                T(mm2_, [b_aT[wi], b_wb[wi]], [pb[bo]])
                                dst = acc[:, ti, dblk * 512:(dblk + 1) * 512]
                                if moe:
                                    gcol_ = gts[:, ti, e:e + 1]
                                    if first:
                                        V(lambda: nc.vector.tensor_scalar(out=dst, in0=ps(bo, 512), scalar1=gcol_, scalar2=None, op0=ALU.mult), [pb[bo], b_g], [b_acc[ti]])
                                    else:
                                        V(lambda: nc.vector.scalar_tensor_tensor(out=dst, in0=ps(bo, 512), scalar=gcol_, in1=dst, op0=ALU.mult, op1=ALU.add), [pb[bo], b_g, b_acc[ti]], [b_acc[ti]])
                                else:
                                    if first:
                                        V(lambda: nc.vector.tensor_copy(out=dst, in_=ps(bo, 512)), [pb[bo]], [b_acc[ti]])
                                    else:
                                        V(lambda: nc.vector.tensor_tensor(out=dst, in0=dst, in1=ps(bo, 512), op=ALU.add), [pb[bo], b_acc[ti]], [b_acc[ti]])
                        it += 1
                S.barrier()
                ph2.close()
                G5 = sbt(ph, [128, D]); b_c = Buf()
                xt = [sbt(ph, [128, D]) for _ in range(2)]; b_xt = [Buf(), Buf()]
                if last:
                    fg = sbt(ph, [128, D]); col = sbt(ph, [128, 4]); b_col = Buf()
                    S.dma(S.sp, fg[:], final_g.partition_broadcast(128), writes=[b_c])
                cur_stream = None
                for ti, t in enumerate(tiles):
                    i = ti % 2
                    stream = 0 if t < 2 else 1
                    if stream != cur_stream:
                        S.dma(S.sp, G5[:], mod_row(l, b, stream, 5), reads=[b_mods], writes=[b_c])
                        cur_stream = stream
                    S.dma(S.sp, xt[i][:], resid_d[b, t * 128:(t + 1) * 128, :], reads=[b_resid[b][t]], writes=[b_xt[i]])
                    V(lambda: nc.vector.tensor_tensor(out=acc[:, ti, :], in0=acc[:, ti, :], in1=G5[:], op=ALU.mult), [b_acc[ti], b_c], [b_acc[ti]])
                    G(lambda: nc.gpsimd.tensor_tensor(out=xt[i][:], in0=xt[i][:], in1=acc[:, ti, :], op=ALU.add), [b_acc[ti], b_xt[i]], [b_xt[i]])
                    if not last:
                        S.dma(S.sp, resid_d[b, t * 128:(t + 1) * 128, :], xt[i][:], reads=[b_xt[i]], writes=[b_resid[b][t]])
                    else:
                        A(lambda: nc.scalar.activation(out=acc[:, ti, :], in_=xt[i][:], func=AF.Square, accum_out=col[:, 0:1]), [b_xt[i]], [b_acc[ti], b_col])
                        rstd_col(col[:, 1:2], col[:, 0:1], 1.0 / D, EPS, b_col)
                        V(lambda: nc.vector.scalar_tensor_tensor(out=xt[i][:], in0=xt[i][:], scalar=col[:, 1:2], in1=fg[:], op0=ALU.mult, op1=ALU.mult), [b_col, b_c, b_xt[i]], [b_xt[i]])
                        S.dma(S.sp, out_d[b, (t - 2) * 128:(t - 1) * 128, :], xt[i][:], reads=[b_xt[i]], writes=[b_out])
                S.barrier()

        if start is None:
            prologue()
        done = False
        for b in range(n_seq):
            for l in range(L):
                if start is None:
                    with ExitStack() as ph0:
                        hT = sbt(ph0, [128, 16, NT], BF16); b_hT = Buf()
                        phase_A(b, l, hT, b_hT)
                        phase_B(b, l, hT, b_hT)
                        S.barrier()
                if stop == "B":
                    done = True; break
                if only in (None, "mla"):
                    mixer_mla(b, l)
                if stop == "mla":
                    done = True; break
                if only in (None, "gqa"):
                    mixer_gqa_swa(b, l, False)
                if only in (None, "swa"):
                    mixer_gqa_swa(b, l, True)
                if stop == "attn":
                    done = True; break
                if only in (None, "gdn"):
                    gdn_prep(b, l)
                    gdn_scan(b, l)
                    gdn_final(b, l)
                if stop == "mix":
                    done = True; break
                phase_D(b, l)
                if stop == "D":
                    done = True; break
                tl = list(qtiles(l))
                half = len(tl) // 2
                phase_E(b, l, tl[:half])
                phase_E(b, l, tl[half:])
            if done:
                break
        S.finish()
    return nc


def _host_consts():
    def tables(rot):
        rows = 2048 // 64
        row = np.repeat(np.arange(rows), 64).astype(np.float32)
        colp = np.tile(np.arange(64), rows).astype(np.float32)
        half = rot // 2
        inv = (10000.0 ** (-np.arange(0, half, 2, dtype=np.float32) / half)).astype(np.float32)
        ar = row[:, None] * inv
        ac = colp[:, None] * inv
        ang = np.concatenate([ar, ar, ac, ac], -1).astype(np.float32)
        q = rot // 4
        sign = np.concatenate([-np.ones(q), np.ones(q), -np.ones(q), np.ones(q)]).astype(np.float32)
        return np.cos(ang).astype(np.float32), (np.sin(ang) * sign[None]).astype(np.float32)
    c128, s128 = tables(128)
    c64, s64 = tables(64)
    cst = np.zeros((128, C_W), np.float32)
    i = np.arange(128)[:, None]
    j = np.arange(128)[None, :]
    cst[:, C_ID:C_ID + 128] = (i == j)
    cst[:, C_ONE:C_ONE + 128] = 1.0
    cst[:, C_SU:C_SU + 128] = np.where(j >= i, 0.0, NEG)
    cst[:, C_SL:C_SL + 128] = np.where(j <= i, 0.0, NEG)
    p = np.arange(64)[:, None]
    f = np.arange(64)[None, :]
    cst[0:64, C_U64:C_U64 + 64] = (p <= f)
    cst[0:64, C_L64:C_L64 + 64] = (p >= f)
    for d in range(2):
        vN = (f < p) if d == 0 else (f > p)
        vM = (f >= p) if d == 0 else (f <= p)
        vS = (f > p) if d == 0 else (f < p)
        cst[0:64, C_MN[d]:C_MN[d] + 256] = np.tile(np.where(vN, 0.0, NEG), (1, 4))
        cst[0:64, C_MM[d]:C_MM[d] + 256] = np.tile(np.where(vM, 0.0, NEG), (1, 4))
        cst[0:64, C_ST[d]:C_ST[d] + 256] = np.tile(vS.astype(np.float32), (1, 4))
    return dict(rc128=np.tile(c128, (1, 4)), rs128=np.tile(s128, (1, 4)), rc64=np.tile(c64, (1, 4)), rs64=np.tile(s64, (1, 4)), cst=cst)


_PROG = {}


def kernel(**inputs):
    n_cores = 8
    n_seq = 2
    if "full" not in _PROG:
        _PROG["full"] = build_program(n_seq=n_seq, n_layers=4)
    nc = _PROG["full"]
    consts = _host_consts()
    shared = {k: np.ascontiguousarray(v) for k, v in inputs.items() if k not in ("x", "c", "ctx")}
    shared["gdn_a_log"] = np.ascontiguousarray(inputs["gdn_a_log"]).reshape(4, 8)
    shared["gdn_dt_bias"] = np.ascontiguousarray(inputs["gdn_dt_bias"]).reshape(4, 8)
    shared.update(consts)
    in_maps = []
    for i in range(n_cores):
        m = dict(shared)
        m["x"] = np.ascontiguousarray(inputs["x"][i * n_seq:(i + 1) * n_seq])
        m["c"] = np.ascontiguousarray(inputs["c"][i * n_seq:(i + 1) * n_seq])
        m["ctx"] = np.ascontiguousarray(inputs["ctx"][i * n_seq:(i + 1) * n_seq])
        in_maps.append(m)
    res = run_bass_kernel_spmd(nc, in_maps, core_ids=list(range(n_cores)))
    return np.concatenate([np.asarray(r["out"]) for r in res.results], axis=0).astype(np.float32)
```

```python
import numpy as np
import concourse.bass as bass
import concourse.mybir as mybir
from concourse.bass_utils import run_bass_kernel_spmd
from contextlib import ExitStack

F32 = mybir.dt.float32
BF16 = mybir.dt.bfloat16
ALU = mybir.AluOpType
AF = mybir.ActivationFunctionType
AX = mybir.AxisListType

D = 2048
NT = 2304
NTILE = 18
DIN = 4816
FFN = 5632
EXD = 4096
NEG = -30000.0
EPS = 1e-6
O_CQ, O_CKV, O_KR = 0, 384, 640
O_BQ, O_BK, O_BV = 704, 1216, 1472
O_GQ, O_GK, O_GV, O_GZ, O_GA, O_GB = 1728, 2240, 2752, 3264, 3776, 3784
O_DQ, O_DK, O_DV = 3792, 4304, 4560

C_ID, C_ONE, C_SU, C_SL, C_U64, C_L64 = 0, 128, 256, 384, 512, 576
C_MN = (640, 640 + 768)
C_MM = (640 + 256, 640 + 768 + 256)
C_ST = (640 + 512, 640 + 768 + 512)
C_W = 640 + 1536

_DBG = {}


class Buf:
    __slots__ = ("w", "r")

    def __init__(self):
        self.w = None
        self.r = {}


class Eng:
    def __init__(self, key, obj, sem, is_pe=False):
        self.key, self.obj, self.sem, self.is_pe = key, obj, sem, is_pe
        self.cnt = 0
        self.seen = {}
        self.slots = []
        self.slot_i = 0


class Slot:
    def __init__(self, key, sem):
        self.key, self.sem, self.cnt = key, sem, 0


class Sched:
    def __init__(self, nc, stack, n_dma_slots=12):
        self.nc = nc
        mk = lambda n: stack.enter_context(nc.semaphore(n))
        self.pe = Eng("pe", nc.tensor, mk("s_pe"), is_pe=True)
        self.dve = Eng("dve", nc.vector, mk("s_dve"))
        self.act = Eng("act", nc.scalar, mk("s_act"))
        self.pool = Eng("pool", nc.gpsimd, mk("s_pool"))
        self.sp = Eng("sp", nc.sync, mk("s_sp"))
        self.engs = [self.pe, self.dve, self.act, self.pool, self.sp]
        for q in (self.sp, self.pool, self.act):
            q.slots = [Slot(f"d_{q.key}{i}", mk(f"d_{q.key}{i}")) for i in range(n_dma_slots)]

    def _wait(self, eng, dep):
        key, sem, val = dep
        if eng.seen.get(key, 0) >= val:
            return
        eng.obj.wait_ge(sem, val)
        eng.seen[key] = val

    def _deps(self, eng, reads, writes):
        for b in reads:
            if b.w is not None and not (eng.is_pe and b.w[0] == eng.key):
                self._wait(eng, b.w)
        for b in writes:
            if b.w is not None and not (eng.is_pe and b.w[0] == eng.key):
                self._wait(eng, b.w)
            for d in b.r.values():
                if d[0] != eng.key:
                    self._wait(eng, d)

    def _mark(self, tok, reads, writes):
        for b in reads:
            b.r[tok[0]] = tok
        for b in writes:
            b.w = tok
            b.r = {}

    def op(self, eng, fn, reads=(), writes=()):
        self._deps(eng, reads, writes)
        ins = fn()
        eng.cnt += 1
        ins.then_inc(eng.sem, 1)
        self._mark((eng.key, eng.sem, eng.cnt), reads, writes)

    def dma(self, q, out, in_, reads=(), writes=(), **kw):
        slot = q.slots[q.slot_i % len(q.slots)]
        q.slot_i += 1
        if slot.cnt > 0:
            self._wait(q, (slot.key, slot.sem, slot.cnt * 16))
        self._deps(q, reads, writes)
        ins = q.obj.dma_start(out=out, in_=in_, **kw)
        slot.cnt += 1
        ins.then_inc(slot.sem, 16)
        self._mark((slot.key, slot.sem, slot.cnt * 16), reads, writes)

    def barrier(self):
        toks = [(e.key, e.sem, e.cnt) for e in self.engs if e.cnt > 0]
        for q in (self.sp, self.pool, self.act):
            toks += [(s.key, s.sem, s.cnt * 16) for s in q.slots if s.cnt > 0]
        for e in self.engs:
            for tk in toks:
                if tk[0] != e.key:
                    self._wait(e, tk)

    def finish(self):
        self.barrier()


def build_program(n_seq=2, n_layers=4, stop=None, dbg=(), start=None, only=None):
    nc = bass.Bass("TRN2", target_bir_lowering=False)
    L = n_layers

    def din(name, shape, dt=F32):
        return nc.dram_tensor(name, list(shape), dt, kind="ExternalInput").ap()

    def dscr(name, shape, dt=F32):
        kind = "ExternalOutput" if name in dbg else "Internal"
        if start == "mix" and name == "P_d":
            kind = "ExternalInput"
        return nc.dram_tensor(name, list(shape), dt, kind=kind).ap()

    x_in = din("x", [n_seq, 2048, D]); ctx_in = din("ctx", [n_seq, 256, D])
    c_in = din("c", [n_seq, D]); cctx_in = din("c_ctx", [D])
    norm1_g = din("norm1_g", [L, D]); norm2_g = din("norm2_g", [L, D])
    w_mod = din("w_mod", [L, D, 6 * D]); b_mod = din("b_mod", [L, 6 * D])
    w_in = din("w_in", [L, D, DIN])
    mla_qg = din("mla_q_norm_g", [L, 384]); mla_kvg = din("mla_kv_norm_g", [L, 256])
    mla_wuq = din("mla_w_uq", [L, 384, 768]); mla_wukv = din("mla_w_ukv", [L, 256, 1024])
    gqa_qg = din("gqa_q_norm_g", [L, 128]); gqa_kg = din("gqa_k_norm_g", [L, 128])
    gdn_cw = din("gdn_conv_w", [L, 5, 1536]); gdn_alog = din("gdn_a_log", [L, 8]); gdn_dtb = din("gdn_dt_bias", [L, 8])
    gdn_ng = din("gdn_norm_g", [L, 128]); swa_sink = din("swa_sink", [L, 4])
    w_out = din("w_out", [L, D, D])
    NF = (L + 1) // 2; NM = L // 2
    ffn_w1 = din("ffn_w1", [NF, D, FFN]); ffn_w3 = din("ffn_w3", [NF, D, FFN]); ffn_w2 = din("ffn_w2", [NF, FFN, D])
    if NM > 0:
        moe_router = din("moe_router", [NM, D, 8])
        moe_w1 = din("moe_w1", [NM, 8, D, EXD]); moe_w3 = din("moe_w3", [NM, 8, D, EXD]); moe_w2 = din("moe_w2", [NM, 8, EXD, D])
    final_g = din("final_norm_g", [D])
    rc128 = din("rc128", [2048, 512]); rs128 = din("rs128", [2048, 512])
    rc64 = din("rc64", [2048, 256]); rs64 = din("rs64", [2048, 256])
    cst_in = din("cst", [128, C_W])
    out_d = nc.dram_tensor("out", [n_seq, 2048, D], F32, kind="ExternalOutput").ap()

    mods_d = dscr("mods_d", [L, n_seq + 1, 6 * D])
    resid_d = dscr("resid_d", [n_seq, NT, D])
    P_d = dscr("P_d", [n_seq, NT, DIN])
    Y_d = dscr("Y_d", [n_seq, NT, D], BF16)
    H2T_d = dscr("H2T_d", [n_seq, 128, 16, NT], BF16)
    GATES_d = dscr("GATES_d", [n_seq, NT, 8])
    GTOK_d = dscr("GTOK_d", [n_seq, NT, 1040])
    GFT_d = dscr("GFT_d", [n_seq, 8, 128, NT])
    GO_d = dscr("GO_d", [n_seq, 2, NT, 512])

    with ExitStack() as st:
        S = Sched(nc, st)

        def V(fn, r=(), w=()): S.op(S.dve, fn, r, w)
        def A(fn, r=(), w=()): S.op(S.act, fn, r, w)
        def G(fn, r=(), w=()): S.op(S.pool, fn, r, w)
        def T(fn, r=(), w=()): S.op(S.pe, fn, r, w)

        _cnt = [0]

        def sbt(stack, shape, dt=F32, name=None):
            _cnt[0] += 1
            return stack.enter_context(nc.sbuf_tensor(name or f"sb{_cnt[0]}", list(shape), dt))

        psall = st.enter_context(nc.psum_tensor("psall", [128, 4096], F32))
        psallb = psall.bitcast(BF16)
        pb = [Buf() for _ in range(8)]

        def ps(bank, n=512, parts=128, off=0):
            return psall[0:parts, bank * 512 + off: bank * 512 + off + n]

        def psb(bank, n=1024, parts=128, off=0):
            return psallb[0:parts, bank * 1024 + off: bank * 1024 + off + n]

        b_mods = Buf()
        b_resid = [[Buf() for _ in range(NTILE)] for _ in range(n_seq)]
        b_P = [[Buf() for _ in range(NTILE)] for _ in range(n_seq)]
        b_Y = [[Buf() for _ in range(NTILE)] for _ in range(n_seq)]
        b_H2T = [[Buf() for _ in range(NTILE)] for _ in range(n_seq)]
        b_GATES = [[Buf() for _ in range(NTILE)] for _ in range(n_seq)]
        b_GTOK = [[Buf() for _ in range(NTILE)] for _ in range(n_seq)]
        b_GFT = [[Buf() for _ in range(NTILE)] for _ in range(n_seq)]
        b_GO = [[[Buf() for _ in range(NTILE)] for _ in range(2)] for _ in range(n_seq)]
        b_out = Buf()

        cst = sbt(st, [128, C_W], F32, "cst_sb")
        identb = sbt(st, [128, 128], BF16, "identb")
        swab = sbt(st, [128, 256], BF16, "swab")
        b_cst = Buf()
        S.dma(S.sp, cst[:], cst_in[:, :], writes=[b_cst])
        V(lambda: nc.vector.tensor_copy(out=identb[:], in_=cst[:, C_ID:C_ID + 128]), [b_cst], [b_cst])
        V(lambda: nc.vector.tensor_copy(out=swab[:], in_=cst[:, C_SU:C_SU + 256]), [b_cst], [b_cst])
        ident = cst[:, C_ID:C_ID + 128]
        ones = cst[:, C_ONE:C_ONE + 128]

        def xsrc(l, b, t):
            if l == 0:
                if t < 2:
                    return ctx_in[b, t * 128:(t + 1) * 128, :]
                return x_in[b, (t - 2) * 128:(t - 1) * 128, :]
            return resid_d[b, t * 128:(t + 1) * 128, :]

        def xsrc_buf(l, b, t):
            return [] if l == 0 else [b_resid[b][t]]

        def rstd_col(out_col, ssq_col, scale, eps, b_in):
            A(lambda: nc.scalar.activation(out=out_col, in_=ssq_col, func=AF.Sqrt, scale=scale, bias=eps), [b_in], [b_in])
            V(lambda: nc.vector.reciprocal(out=out_col, in_=out_col), [b_in], [b_in])

        def transpose_group(dst, srcs, bank, b_src, b_dst, dt_bf=True, evac="act", parts_out=128):
            n = len(srcs)
            K = srcs[0].shape[0]
            M = srcs[0].shape[1]

            def tr():
                for i, s_ in enumerate(srcs):
                    if dt_bf:
                        ins = nc.tensor.transpose(out=psb(bank, K, M, i * K), in_=s_, identity=identb[0:K, 0:K])
                    else:
                        ins = nc.tensor.transpose(out=ps(bank, K, M, i * K), in_=s_, identity=ident[0:K, 0:K])
                return ins
            T(tr, [b_src, b_cst], [pb[bank]])
            src = (psb(bank, n * K, M) if dt_bf else ps(bank, n * K, M)).rearrange("p (n k) -> p n k", k=K)
            if evac == "act":
                A(lambda: nc.scalar.copy(out=dst, in_=src), [pb[bank]], [b_dst])
            else:
                V(lambda: nc.vector.tensor_copy(out=dst, in_=src), [pb[bank]], [b_dst])

        def rope(out3, x3, cos3, sin3, tmp1, tmp2, q, bufs_r, bufs_w):
            V(lambda: nc.vector.tensor_tensor(out=tmp1, in0=x3, in1=cos3, op=ALU.mult), bufs_r, bufs_w)

            def sw():
                for a in range(2):
                    for bd in range(2):
                        bs = 1 - bd
                        o0 = (2 * a + bd) * q
                        s0 = (2 * a + bs) * q
                        ins = nc.vector.tensor_tensor(out=tmp2[:, :, o0:o0 + q], in0=x3[:, :, s0:s0 + q], in1=sin3[:, :, o0:o0 + q], op=ALU.mult)
                return ins
            V(sw, bufs_r, bufs_w)
            V(lambda: nc.vector.tensor_tensor(out=out3, in0=tmp1, in1=tmp2, op=ALU.add), bufs_r, bufs_w)

        def prologue():
            R_ = n_seq + 1
            with ExitStack() as ph:
                cT = sbt(ph, [128, 16, R_]); b_cT = Buf()
                modsb = sbt(ph, [R_, 6 * D]); b_modsb = Buf()
                bm = sbt(ph, [R_, 6 * D]); b_bm = Buf()
                wbuf = [sbt(ph, [128, 16, 512]) for _ in range(2)]; b_w = [Buf(), Buf()]
                for r in range(R_):
                    src = c_in[r] if r < n_seq else cctx_in
                    S.dma(S.sp, cT[:, :, r:r + 1], src.rearrange("(j p o) -> p j o", p=128, o=1), writes=[b_cT], allow_slow_non_contiguous=True)
                A(lambda: nc.scalar.activation(out=cT[:], in_=cT[:], func=AF.Silu), [b_cT], [b_cT])
                for l in range(L):
                    S.dma(S.sp, bm[:], b_mod[l].partition_broadcast(R_), writes=[b_bm])
                    for n in range(24):
                        wb_ = wbuf[n % 2]
                        S.dma(S.sp if n % 2 else S.act, wb_[:], w_mod[l, :, n * 512:(n + 1) * 512].rearrange("(j p) n -> p j n", p=128), writes=[b_w[n % 2]])
                        bank = n % 2

                        def mm():
                            for k in range(16):
                                ins = nc.tensor.matmul(ps(bank, 512, R_), lhsT=cT[:, k, :], rhs=wb_[:, k, :], start=(k == 0), stop=(k == 15))
                            return ins
                        T(mm, [b_cT, b_w[n % 2]], [pb[bank]])
                        V(lambda: nc.vector.tensor_tensor(out=modsb[:, n * 512:(n + 1) * 512], in0=ps(bank, 512, R_), in1=bm[:, n * 512:(n + 1) * 512], op=ALU.add),
                          [pb[bank], b_bm], [b_modsb])
                    S.dma(S.sp, mods_d[l], modsb[:], reads=[b_modsb], writes=[b_mods])
                S.barrier()

        def mod_row(l, b, stream, idx):
            row = n_seq if stream == 0 else b
            return mods_d[l, row, idx * D:(idx + 1) * D].partition_broadcast(128)

        def build_mod_consts(ph_tiles, l, b, stream, gain_ap, i_shift, i_scale, b_c):
            Amul, Badd, tmp = ph_tiles
            S.dma(S.sp, tmp[:], gain_ap.partition_broadcast(128), writes=[b_c])
            S.dma(S.sp, Amul[:], mod_row(l, b, stream, i_scale), reads=[b_mods], writes=[b_c])
            S.dma(S.sp, Badd[:], mod_row(l, b, stream, i_shift), reads=[b_mods], writes=[b_c])
            V(lambda: nc.vector.scalar_tensor_tensor(out=Amul[:], in0=Amul[:], scalar=1.0, in1=tmp[:], op0=ALU.add, op1=ALU.mult), [b_c], [b_c])

        def phase_A(b, l, hT, b_hT):
            with ExitStack() as ph:
                Amul = sbt(ph, [128, D]); Badd = sbt(ph, [128, D]); tmp = sbt(ph, [128, D]); b_c = Buf()
                xt = [sbt(ph, [128, D]) for _ in range(2)]; b_xt = [Buf(), Buf()]
                hf = sbt(ph, [128, D]); b_hf = Buf()
                hb = [sbt(ph, [128, D], BF16) for _ in range(2)]; b_hb = [Buf(), Buf()]
                col = sbt(ph, [128, 4]); b_col = Buf()
                for t in range(NTILE):
                    if t == 0 or t == 2:
                        build_mod_consts((Amul, Badd, tmp), l, b, 0 if t == 0 else 1, norm1_g[l], 0, 1, b_c)
                    i = t % 2
                    S.dma(S.sp, xt[i][:], xsrc(l, b, t), reads=xsrc_buf(l, b, t), writes=[b_xt[i]])
                    A(lambda: nc.scalar.activation(out=hf[:], in_=xt[i][:], func=AF.Square, accum_out=col[:, 0:1]), [b_xt[i]], [b_hf, b_col])
                    rstd_col(col[:, 1:2], col[:, 0:1], 1.0 / D, EPS, b_col)
                    V(lambda: nc.vector.scalar_tensor_tensor(out=hf[:], in0=xt[i][:], scalar=col[:, 1:2], in1=Amul[:], op0=ALU.mult, op1=ALU.mult),
                      [b_xt[i], b_col, b_c], [b_hf])
                    G(lambda: nc.gpsimd.tensor_tensor(out=hb[i][:], in0=hf[:], in1=Badd[:], op=ALU.add), [b_hf, b_c], [b_hb[i]])
                    for g_ in range(2):
                        transpose_group(hT[:, g_ * 8:(g_ + 1) * 8, t * 128:(t + 1) * 128],
                                        [hb[i][:, (g_ * 8 + j) * 128:(g_ * 8 + j + 1) * 128] for j in range(8)],
                                        4 + g_, b_hb[i], b_hT, evac="act" if g_ else "dve")
                S.barrier()

        def phase_B(b, l, hT, b_hT):
            with ExitStack() as ph:
                wb_ = [sbt(ph, [128, 16, 512], BF16) for _ in range(2)]; b_wb = [Buf(), Buf()]
                stg = [sbt(ph, [128, 512]) for _ in range(4)]; b_stg = [Buf() for _ in range(4)]
                chunks = [(c0, min(512, DIN - c0)) for c0 in range(0, DIN, 512)]
                it = 0
                for ci, (c0, cw) in enumerate(chunks):
                    w_ = wb_[ci % 2]
                    S.dma(S.pool, w_[:, :, 0:cw], w_in[l, :, c0:c0 + cw].rearrange("(j p) n -> p j n", p=128), writes=[b_wb[ci % 2]])
                    for t in range(NTILE):
                        bank = it % 4
                        it += 1

                        def mm():
                            for k in range(16):
                                ins = nc.tensor.matmul(ps(bank, cw), lhsT=hT[:, k, t * 128:(t + 1) * 128], rhs=w_[:, k, 0:cw], start=(k == 0), stop=(k == 15))
                            return ins
                        T(mm, [b_hT, b_wb[ci % 2]], [pb[bank]])
                        if it % 2:
                            A(lambda: nc.scalar.copy(out=stg[bank][:, 0:cw], in_=ps(bank, cw)), [pb[bank]], [b_stg[bank]])
                        else:
                            V(lambda: nc.vector.tensor_copy(out=stg[bank][:, 0:cw], in_=ps(bank, cw)), [pb[bank]], [b_stg[bank]])
                        S.dma(S.sp, P_d[b, t * 128:(t + 1) * 128, c0:c0 + cw], stg[bank][:, 0:cw], reads=[b_stg[bank]], writes=[b_P[b][t]])
                S.barrier()

        def attn_core(A_, qparts, chunks, scale, out_ap, b_q, b_k, b_out, sink=None):
            n = len(chunks)
            nb = (n * 128 + 511) // 512
            sbufs = pb[0:nb]

            def qk():
                for ci, (kparts, v_ap, mask) in enumerate(chunks):
                    o = psall[:, ci * 128:(ci + 1) * 128]
                    np_ = len(kparts)
                    for pi in range(np_):
                        ins = nc.tensor.matmul(o, lhsT=qparts[pi], rhs=kparts[pi], start=(pi == 0), stop=(pi == np_ - 1 and mask is None))
                    if mask is not None:
                        ins = nc.tensor.matmul(o, lhsT=identb[:], rhs=mask, start=False, stop=True)
                return ins
            T(qk, [b_q, b_k, b_cst], sbufs)
            col = A_["col"]; b_col = A_["b_col"]
            V(lambda: nc.vector.reduce_max(out=col[:, 0:1], in_=psall[:, 0:n * 128], axis=AX.X), sbufs, [b_col])
            if sink is None:
                V(lambda: nc.vector.tensor_scalar(out=col[:, 1:2], in0=col[:, 0:1], scalar1=-scale, scalar2=None, op0=ALU.mult), [b_col], [b_col])
            else:
                V(lambda: nc.vector.tensor_scalar(out=col[:, 1:2], in0=col[:, 0:1], scalar1=-scale, scalar2=sink[1], op0=ALU.mult, op1=ALU.min), [b_col, b_cst], [b_col])
            pexp = A_["pexp"]; b_pexp = A_["b_pexp"]
            A(lambda: nc.scalar.activation(out=pexp[:, 0:n * 128], in_=psall[:, 0:n * 128], func=AF.Exp, bias=col[:, 1:2], scale=scale, accum_out=col[:, 2:3]),
              sbufs + [b_col], [b_pexp, b_col])
            if sink is not None:
                A(lambda: nc.scalar.activation(out=col[:, 3:4], in_=sink[0], func=AF.Exp, bias=col[:, 1:2], scale=1.0), [b_col, b_cst], [b_col])
                V(lambda: nc.vector.tensor_tensor(out=col[:, 2:3], in0=col[:, 2:3], in1=col[:, 3:4], op=ALU.add), [b_col], [b_col])
            V(lambda: nc.vector.reciprocal(out=col[:, 4:5], in_=col[:, 2:3]), [b_col], [b_col])
            PT = A_["PT"]; b_PT = A_["b_PT"]
            gi = 0
            for c0 in range(0, n, 8):
                c1 = min(n, c0 + 8)
                transpose_group(PT[:, c0:c1, :], [pexp[:, ci * 128:(ci + 1) * 128] for ci in range(c0, c1)], 5 + gi % 2, b_pexp, b_PT,
                                evac="act" if gi % 2 else "dve")
                gi += 1
            dv = chunks[0][1].shape[-1]

            def pv():
                for ci, (kparts, v_ap, mask) in enumerate(chunks):
                    ins = nc.tensor.matmul(ps(7, dv), lhsT=PT[:, ci, :], rhs=v_ap, start=(ci == 0), stop=(ci == n - 1))
                return ins
            T(pv, [b_PT, b_k], [pb[7]])
            V(lambda: nc.vector.tensor_scalar(out=out_ap, in0=ps(7, dv), scalar1=col[:, 4:5], scalar2=None, op0=ALU.mult), [pb[7], b_col], [b_out])

        def attn_scratch(ph):
            return dict(col=sbt(ph, [128, 8]), b_col=Buf(), pexp=sbt(ph, [128, NT], BF16), b_pexp=Buf(),
                        PT=sbt(ph, [128, NTILE, 128], BF16), b_PT=Buf())

        def qtiles(l):
            return range(NTILE) if l < L - 1 else range(2, NTILE)

        def load_rope(ph_t, src, t, w, b_r):
            S.dma(S.sp, ph_t[:, 0:w], src[(t - 2) * 128:(t - 1) * 128, 0:w], writes=[b_r])

        def mixer_mla(b, l):
            with ExitStack() as ph:
                wuq = sbt(ph, [128, 3, 768], BF16); wukv = sbt(ph, [128, 2, 1024], BF16); b_w = Buf()
                gq = sbt(ph, [128, 384]); gkv = sbt(ph, [128, 256])
                S.dma(S.pool, wuq[:], mla_wuq[l].rearrange("(j p) n -> p j n", p=128), writes=[b_w])
                S.dma(S.pool, wukv[:], mla_wukv[l].rearrange("(j p) n -> p j n", p=128), writes=[b_w])
                S.dma(S.sp, gq[:], mla_qg[l].partition_broadcast(128), writes=[b_w])
                S.dma(S.sp, gkv[:], mla_kvg[l].partition_broadcast(128), writes=[b_w])
                KnT = sbt(ph, [128, 4, NT], BF16); KrT = sbt(ph, [64, NT], BF16); Vv = sbt(ph, [128, NTILE, 4, 128], BF16); b_kv = Buf()
                pa = [sbt(ph, [128, 384]) for _ in range(2)]; b_pa = [Buf(), Buf()]
                junk = sbt(ph, [128, 768]); b_t = Buf()
                col = sbt(ph, [128, 4])
                xn = sbt(ph, [128, 384], BF16); xnT = sbt(ph, [128, 3, 128], BF16)
                knb = sbt(ph, [128, 4, 128], BF16); krb = sbt(ph, [128, 64], BF16)
                rc = sbt(ph, [128, 256]); rs = sbt(ph, [128, 256]); b_r = Buf()
                t1 = sbt(ph, [128, 256]); t2 = sbt(ph, [128, 256])
                qf = sbt(ph, [128, 768]); qb = sbt(ph, [128, 4, 192], BF16)
                QnT = sbt(ph, [128, 4, 128], BF16); QrT = sbt(ph, [64, 4, 128], BF16); b_q = Buf()
                yt = [sbt(ph, [128, 512], BF16) for _ in range(2)]; b_yt = [Buf(), Buf()]
                A_ = attn_scratch(ph)
                for t in range(NTILE):
                    i = t % 2
                    S.dma(S.sp, pa[i][:, 0:320], P_d[b, t * 128:(t + 1) * 128, O_CKV:O_CKV + 320], reads=[b_P[b][t]], writes=[b_pa[i]])
                    if t >= 2:
                        load_rope(rc, rc64, t, 64, b_r); load_rope(rs, rs64, t, 64, b_r)
                    s1 = _DBG.get("s1", 9)
                    if s1 < 2:
                        continue
                    A(lambda: nc.scalar.activation(out=junk[:, 0:256], in_=pa[i][:, 0:256], func=AF.Square, accum_out=col[:, 0:1]), [b_pa[i]], [b_t])
                    rstd_col(col[:, 1:2], col[:, 0:1], 1.0 / 256, EPS, b_t)
                    V(lambda: nc.vector.scalar_tensor_tensor(out=xn[:, 0:256], in0=pa[i][:, 0:256], scalar=col[:, 1:2], in1=gkv[:], op0=ALU.mult, op1=ALU.mult),
                      [b_pa[i], b_t, b_w], [b_t])
                    if s1 < 3:
                        continue
                    transpose_group(xnT[:, 0:2, :], [xn[:, j * 128:(j + 1) * 128] for j in range(2)], 5, b_t, b_t, evac="dve")

                    def mmkv():
                        for hb_ in range(2):
                            for k in range(2):
                                ins = nc.tensor.matmul(ps(5 + hb_, 512), lhsT=xnT[:, k, :], rhs=wukv[:, k, hb_ * 512:(hb_ + 1) * 512], start=(k == 0), stop=(k == 1))
                        return ins
                    if s1 < 3.2:
                        continue
                    T(mmkv, [b_t, b_w], [pb[5], pb[6]])
                    kv3 = psall[:, 5 * 512:7 * 512].rearrange("p (h c) -> p h c", c=256)
                    if s1 < 3.4:
                        continue
                    for hb_ in range(2):
                        kvb = ps(5 + hb_, 512).rearrange("p (h c) -> p h c", c=256)
                        A(lambda: nc.scalar.copy(out=Vv[:, t, 2 * hb_:2 * hb_ + 2, :], in_=kvb[:, :, 128:256]), [pb[5 + hb_]], [b_kv])
                    if s1 < 3.6:
                        continue
                    for hb_ in range(2):
                        kvb = ps(5 + hb_, 512).rearrange("p (h c) -> p h c", c=256)
                        A(lambda: nc.scalar.copy(out=knb[:, 2 * hb_:2 * hb_ + 2, :], in_=kvb[:, :, 0:128]), [pb[5 + hb_]], [b_t])
                    if s1 < 4:
                        continue
                    transpose_group(KnT[:, :, t * 128:(t + 1) * 128], [knb[:, h, :] for h in range(4)], 7, b_t, b_kv, evac="act")
                    if s1 < 5:
                        continue
                    if t >= 2:
                        x3 = pa[i][:, 256:320].unsqueeze(1)
                        rope(krb[:].unsqueeze(1), x3, rc[:, 0:64].unsqueeze(1), rs[:, 0:64].unsqueeze(1), t1[:, 0:64].unsqueeze(1), t2[:, 0:64].unsqueeze(1), 16,
                             [b_pa[i], b_r, b_t], [b_t])
                    else:
                        V(lambda: nc.vector.tensor_copy(out=krb[:], in_=pa[i][:, 256:320]), [b_pa[i]], [b_t])
                    if s1 < 6:
                        continue
                    transpose_group(KrT[:, t * 128:(t + 1) * 128].unsqueeze(1), [krb[:, :]], 7, b_t, b_kv, evac="dve")
                for t in (qtiles(l) if _DBG.get("mla_cut", 3) >= 2 else []):
                    i = t % 2
                    S.dma(S.sp, pa[i][:, 0:384], P_d[b, t * 128:(t + 1) * 128, O_CQ:O_CQ + 384], reads=[b_P[b][t]], writes=[b_pa[i]])
                    if t >= 2:
                        load_rope(rc, rc64, t, 256, b_r); load_rope(rs, rs64, t, 256, b_r)
                    A(lambda: nc.scalar.activation(out=junk[:, 0:384], in_=pa[i][:, 0:384], func=AF.Square, accum_out=col[:, 0:1]), [b_pa[i]], [b_t])
                    rstd_col(col[:, 1:2], col[:, 0:1], 1.0 / 384, EPS, b_t)
                    V(lambda: nc.vector.scalar_tensor_tensor(out=xn[:, 0:384], in0=pa[i][:, 0:384], scalar=col[:, 1:2], in1=gq[:], op0=ALU.mult, op1=ALU.mult),
                      [b_pa[i], b_t, b_w], [b_t])
                    transpose_group(xnT[:, 0:3, :], [xn[:, j * 128:(j + 1) * 128] for j in range(3)], 5, b_t, b_t, evac="dve")

                    def mmq():
                        for (bank, c0, cw) in ((5, 0, 512), (6, 512, 256)):
                            for k in range(3):
                                ins = nc.tensor.matmul(ps(bank, cw), lhsT=xnT[:, k, :], rhs=wuq[:, k, c0:c0 + cw], start=(k == 0), stop=(k == 2))
                        return ins
                    T(mmq, [b_t, b_w], [pb[5], pb[6]])
                    A(lambda: nc.scalar.copy(out=qf[:], in_=psall[:, 5 * 512:5 * 512 + 768]), [pb[5], pb[6]], [b_t])
                    qf3 = qf[:].rearrange("p (h c) -> p h c", c=192)
                    V(lambda: nc.vector.tensor_copy(out=qb[:, :, 0:128], in_=qf3[:, :, 0:128]), [b_t], [b_q])
                    if t >= 2:
                        rope(qb[:, :, 128:192], qf3[:, :, 128:192], rc[:].rearrange("p (h c) -> p h c", c=64), rs[:].rearrange("p (h c) -> p h c", c=64),
                             t1[:].rearrange("p (h c) -> p h c", c=64), t2[:].rearrange("p (h c) -> p h c", c=64), 16, [b_t, b_r], [b_q, b_t])
                    else:
                        V(lambda: nc.vector.tensor_copy(out=qb[:, :, 128:192], in_=qf3[:, :, 128:192]), [b_t], [b_q])
                    transpose_group(QnT[:], [qb[:, h, 0:128] for h in range(4)], 5, b_q, b_q, evac="act")
                    transpose_group(QrT[:], [qb[:, h, 128:192] for h in range(4)], 6, b_q, b_q, evac="dve")
                    nk = 2 if t < 2 else NTILE
                    for h in (range(4) if _DBG.get("mla_cut", 3) >= 3 else []):
                        chunks = [([KnT[:, h, kc * 128:(kc + 1) * 128], KrT[:, kc * 128:(kc + 1) * 128]], Vv[:, kc, h, :], None) for kc in range(nk)]
                        attn_core(A_, [QnT[:, h, :], QrT[:, h, :]], chunks, 192 ** -0.5, yt[i][:, h * 128:(h + 1) * 128], b_q, b_kv, b_yt[i])
                    S.dma(S.sp, Y_d[b, t * 128:(t + 1) * 128, 0:512], yt[i][:], reads=[b_yt[i]], writes=[b_Y[b][t]])
                S.barrier()

        def mixer_gqa_swa(b, l, swa):
            oq, ok, ov = (O_DQ, O_DK, O_DV) if swa else (O_BQ, O_BK, O_BV)
            ycol = 1536 if swa else 512
            with ExitStack() as ph:
                b_w = Buf()
                if not swa:
                    gq = sbt(ph, [128, 512]); gk = sbt(ph, [128, 256])
                    for h in range(4):
                        S.dma(S.sp, gq[:, h * 128:(h + 1) * 128], gqa_qg[l].partition_broadcast(128), writes=[b_w])
                    for h in range(2):
                        S.dma(S.sp, gk[:, h * 128:(h + 1) * 128], gqa_kg[l].partition_broadcast(128), writes=[b_w])
                else:
                    snk = sbt(ph, [128, 8])
                    S.dma(S.sp, snk[:, 0:4], swa_sink[l].partition_broadcast(128), writes=[b_w])
                    V(lambda: nc.vector.tensor_scalar(out=snk[:, 4:8], in0=snk[:, 0:4], scalar1=-1.0, scalar2=None, op0=ALU.mult), [b_w], [b_w])
                KT = sbt(ph, [128, 2, NT], BF16); Vv = sbt(ph, [128, NTILE, 2, 128], BF16); b_kv = Buf()
                pa = [sbt(ph, [128, 512]) for _ in range(2)]; b_pa = [Buf(), Buf()]
                sq = sbt(ph, [128, 512]); b_t = Buf()
                col = sbt(ph, [128, 16])
                xf = sbt(ph, [128, 512]); xb = sbt(ph, [128, 512], BF16)
                rc = sbt(ph, [128, 512]); rs = sbt(ph, [128, 512]); b_r = Buf()
                t1 = sbt(ph, [128, 512]); t2 = sbt(ph, [128, 512])
                QT = sbt(ph, [128, 4, 128], BF16); b_q = Buf()
                yt = [sbt(ph, [128, 512], BF16) for _ in range(2)]; b_yt = [Buf(), Buf()]
                A_ = attn_scratch(ph)
                if swa:
                    b_w = b_w

                def prep(src_ap, H, gain, t, b_src):
                    W_ = H * 128
                    cur = src_ap
                    if not swa:
                        V(lambda: nc.vector.tensor_tensor(out=sq[:, 0:W_], in0=src_ap, in1=src_ap, op=ALU.mult), [b_src], [b_t])
                        V(lambda: nc.vector.reduce_sum(out=col[:, 0:H], in_=sq[:, 0:W_].rearrange("p (h c) -> p h c", c=128), axis=AX.X), [b_t], [b_t])
                        rstd_col(col[:, 8:8 + H], col[:, 0:H], 1.0 / 128, EPS, b_t)
                        V(lambda: nc.vector.tensor_tensor(out=xf[:, 0:W_].rearrange("p (h c) -> p h c", c=128), in0=src_ap.rearrange("p (h c) -> p h c", c=128),
                                                          in1=col[:, 8:8 + H].unsqueeze(2).to_broadcast([128, H, 128]), op=ALU.mult), [b_src, b_t], [b_t])
                        V(lambda: nc.vector.tensor_tensor(out=xf[:, 0:W_], in0=xf[:, 0:W_], in1=gain[:, 0:W_], op=ALU.mult), [b_t, b_w], [b_t])
                        cur = xf[:, 0:W_]
                    r3 = lambda a: a[:, 0:W_].rearrange("p (h c) -> p h c", c=128)
                    if t >= 2:
                        rope(r3(xb), cur.rearrange("p (h c) -> p h c", c=128), r3(rc), r3(rs), r3(t1), r3(t2), 32, [b_src, b_t, b_r], [b_t])
                    else:
                        V(lambda: nc.vector.tensor_copy(out=xb[:, 0:W_], in_=cur), [b_src, b_t], [b_t])

                for t in range(NTILE):
                    i = t % 2
                    S.dma(S.sp, pa[i][:], P_d[b, t * 128:(t + 1) * 128, ok:ok + 512], reads=[b_P[b][t]], writes=[b_pa[i]])
                    if t >= 2:
                        load_rope(rc, rc128, t, 256, b_r); load_rope(rs, rs128, t, 256, b_r)
                    prep(pa[i][:, 0:256], 2, None if swa else gk, t, b_pa[i])
                    transpose_group(KT[:, :, t * 128:(t + 1) * 128], [xb[:, h * 128:(h + 1) * 128] for h in range(2)], 7, b_t, b_kv, evac="act")
                    V(lambda: nc.vector.tensor_copy(out=Vv[:, t, :, :], in_=pa[i][:, 256:512].rearrange("p (h c) -> p h c", c=128)), [b_pa[i]], [b_kv])
                for t in qtiles(l):
                    i = t % 2
                    S.dma(S.sp, pa[i][:], P_d[b, t * 128:(t + 1) * 128, oq:oq + 512], reads=[b_P[b][t]], writes=[b_pa[i]])
                    if t >= 2:
                        load_rope(rc, rc128, t, 512, b_r); load_rope(rs, rs128, t, 512, b_r)
                    prep(pa[i][:, 0:512], 4, None if swa else gq, t, b_pa[i])
                    transpose_group(QT[:], [xb[:, h * 128:(h + 1) * 128] for h in range(4)], 5, b_t, b_q, evac="act")
                    for h in range(4):
                        kvh = h // 2
                        if swa and t >= 2:
                            n_ = t - 2
                            kl = [(0, None), (1, None)]
                            if n_ >= 1:
                                kl.append((t - 1, swab[:, 0:128]))
                            kl.append((t, None))
                            if n_ <= 14:
                                kl.append((t + 1, swab[:, 128:256]))
                        else:
                            kl = [(kc, None) for kc in range(2 if t < 2 else NTILE)]
                        chunks = [([KT[:, kvh, kc * 128:(kc + 1) * 128]], Vv[:, kc, kvh, :], m) for kc, m in kl]
                        attn_core(A_, [QT[:, h, :]], chunks, 128 ** -0.5, yt[i][:, h * 128:(h + 1) * 128], b_q, b_kv, b_yt[i],
                                  sink=(snk[:, h:h + 1], snk[:, 4 + h:5 + h]) if swa else None)
                    S.dma(S.sp, Y_d[b, t * 128:(t + 1) * 128, ycol:ycol + 512], yt[i][:], reads=[b_yt[i]], writes=[b_Y[b][t]])
                S.barrier()

        def gdn_prep(b, l):
            with ExitStack() as ph:
                cwb2 = sbt(ph, [128, 5 * 1536]); b_w = Buf()
                S.dma(S.sp, cwb2[:], gdn_cw[l].rearrange("j c -> (j c)").partition_broadcast(128), writes=[b_w])
                cwb = cwb2[:].rearrange("p (j c) -> p j c", c=1536)
                gc = sbt(ph, [128, 16])
                S.dma(S.sp, gc[:, 0:8], gdn_alog[l].partition_broadcast(128), writes=[b_w])
                S.dma(S.sp, gc[:, 8:16], gdn_dtb[l].partition_broadcast(128), writes=[b_w])
                A(lambda: nc.scalar.activation(out=gc[:, 0:8], in_=gc[:, 0:8], func=AF.Exp), [b_w], [b_w])
                V(lambda: nc.vector.tensor_scalar(out=gc[:, 0:8], in0=gc[:, 0:8], scalar1=-1.0, scalar2=None, op0=ALU.mult), [b_w], [b_w])
                xs = [[sbt(ph, [128, 1536]) for _ in range(5)] for _ in range(2)]; b_xs = [[Buf() for _ in range(5)] for _ in range(2)]
                ab = [sbt(ph, [128, 16]) for _ in range(2)]; b_ab = [Buf(), Buf()]
                acc = sbt(ph, [128, 1536]); tm = sbt(ph, [128, 1536]); b_acc = Buf(); b_tm = Buf()
                col = sbt(ph, [128, 16]); b_col = Buf()
                tok = [sbt(ph, [128, 1040]) for _ in range(2)]; b_tok = [Buf(), Buf()]
                qkn = sbt(ph, [128, 1024]); b_qkn = Buf()
                ft = [sbt(ph, [128, 8, 128]) for _ in range(2)]; b_ft = [Buf(), Buf()]
                for t in range(NTILE):
                    i = t % 2
                    lo, hi = (0, 256) if t < 2 else (256, NT)
                    for j in range(5):
                        o = j - 2
                        r0 = t * 128 + o
                        r1 = r0 + 128
                        v0, v1 = max(r0, lo), min(r1, hi)
                        if v0 != r0 or v1 != r1:
                            G(lambda: nc.gpsimd.memset(xs[i][j][:], 0.0), [], [b_xs[i][j]])
                        t0_, t1_ = max(0, (v0 - 2) // 128), min(NTILE - 1, (v1 + 1) // 128)
                        S.dma(S.sp if j % 2 else S.act, xs[i][j][v0 - r0:v1 - r0, :], P_d[b, v0:v1, O_GQ:O_GQ + 1536],
                              reads=[b_P[b][tt] for tt in range(max(0, t - 1), min(NTILE, t + 2))], writes=[b_xs[i][j]])
                    S.dma(S.sp, ab[i][:], P_d[b, t * 128:(t + 1) * 128, O_GA:O_GA + 16], reads=[b_P[b][t]], writes=[b_ab[i]])
                    V(lambda: nc.vector.tensor_tensor(out=acc[:], in0=xs[i][2][:], in1=cwb[:, 2, :], op=ALU.mult), [b_xs[i][2], b_w], [b_acc])
                    for j in (0, 1, 3, 4):
                        G(lambda: nc.gpsimd.tensor_tensor(out=tm[:], in0=xs[i][j][:], in1=cwb[:, j, :], op=ALU.mult), [b_xs[i][j], b_w], [b_tm])
                        V(lambda: nc.vector.tensor_tensor(out=acc[:], in0=acc[:], in1=tm[:], op=ALU.add), [b_tm, b_acc], [b_acc])
                    A(lambda: nc.scalar.activation(out=acc[:], in_=acc[:], func=AF.Silu), [b_acc], [b_acc])
                    V(lambda: nc.vector.tensor_tensor(out=tm[:, 0:1024], in0=acc[:, 0:1024], in1=acc[:, 0:1024], op=ALU.mult), [b_acc], [b_tm])
                    V(lambda: nc.vector.reduce_sum(out=col[:, 0:8], in_=tm[:, 0:1024].rearrange("p (h c) -> p h c", c=128), axis=AX.X), [b_tm], [b_col])
                    rstd_col(col[:, 8:16], col[:, 0:8], 1.0, 1e-6, b_col)
                    V(lambda: nc.vector.tensor_scalar(out=col[:, 8:12], in0=col[:, 8:12], scalar1=128 ** -0.5, scalar2=None, op0=ALU.mult), [b_col], [b_col])
                    V(lambda: nc.vector.tensor_tensor(out=qkn[:].rearrange("p (h c) -> p h c", c=128), in0=acc[:, 0:1024].rearrange("p (h c) -> p h c", c=128),
                                                      in1=col[:, 8:16].unsqueeze(2).to_broadcast([128, 8, 128]), op=ALU.mult), [b_acc, b_col], [b_qkn])
                    A(lambda: nc.scalar.copy(out=tok[i][:, 0:512], in_=qkn[:, 512:1024]), [b_qkn], [b_tok[i]])
                    A(lambda: nc.scalar.copy(out=tok[i][:, 512:1024], in_=acc[:, 1024:1536]), [b_acc], [b_tok[i]])
                    V(lambda: nc.vector.tensor_tensor(out=col[:, 0:8], in0=ab[i][:, 0:8], in1=gc[:, 8:16], op=ALU.add), [b_ab[i], b_w, b_col], [b_col])
                    A(lambda: nc.scalar.activation(out=col[:, 0:8], in_=col[:, 0:8], func=AF.Exp), [b_col], [b_col])
                    A(lambda: nc.scalar.activation(out=col[:, 0:8], in_=col[:, 0:8], func=AF.Ln, bias=1.0), [b_col], [b_col])
                    V(lambda: nc.vector.tensor_tensor(out=tok[i][:, 1024:1032], in0=col[:, 0:8], in1=gc[:, 0:8], op=ALU.mult), [b_col, b_w], [b_tok[i]])
                    A(lambda: nc.scalar.activation(out=tok[i][:, 1032:1040], in_=ab[i][:, 8:16], func=AF.Sigmoid), [b_ab[i]], [b_tok[i]])
                    S.dma(S.sp, GTOK_d[b, t * 128:(t + 1) * 128, :], tok[i][:], reads=[b_tok[i]], writes=[b_GTOK[b][t]])
                    for g_ in range(2):
                        transpose_group(ft[i][:, g_ * 4:(g_ + 1) * 4, :], [qkn[:, (g_ * 4 + j) * 128:(g_ * 4 + j + 1) * 128] for j in range(4)],
                                        4 + g_, b_qkn, b_ft[i], dt_bf=False, evac="act" if g_ else "dve")
                    S.dma(S.act, GFT_d[b, :, :, t * 128:(t + 1) * 128].rearrange("g d t -> d g t"), ft[i][:], reads=[b_ft[i]], writes=[b_GFT[b][t]])
                S.barrier()

        def gdn_scan(bs, l):
            with ExitStack() as ph:
                orders = [list(range(36)), [3, 2, 1, 0] + list(range(35, 3, -1))]
                st_ = []
                for (b_, d) in [(b_, d) for b_ in bs for d in range(2)]:
                    Z = dict(d=d, seq=b_)
                    Z["S"] = sbt(ph, [128, 4, 128]); Z["b_S"] = Buf()
                    Z["tok"] = [sbt(ph, [64, 1040]) for _ in range(2)]; Z["b_tok"] = [Buf(), Buf()]
                    Z["ft"] = [sbt(ph, [128, 8, 64]) for _ in range(2)]; Z["b_ft"] = [Buf(), Buf()]
                    for nm in ("gcol", "egl", "dl", "bg", "eg"):
                        Z[nm] = sbt(ph, [128, 4])
                    for nm in ("diagG", "diagB", "KK", "KQ", "DN", "DM", "Nm", "Mm", "X", "N2", "M2", "intraT", "tA"):
                        Z[nm] = sbt(ph, [64, 4, 64])
                    for nm in ("vb", "kbg", "kdec", "u", "vnew", "o"):
                        Z[nm] = sbt(ph, [64, 4, 128])
                    Z["wT"] = sbt(ph, [128, 4, 64]); Z["qgT"] = sbt(ph, [128, 4, 64]); Z["eG"] = sbt(ph, [128, 4, 64])
                    Z["b"] = Buf()
                    Z["banks"] = (2 * len(st_), 2 * len(st_) + 1)
                    st_.append(Z)
                    V(lambda: nc.vector.memset(Z["S"][:], 0.0), [], [Z["b_S"]])
                I64 = cst[0:64, C_ID:C_ID + 64]
                def body(Z, step):
                    if True:
                        d = Z["d"]; b = Z["seq"]
                        c = orders[d][step]
                        i = step % 2
                        bz = Z["b"]
                        BA, BB = Z["banks"]
                        B0 = B1 = BA
                        B2 = B3 = BB
                        tk = Z["tok"][i]; ft = Z["ft"][i]
                        S.dma(S.sp, tk[:], GTOK_d[b, c * 64:(c + 1) * 64, :], reads=[b_GTOK[b][c // 2]], writes=[Z["b_tok"][i]])
                        yield
                        S.dma(S.act, ft[:], GFT_d[b, :, :, c * 64:(c + 1) * 64].rearrange("g d t -> d g t"), reads=[b_GFT[b][c // 2]], writes=[Z["b_ft"][i]])
                        yield
                        rd = [Z["b_tok"][i], Z["b_ft"][i], b_cst, bz]
                        k_tok = tk[:, 0:512].rearrange("p (h c) -> p h c", c=128)
                        v_tok = tk[:, 512:1024].rearrange("p (h c) -> p h c", c=128)
                        gs = tk[:, 1024 + 4 * d:1028 + 4 * d]
                        beta = tk[:, 1032 + 4 * d:1036 + 4 * d]
                        qT = ft[:, 0:4, :]; kT = ft[:, 4:8, :]
                        tri = cst[0:64, (C_U64 if d == 0 else C_L64):(C_U64 if d == 0 else C_L64) + 64]
                        mN = cst[0:64, C_MN[d]:C_MN[d] + 256].rearrange("p (h c) -> p h c", c=64)
                        mM = cst[0:64, C_MM[d]:C_MM[d] + 256].rearrange("p (h c) -> p h c", c=64)
                        mS = cst[0:64, C_ST[d]:C_ST[d] + 256].rearrange("p (h c) -> p h c", c=64)
                        bc64 = lambda a: a.unsqueeze(2).to_broadcast([64, 4, 64])
                        bc128 = lambda a: a.unsqueeze(2).to_broadcast([64, 4, 128])
                        p3 = lambda bank, parts=64, w=64: ps(bank, 4 * w, parts).rearrange("p (h c) -> p h c", c=w)
                        def mm1():
                            nc.tensor.matmul(ps(B0, 4, 64), lhsT=tri, rhs=gs, start=True, stop=True)
                            return nc.tensor.matmul(ps(B0, 4, 128, 8), lhsT=ones[0:64, :], rhs=gs, start=True, stop=True)
                        T(mm1, rd, [pb[B0]])
                        yield
                        V(lambda: nc.vector.tensor_copy(out=Z["gcol"][0:64, :], in_=ps(B0, 4, 64)), [pb[B0]], [bz])
                        yield
                        A(lambda: nc.scalar.activation(out=Z["egl"][:], in_=ps(B0, 4, 128, 8), func=AF.Exp), [pb[B0]], [bz])
                        yield
                        V(lambda: nc.vector.tensor_tensor(out=Z["dl"][0:64, :], in0=ps(B0, 4, 64, 8), in1=Z["gcol"][0:64, :], op=ALU.subtract), [pb[B0], bz], [bz])
                        yield
                        A(lambda: nc.scalar.activation(out=Z["dl"][0:64, :], in_=Z["dl"][0:64, :], func=AF.Exp), [bz], [bz])
                        yield
                        A(lambda: nc.scalar.activation(out=Z["eg"][0:64, :], in_=Z["gcol"][0:64, :], func=AF.Exp), [bz], [bz])
                        yield
                        V(lambda: nc.vector.tensor_tensor(out=Z["bg"][0:64, :], in0=Z["eg"][0:64, :], in1=beta, op=ALU.mult), rd, [bz])
                        yield
                        V(lambda: nc.vector.tensor_tensor(out=Z["diagG"][:], in0=I64.unsqueeze(1).to_broadcast([64, 4, 64]), in1=bc64(Z["gcol"][0:64, :]), op=ALU.mult), rd, [bz])
                        yield
                        V(lambda: nc.vector.tensor_tensor(out=Z["diagB"][:], in0=I64.unsqueeze(1).to_broadcast([64, 4, 64]), in1=bc64(beta), op=ALU.mult), rd, [bz])
                        yield

                        def mm2():
                            nc.tensor.matmul(ps(B1, 256, 128), lhsT=ones[0:64, :], rhs=Z["diagG"][:].rearrange("p h c -> p (h c)"), start=True, stop=True)
                            return nc.tensor.matmul(ps(B1, 256, 64, 256), lhsT=ones[0:64, 0:64], rhs=Z["diagB"][:].rearrange("p h c -> p (h c)"), start=True, stop=True)
                        T(mm2, rd, [pb[B1]])
                        yield
                        Grow = ps(B1, 256, 64).rearrange("p (h c) -> p h c", c=64)
                        Brow = ps(B1, 256, 64, 256).rearrange("p (h c) -> p h c", c=64)
                        def mm3():
                            for h in range(4):
                                nc.tensor.matmul(ps(B2, 64, 64, h * 64), lhsT=kT[:, h, :], rhs=kT[:, h, :], start=True, stop=True)
                            for h in range(4):
                                ins = nc.tensor.matmul(ps(B2, 64, 64, 256 + h * 64), lhsT=kT[:, h, :], rhs=qT[:, h, :], start=True, stop=True)
                            return ins
                        T(mm3, rd, [pb[B2]])
                        yield
                        A(lambda: nc.scalar.copy(out=Z["KK"][:], in_=ps(B2, 256, 64).rearrange("p (h c) -> p h c", c=64)), [pb[B2]], [bz])
                        yield
                        A(lambda: nc.scalar.copy(out=Z["KQ"][:], in_=ps(B2, 256, 64, 256).rearrange("p (h c) -> p h c", c=64)), [pb[B2]], [bz])
                        yield
                        V(lambda: nc.vector.tensor_tensor(out=Z["DN"][:], in0=bc64(Z["gcol"][0:64, :]), in1=Grow, op=ALU.subtract), [pb[B1], bz], [bz])
                        yield
                        V(lambda: nc.vector.tensor_tensor(out=Z["DN"][:], in0=Z["DN"][:], in1=mN, op=ALU.add), rd, [bz])
                        yield
                        A(lambda: nc.scalar.activation(out=Z["DN"][:], in_=Z["DN"][:], func=AF.Exp), [bz], [bz])
                        yield
                        V(lambda: nc.vector.scalar_tensor_tensor(out=Z["tA"][:], in0=Z["DN"][:], scalar=-1.0, in1=Z["KK"][:], op0=ALU.mult, op1=ALU.mult), [bz], [bz])
                        yield
                        V(lambda: nc.vector.tensor_tensor(out=Z["Nm"][:], in0=Z["tA"][:], in1=bc64(beta), op=ALU.mult), rd, [bz])
                        yield
                        V(lambda: nc.vector.tensor_tensor(out=Z["DM"][:], in0=Grow, in1=bc64(Z["gcol"][0:64, :]), op=ALU.subtract), [pb[B1], bz], [bz])
                        yield
                        V(lambda: nc.vector.tensor_tensor(out=Z["DM"][:], in0=Z["DM"][:], in1=mM, op=ALU.add), rd, [bz])
                        yield
                        A(lambda: nc.scalar.activation(out=Z["DM"][:], in_=Z["DM"][:], func=AF.Exp), [bz], [bz])
                        yield
                        V(lambda: nc.vector.tensor_tensor(out=Z["intraT"][:], in0=Z["DM"][:], in1=Z["KQ"][:], op=ALU.mult), [bz], [bz])
                        yield
                        V(lambda: nc.vector.tensor_tensor(out=Z["tA"][:], in0=Z["DM"][:], in1=mS, op=ALU.mult), rd, [bz])
                        yield
                        V(lambda: nc.vector.scalar_tensor_tensor(out=Z["tA"][:], in0=Z["tA"][:], scalar=-1.0, in1=Z["KK"][:], op0=ALU.mult, op1=ALU.mult), [bz], [bz])
                        yield
                        V(lambda: nc.vector.tensor_tensor(out=Z["Mm"][:], in0=Z["tA"][:], in1=Brow, op=ALU.mult), [pb[B1], bz], [bz])
                        yield
                        V(lambda: nc.vector.tensor_tensor(out=Z["X"][:], in0=Z["Mm"][:], in1=I64.unsqueeze(1).to_broadcast([64, 4, 64]), op=ALU.add), rd, [bz])
                        yield
                        A(lambda: nc.scalar.activation(out=Z["eG"][:], in_=ps(B1, 256, 128).rearrange("p (h c) -> p h c", c=64), func=AF.Exp), [pb[B1]], [bz])
                        yield
                        V(lambda: nc.vector.tensor_tensor(out=Z["qgT"][:], in0=qT, in1=Z["eG"][:], op=ALU.mult), rd, [bz])
                        yield
                        Ncur, Mcur = Z["Nm"], Z["Mm"]
                        Nn, Mn = Z["N2"], Z["M2"]
                        for s_ in range(5):
                            last = (s_ == 4)

                            def sqr():
                                for h in range(4):
                                    ins = nc.tensor.matmul(ps(B2, 64, 64, h * 64), lhsT=Mcur[:, h, :], rhs=Ncur[:, h, :], start=True, stop=True)
                                if not last:
                                    for h in range(4):
                                        ins = nc.tensor.matmul(ps(B2, 64, 64, 256 + h * 64), lhsT=Ncur[:, h, :], rhs=Mcur[:, h, :], start=True, stop=True)
                                return ins
                            T(sqr, [bz], [pb[B2]])
                            yield
                            A(lambda: nc.scalar.copy(out=Nn[:], in_=p3(B2)), [pb[B2]], [bz])
                            yield
                            if not last:
                                V(lambda: nc.vector.tensor_copy(out=Mn[:], in_=ps(B2, 256, 64, 256).rearrange("p (h c) -> p h c", c=64)), [pb[B2]], [bz])

                            def xup():
                                for h in range(4):
                                    ins = nc.tensor.matmul(ps(B3, 64, 64, h * 64), lhsT=Nn[:, h, :], rhs=Z["X"][:, h, :], start=True, stop=True)
                                return ins
                            T(xup, [bz], [pb[B3]])
                            yield
                            V(lambda: nc.vector.tensor_tensor(out=Z["X"][:], in0=Z["X"][:], in1=p3(B3), op=ALU.add), [pb[B3], bz], [bz])
                            yield
                            Ncur, Nn = Nn, Ncur
                            Mcur, Mn = Mn, Mcur
                        V(lambda: nc.vector.tensor_tensor(out=Z["vb"][:], in0=v_tok, in1=bc128(beta), op=ALU.mult), rd, [bz])
                        yield
                        G(lambda: nc.gpsimd.tensor_tensor(out=Z["kbg"][:], in0=k_tok, in1=bc128(Z["bg"][0:64, :]), op=ALU.mult), rd, [bz])
                        yield
                        G(lambda: nc.gpsimd.tensor_tensor(out=Z["kdec"][:], in0=k_tok, in1=bc128(Z["dl"][0:64, :]), op=ALU.mult), rd, [bz])
                        yield

                        def mm7a():
                            for h in range(4):
                                ins = nc.tensor.matmul(ps(B2, 128, 64, h * 128), lhsT=Z["X"][:, h, :], rhs=Z["vb"][:, h, :], start=True, stop=True)
                            return ins
                        T(mm7a, [bz], [pb[B2]])
                        yield
                        A(lambda: nc.scalar.copy(out=Z["u"][:], in_=p3(B2, 64, 128)), [pb[B2]], [bz])
                        yield

                        def mm7b():
                            for h in range(4):
                                ins = nc.tensor.matmul(ps(B3, 64, 128, h * 64), lhsT=Z["kbg"][:, h, :], rhs=Z["X"][:, h, :], start=True, stop=True)
                            return ins
                        T(mm7b, [bz], [pb[B3]])
                        yield
                        V(lambda: nc.vector.tensor_copy(out=Z["wT"][:], in_=p3(B3, 128, 64)), [pb[B3]], [bz])
                        yield
                        Sx = Z["S"]

                        def mm8():
                            for h in range(4):
                                ins = nc.tensor.matmul(ps(B0, 128, 64, h * 128), lhsT=Z["wT"][:, h, :], rhs=Sx[:, h, :], start=True, stop=True)
                            return ins
                        T(mm8, [bz, Z["b_S"]], [pb[B0]])
                        yield
                        V(lambda: nc.vector.tensor_tensor(out=Z["vnew"][:], in0=Z["u"][:], in1=p3(B0, 64, 128), op=ALU.subtract), [pb[B0], bz], [bz])
                        yield

                        def mm9a():
                            for h in range(4):
                                nc.tensor.matmul(ps(B2, 128, 64, h * 128), lhsT=Z["qgT"][:, h, :], rhs=Sx[:, h, :], start=True, stop=False)
                                ins = nc.tensor.matmul(ps(B2, 128, 64, h * 128), lhsT=Z["intraT"][:, h, :], rhs=Z["vnew"][:, h, :], start=False, stop=True)
                            return ins
                        T(mm9a, [bz, Z["b_S"]], [pb[B2]])
                        yield
                        A(lambda: nc.scalar.copy(out=Z["o"][:], in_=p3(B2, 64, 128)), [pb[B2]], [bz])
                        yield

                        def mm9b():
                            for h in range(4):
                                ins = nc.tensor.matmul(ps(B3, 128, 128, h * 128), lhsT=Z["kdec"][:, h, :], rhs=Z["vnew"][:, h, :], start=True, stop=True)
                            return ins
                        T(mm9b, [bz, Z["b_S"]], [pb[B3]])
                        yield
                        S.dma(S.sp, GO_d[b, d, c * 64:(c + 1) * 64, :], Z["o"][:].rearrange("p h c -> p (h c)"), reads=[bz], writes=[b_GO[b][d][c // 2]])
                        yield
                        V(lambda: nc.vector.tensor_tensor(out=Sx[:], in0=Sx[:], in1=Z["egl"][:].unsqueeze(2).to_broadcast([128, 4, 128]), op=ALU.mult), [bz, Z["b_S"]], [Z["b_S"]])
                        yield
                        V(lambda: nc.vector.tensor_tensor(out=Sx[:], in0=Sx[:], in1=p3(B3, 128, 128), op=ALU.add), [pb[B3], Z["b_S"]], [Z["b_S"]])
                        yield
                for step in range(36):
                    alive = [body(Z_, step) for Z_ in st_]
                    while alive:
                        for g_ in list(alive):
                            try:
                                next(g_)
                            except StopIteration:
                                alive.remove(g_)
                S.barrier()

        def gdn_final(b, l):
            with ExitStack() as ph:
                gn = sbt(ph, [128, 512]); b_w = Buf()
                for h in range(4):
                    S.dma(S.sp, gn[:, h * 128:(h + 1) * 128], gdn_ng[l].partition_broadcast(128), writes=[b_w])
                of = [sbt(ph, [128, 512]) for _ in range(2)]; ob = [sbt(ph, [128, 512]) for _ in range(2)]; zz = [sbt(ph, [128, 512]) for _ in range(2)]
                b_in = [Buf(), Buf()]
                sq = sbt(ph, [128, 512]); col = sbt(ph, [128, 8]); b_t = Buf()
                yt = [sbt(ph, [128, 512], BF16) for _ in range(2)]; b_yt = [Buf(), Buf()]
                for t in qtiles(l):
                    i = t % 2
                    S.dma(S.sp, of[i][:], GO_d[b, 0, t * 128:(t + 1) * 128, :], reads=[b_GO[b][0][t]], writes=[b_in[i]])
                    S.dma(S.act, ob[i][:], GO_d[b, 1, t * 128:(t + 1) * 128, :], reads=[b_GO[b][1][t]], writes=[b_in[i]])
                    S.dma(S.sp, zz[i][:], P_d[b, t * 128:(t + 1) * 128, O_GZ:O_GZ + 512], reads=[b_P[b][t]], writes=[b_in[i]])
                    V(lambda: nc.vector.tensor_tensor(out=of[i][:], in0=of[i][:], in1=ob[i][:], op=ALU.add), [b_in[i]], [b_in[i]])
                    V(lambda: nc.vector.tensor_tensor(out=sq[:], in0=of[i][:], in1=of[i][:], op=ALU.mult), [b_in[i]], [b_t])
                    V(lambda: nc.vector.reduce_sum(out=col[:, 0:4], in_=sq[:].rearrange("p (h c) -> p h c", c=128), axis=AX.X), [b_t], [b_t])
                    rstd_col(col[:, 4:8], col[:, 0:4], 1.0 / 128, EPS, b_t)
                    V(lambda: nc.vector.tensor_tensor(out=sq[:].rearrange("p (h c) -> p h c", c=128), in0=of[i][:].rearrange("p (h c) -> p h c", c=128),
                                                      in1=col[:, 4:8].unsqueeze(2).to_broadcast([128, 4, 128]), op=ALU.mult), [b_in[i], b_t], [b_t])
                    V(lambda: nc.vector.tensor_tensor(out=sq[:], in0=sq[:], in1=gn[:], op=ALU.mult), [b_t, b_w], [b_t])
                    A(lambda: nc.scalar.activation(out=zz[i][:], in_=zz[i][:], func=AF.Silu), [b_in[i]], [b_in[i]])
                    V(lambda: nc.vector.tensor_tensor(out=yt[i][:], in0=sq[:], in1=zz[i][:], op=ALU.mult), [b_t, b_in[i]], [b_yt[i]])
                    S.dma(S.sp, Y_d[b, t * 128:(t + 1) * 128, 1024:1536], yt[i][:], reads=[b_yt[i]], writes=[b_Y[b][t]])
                S.barrier()

        def phase_D(b, l):
            moe = (l % 2 == 1)
            with ExitStack() as ph:
                wo = sbt(ph, [128, 16, D], BF16); b_wo = Buf()
                for c4 in range(4):
                    S.dma(S.pool, wo[:, :, c4 * 512:(c4 + 1) * 512], w_out[l, :, c4 * 512:(c4 + 1) * 512].rearrange("(j p) n -> p j n", p=128), writes=[b_wo])
                Amul = sbt(ph, [128, D]); Badd = sbt(ph, [128, D]); Gmsa = sbt(ph, [128, D]); b_c = Buf()
                tmp = sbt(ph, [128, D]); b_tmp = Buf()
                yb = [sbt(ph, [128, D], BF16) for _ in range(2)]; b_yb = [Buf(), Buf()]
                yT = sbt(ph, [128, 16, 128], BF16); b_yT = Buf()
                xt = [sbt(ph, [128, D]) for _ in range(2)]; b_xt = [Buf(), Buf()]
                xm = sbt(ph, [128, D]); b_xm = Buf()
                hf = sbt(ph, [128, D]); b_hf = Buf()
                hb = sbt(ph, [128, D], BF16); b_hb = Buf()
                hT_ = [sbt(ph, [128, 16, 128], BF16) for _ in range(2)]; b_hT_ = [Buf(), Buf()]
                col = sbt(ph, [128, 16]); b_col = Buf()
                if moe:
                    rt = sbt(ph, [128, 16, 8]); b_rt = Buf()
                    S.dma(S.sp, rt[:], moe_router[l // 2].rearrange("(j p) e -> p j e", p=128), writes=[b_rt])
                    h32T = sbt(ph, [128, 16, 128]); b_h32T = Buf()
                    lg = sbt(ph, [128, 32]); b_lg = Buf()
                    gt = [sbt(ph, [128, 8]) for _ in range(2)]; b_gt = [Buf(), Buf()]
                for t in qtiles(l):
                    i = t % 2
                    if t == 0 or t == 2:
                        build_mod_consts((Amul, Badd, tmp), l, b, 0 if t == 0 else 1, norm2_g[l], 3, 4, b_c)
                        S.dma(S.sp, Gmsa[:], mod_row(l, b, 0 if t == 0 else 1, 2), reads=[b_mods], writes=[b_c])
                    S.dma(S.sp, yb[i][:], Y_d[b, t * 128:(t + 1) * 128, :], reads=[b_Y[b][t]], writes=[b_yb[i]])
                    S.dma(S.act, xt[i][:], xsrc(l, b, t), reads=xsrc_buf(l, b, t), writes=[b_xt[i]])
                    for g_ in range(2):
                        transpose_group(yT[:, g_ * 8:(g_ + 1) * 8, :], [yb[i][:, (g_ * 8 + j) * 128:(g_ * 8 + j + 1) * 128] for j in range(8)],
                                        4 + g_, b_yb[i], b_yT, evac="act" if g_ else "dve")

                    def mm():
                        for c4 in range(4):
                            for k in range(16):
                                ins = nc.tensor.matmul(ps(c4, 512), lhsT=yT[:, k, :], rhs=wo[:, k, c4 * 512:(c4 + 1) * 512], start=(k == 0), stop=(k == 15))
                        return ins
                    T(mm, [b_yT, b_wo], pb[0:4])
                    V(lambda: nc.vector.tensor_tensor(out=xm[:], in0=psall[:, 0:D], in1=Gmsa[:], op=ALU.mult), pb[0:4] + [b_c], [b_xm])
                    G(lambda: nc.gpsimd.tensor_tensor(out=xm[:], in0=xm[:], in1=xt[i][:], op=ALU.add), [b_xm, b_xt[i]], [b_xm])
                    S.dma(S.sp, resid_d[b, t * 128:(t + 1) * 128, :], xm[:], reads=[b_xm] + xsrc_buf(l, b, t), writes=[b_resid[b][t]])
                    A(lambda: nc.scalar.activation(out=hf[:], in_=xm[:], func=AF.Square, accum_out=col[:, 0:1]), [b_xm], [b_hf, b_col])
                    rstd_col(col[:, 1:2], col[:, 0:1], 1.0 / D, EPS, b_col)
                    V(lambda: nc.vector.scalar_tensor_tensor(out=hf[:], in0=xm[:], scalar=col[:, 1:2], in1=Amul[:], op0=ALU.mult, op1=ALU.mult), [b_xm, b_col, b_c], [b_hf])
                    V(lambda: nc.vector.tensor_tensor(out=hf[:], in0=hf[:], in1=Badd[:], op=ALU.add), [b_hf, b_c], [b_hf])
                    G(lambda: nc.gpsimd.tensor_copy(out=hb[:], in_=hf[:]), [b_hf], [b_hb])
                    for g_ in range(2):
                        transpose_group(hT_[i][:, g_ * 8:(g_ + 1) * 8, :], [hb[:, (g_ * 8 + j) * 128:(g_ * 8 + j + 1) * 128] for j in range(8)],
                                        4 + g_, b_hb, b_hT_[i], evac="act" if g_ else "dve")
                    S.dma(S.sp, H2T_d[b, :, :, t * 128:(t + 1) * 128], hT_[i][:], reads=[b_hT_[i]], writes=[b_H2T[b][t]])
                    if moe:
                        for g_ in range(4):
                            transpose_group(h32T[:, g_ * 4:(g_ + 1) * 4, :], [hf[:, (g_ * 4 + j) * 128:(g_ * 4 + j + 1) * 128] for j in range(4)],
                                            6 + g_ % 2, b_hf, b_h32T, dt_bf=False, evac="act" if g_ % 2 else "dve")

                        def mmr():
                            for k in range(16):
                                ins = nc.tensor.matmul(ps(6, 8), lhsT=h32T[:, k, :], rhs=rt[:, k, :], start=(k == 0), stop=(k == 15))
                            return ins
                        T(mmr, [b_h32T, b_rt], [pb[6]])
                        V(lambda: nc.vector.tensor_copy(out=lg[:, 0:8], in_=ps(6, 8)), [pb[6]], [b_lg])
                        V(lambda: nc.vector.reduce_max(out=col[:, 4:5], in_=lg[:, 0:8], axis=AX.X), [b_lg], [b_col])
                        V(lambda: nc.vector.tensor_scalar(out=lg[:, 8:16], in0=lg[:, 0:8], scalar1=col[:, 4:5], scalar2=-1e30, op0=ALU.is_ge, op1=ALU.mult), [b_lg, b_col], [b_lg])
                        V(lambda: nc.vector.tensor_tensor(out=lg[:, 8:16], in0=lg[:, 8:16], in1=lg[:, 0:8], op=ALU.add), [b_lg], [b_lg])
                        V(lambda: nc.vector.reduce_max(out=col[:, 5:6], in_=lg[:, 8:16], axis=AX.X), [b_lg], [b_col])
                        V(lambda: nc.vector.tensor_scalar(out=col[:, 6:7], in0=col[:, 4:5], scalar1=-1.0, scalar2=None, op0=ALU.mult), [b_col], [b_col])
                        A(lambda: nc.scalar.activation(out=lg[:, 16:24], in_=lg[:, 0:8], func=AF.Exp, bias=col[:, 6:7], scale=1.0), [b_lg, b_col], [b_lg])
                        V(lambda: nc.vector.tensor_scalar(out=lg[:, 24:32], in0=lg[:, 0:8], scalar1=col[:, 5:6], scalar2=None, op0=ALU.is_ge), [b_lg, b_col], [b_lg])
                        V(lambda: nc.vector.tensor_tensor(out=lg[:, 16:24], in0=lg[:, 16:24], in1=lg[:, 24:32], op=ALU.mult), [b_lg], [b_lg])
                        V(lambda: nc.vector.reduce_sum(out=col[:, 7:8], in_=lg[:, 16:24], axis=AX.X), [b_lg], [b_col])
                        V(lambda: nc.vector.reciprocal(out=col[:, 8:9], in_=col[:, 7:8]), [b_col], [b_col])
                        V(lambda: nc.vector.tensor_scalar(out=gt[i][:], in0=lg[:, 16:24], scalar1=col[:, 8:9], scalar2=None, op0=ALU.mult), [b_lg, b_col], [b_gt[i]])
                        S.dma(S.sp, GATES_d[b, t * 128:(t + 1) * 128, :], gt[i][:], reads=[b_gt[i]], writes=[b_GATES[b][t]])
                S.barrier()

        def phase_E(b, l, tiles):
            moe = (l % 2 == 1)
            last = (l == L - 1)
            nt = len(tiles)
            t0 = tiles[0]
            ntok = nt * 128
            FB = 256
            if ntok % 512 == 0:
                tblocks = [(o, 512) for o in range(0, ntok, 512)]
            else:
                tblocks = [(o, 384) for o in range(0, ntok, 384)]
            with ExitStack() as ph:
                h2T = sbt(ph, [128, 16, ntok], BF16); b_h2T = Buf()
                S.dma(S.sp, h2T[:], H2T_d[b, :, :, t0 * 128:t0 * 128 + ntok], reads=[b_H2T[b][t] for t in tiles], writes=[b_h2T])
                acc = sbt(ph, [128, nt, D]); b_acc = [Buf() for _ in range(nt)]
                if moe:
                    gts = sbt(ph, [128, nt, 8]); b_g = Buf()
                    S.dma(S.sp, gts[:], GATES_d[b, t0 * 128:t0 * 128 + ntok, :].rearrange("(n p) e -> p n e", p=128), reads=[b_GATES[b][t] for t in tiles], writes=[b_g])
                ph2 = ExitStack()
                NW = 3
                w1b = [sbt(ph2, [128, 16, FB], BF16) for _ in range(2)]; w3b = [sbt(ph2, [128, 16, FB], BF16) for _ in range(2)]
                b_w13 = [Buf(), Buf()]
                w2b = [sbt(ph2, [128, FB // 128, D], BF16) for _ in range(NW)]; b_wb = [Buf() for _ in range(NW)]
                sg = [sbt(ph2, [128, 512]) for _ in range(2)]; b_sg = [Buf(), Buf()]
                aT = [sbt(ph2, [128, FB // 128, ntok], BF16) for _ in range(2)]; b_aT = [Buf(), Buf()]
                if moe:
                    experts = [(moe_w1[l // 2, e], moe_w3[l // 2, e], moe_w2[l // 2, e], EXD, e) for e in range(8)]
                else:
                    experts = [(ffn_w1[l // 2], ffn_w3[l // 2], ffn_w2[l // 2], FFN, None)]
                fbs = [(W1, W3, W2, fb * FB, e) for (W1, W3, W2, F_, e) in experts for fb in range(F_ // FB)]
                nfb = len(fbs)
                cnt = dict(gu=0, ob=0)

                def emit_gu(it, o_, w_, fc):
                    wi = it % 2
                    ai = it % 2
                    bg_, bu_ = (0, 1) if cnt["gu"] % 2 == 0 else (2, 3)
                    si = cnt["gu"] % 2
                    cnt["gu"] += 1

                    def mmgu():
                        for k in range(16):
                            nc.tensor.matmul(ps(bg_, w_), lhsT=w1b[wi][:, k, fc * 128:(fc + 1) * 128], rhs=h2T[:, k, o_:o_ + w_], start=(k == 0), stop=(k == 15))
                        for k in range(16):
                            ins = nc.tensor.matmul(ps(bu_, w_), lhsT=w3b[wi][:, k, fc * 128:(fc + 1) * 128], rhs=h2T[:, k, o_:o_ + w_], start=(k == 0), stop=(k == 15))
                        return ins
                    T(mmgu, [b_w13[wi], b_h2T], [pb[bg_], pb[bu_]])
                    A(lambda: nc.scalar.activation(out=sg[si][:, 0:w_], in_=ps(bg_, w_), func=AF.Silu), [pb[bg_]], [b_sg[si]])
                    V(lambda: nc.vector.tensor_tensor(out=aT[ai][:, fc, o_:o_ + w_], in0=sg[si][:, 0:w_], in1=ps(bu_, w_), op=ALU.mult), [pb[bu_], b_sg[si]], [b_aT[ai]])

                def emit_w2(it, ti, dblk):
                    wi = it % NW
                    ai = it % 2
                    e = fbs[it][4]
                    first = (it == 0)
                    bo = 4 + cnt["ob"] % 4
                    cnt["ob"] += 1

                    def mm2_():
                        for fc in range(FB // 128):
                            ins = nc.tensor.matmul(ps(bo, 512), lhsT=aT[ai][:, fc, ti * 128:(ti + 1) * 128], rhs=w2b[wi][:, fc, dblk * 512:(dblk + 1) * 512],
                                                   start=(fc == 0), stop=(fc == FB // 128 - 1))
                        return ins
                    T(mm2_, [b_aT[ai], b_wb[wi]], [pb[bo]])
                    dst = acc[:, ti, dblk * 512:(dblk + 1) * 512]
                    if moe:
                        gcol_ = gts[:, ti, e:e + 1]
                        if first:
                            V(lambda: nc.vector.tensor_scalar(out=dst, in0=ps(bo, 512), scalar1=gcol_, scalar2=None, op0=ALU.mult), [pb[bo], b_g], [b_acc[ti]])
                        else:
                            V(lambda: nc.vector.scalar_tensor_tensor(out=dst, in0=ps(bo, 512), scalar=gcol_, in1=dst, op0=ALU.mult, op1=ALU.add), [pb[bo], b_g, b_acc[ti]], [b_acc[ti]])
                    else:
                        if first:
                            V(lambda: nc.vector.tensor_copy(out=dst, in_=ps(bo, 512)), [pb[bo]], [b_acc[ti]])
                        else:
                            V(lambda: nc.vector.tensor_tensor(out=dst, in0=dst, in1=ps(bo, 512), op=ALU.add), [pb[bo], b_acc[ti]], [b_acc[ti]])

                for it in range(nfb + 1):
                    gu_list = []
                    w2_list = []
                    if it < nfb:
                        (W1, W3, W2, f0, e_) = fbs[it]
                        wi = it % NW
                        S.dma(S.pool, w1b[it % 2][:], W1[:, f0:f0 + FB].rearrange("(j p) n -> p j n", p=128), writes=[b_w13[it % 2]])
                        S.dma(S.pool, w3b[it % 2][:], W3[:, f0:f0 + FB].rearrange("(j p) n -> p j n", p=128), writes=[b_w13[it % 2]])
                        S.dma(S.pool, w2b[wi][:], W2[f0:f0 + FB, :].rearrange("(c p) n -> p c n", p=128), writes=[b_wb[wi]])
                        gu_list = [(o_, w_, fc) for (o_, w_) in tblocks for fc in range(FB // 128)]
                    if it >= 1:
                        w2_list = [(ti, dblk) for ti in range(nt) for dblk in range(4)]
                    ng = max(1, len(gu_list))
                    per = (len(w2_list) + ng - 1) // ng
                    wj = 0
                    for gi in range(ng):
                        if gi < len(gu_list):
                            emit_gu(it, *gu_list[gi])
                        for _ in range(per):
                            if wj < len(w2_list):
                                emit_w2(it - 1, *w2_list[wj])
                                wj += 1
                    while wj < len(w2_list):
                        emit_w2(it - 1, *w2_list[wj])
                        wj += 1
                S.barrier()
                ph2.close()
                G5 = sbt(ph, [128, D]); b_c = Buf()
                xt = [sbt(ph, [128, D]) for _ in range(2)]; b_xt = [Buf(), Buf()]
                if last:
                    fg = sbt(ph, [128, D]); col = sbt(ph, [128, 4]); b_col = Buf()
                    S.dma(S.sp, fg[:], final_g.partition_broadcast(128), writes=[b_c])
                cur_stream = None
                for ti, t in enumerate(tiles):
                    i = ti % 2
                    stream = 0 if t < 2 else 1
                    if stream != cur_stream:
                        S.dma(S.sp, G5[:], mod_row(l, b, stream, 5), reads=[b_mods], writes=[b_c])
                        cur_stream = stream
                    S.dma(S.sp, xt[i][:], resid_d[b, t * 128:(t + 1) * 128, :], reads=[b_resid[b][t]], writes=[b_xt[i]])
                    V(lambda: nc.vector.tensor_tensor(out=acc[:, ti, :], in0=acc[:, ti, :], in1=G5[:], op=ALU.mult), [b_acc[ti], b_c], [b_acc[ti]])
                    G(lambda: nc.gpsimd.tensor_tensor(out=xt[i][:], in0=xt[i][:], in1=acc[:, ti, :], op=ALU.add), [b_acc[ti], b_xt[i]], [b_xt[i]])
                    if not last:
                        S.dma(S.sp, resid_d[b, t * 128:(t + 1) * 128, :], xt[i][:], reads=[b_xt[i]], writes=[b_resid[b][t]])
                    else:
                        A(lambda: nc.scalar.activation(out=acc[:, ti, :], in_=xt[i][:], func=AF.Square, accum_out=col[:, 0:1]), [b_xt[i]], [b_acc[ti], b_col])
                        rstd_col(col[:, 1:2], col[:, 0:1], 1.0 / D, EPS, b_col)
                        V(lambda: nc.vector.scalar_tensor_tensor(out=xt[i][:], in0=xt[i][:], scalar=col[:, 1:2], in1=fg[:], op0=ALU.mult, op1=ALU.mult), [b_col, b_c, b_xt[i]], [b_xt[i]])
                        S.dma(S.sp, out_d[b, (t - 2) * 128:(t - 1) * 128, :], xt[i][:], reads=[b_xt[i]], writes=[b_out])
                S.barrier()

        if start is None:
            prologue()
        for l in range(L):
            if start is None:
                for b in range(n_seq):
                    with ExitStack() as ph0:
                        hT = sbt(ph0, [128, 16, NT], BF16); b_hT = Buf()
                        phase_A(b, l, hT, b_hT)
                        phase_B(b, l, hT, b_hT)
                        S.barrier()
            if stop == "B":
                break
            for b in range(n_seq):
                if only in (None, "mla"):
                    mixer_mla(b, l)
                if only in (None, "gqa"):
                    mixer_gqa_swa(b, l, False)
                if only in (None, "swa"):
                    mixer_gqa_swa(b, l, True)
            if only in (None, "gdn"):
                for b in range(n_seq):
                    gdn_prep(b, l)
                gdn_scan(list(range(n_seq)), l)
                for b in range(n_seq):
                    gdn_final(b, l)
            if stop == "mix":
                break
            for b in range(n_seq):
                phase_D(b, l)
                if stop == "D":
                    continue
                tl = list(qtiles(l))
                half = len(tl) // 2
                phase_E(b, l, tl[:half])
                phase_E(b, l, tl[half:])
            if stop == "D":
                break
        S.finish()
    return nc


def _host_consts():
    def tables(rot):
        rows = 2048 // 64
        row = np.repeat(np.arange(rows), 64).astype(np.float32)
        colp = np.tile(np.arange(64), rows).astype(np.float32)
        half = rot // 2
        inv = (10000.0 ** (-np.arange(0, half, 2, dtype=np.float32) / half)).astype(np.float32)
        ar = row[:, None] * inv
        ac = colp[:, None] * inv
        ang = np.concatenate([ar, ar, ac, ac], -1).astype(np.float32)
        q = rot // 4
        sign = np.concatenate([-np.ones(q), np.ones(q), -np.ones(q), np.ones(q)]).astype(np.float32)
        return np.cos(ang).astype(np.float32), (np.sin(ang) * sign[None]).astype(np.float32)
    c128, s128 = tables(128)
    c64, s64 = tables(64)
    cst = np.zeros((128, C_W), np.float32)
    i = np.arange(128)[:, None]
    j = np.arange(128)[None, :]
    cst[:, C_ID:C_ID + 128] = (i == j)
    cst[:, C_ONE:C_ONE + 128] = 1.0
    cst[:, C_SU:C_SU + 128] = np.where(j >= i, 0.0, NEG)
    cst[:, C_SL:C_SL + 128] = np.where(j <= i, 0.0, NEG)
    p = np.arange(64)[:, None]
    f = np.arange(64)[None, :]
    cst[0:64, C_U64:C_U64 + 64] = (p <= f)
    cst[0:64, C_L64:C_L64 + 64] = (p >= f)
    for d in range(2):
        vN = (f < p) if d == 0 else (f > p)
        vM = (f >= p) if d == 0 else (f <= p)
        vS = (f > p) if d == 0 else (f < p)
        cst[0:64, C_MN[d]:C_MN[d] + 256] = np.tile(np.where(vN, 0.0, NEG), (1, 4))
        cst[0:64, C_MM[d]:C_MM[d] + 256] = np.tile(np.where(vM, 0.0, NEG), (1, 4))
        cst[0:64, C_ST[d]:C_ST[d] + 256] = np.tile(vS.astype(np.float32), (1, 4))
    return dict(rc128=np.tile(c128, (1, 4)), rs128=np.tile(s128, (1, 4)), rc64=np.tile(c64, (1, 4)), rs64=np.tile(s64, (1, 4)), cst=cst)


_PROG = {}


def kernel(**inputs):
    n_cores = 8
    n_seq = 2
    if "full" not in _PROG:
        _PROG["full"] = build_program(n_seq=n_seq, n_layers=4)
    nc = _PROG["full"]
    consts = _host_consts()
    shared = {k: np.ascontiguousarray(v) for k, v in inputs.items() if k not in ("x", "c", "ctx")}
    shared["gdn_a_log"] = np.ascontiguousarray(inputs["gdn_a_log"]).reshape(4, 8)
    shared["gdn_dt_bias"] = np.ascontiguousarray(inputs["gdn_dt_bias"]).reshape(4, 8)
    shared.update(consts)
    in_maps = []
    for i in range(n_cores):
        m = dict(shared)
        m["x"] = np.ascontiguousarray(inputs["x"][i * n_seq:(i + 1) * n_seq])
        m["c"] = np.ascontiguousarray(inputs["c"][i * n_seq:(i + 1) * n_seq])
        m["ctx"] = np.ascontiguousarray(inputs["ctx"][i * n_seq:(i + 1) * n_seq])
        in_maps.append(m)
    res = run_bass_kernel_spmd(nc, in_maps, core_ids=list(range(n_cores)))
    return np.concatenate([np.asarray(r["out"]) for r in res.results], axis=0).astype(np.float32)
```
